# Optimizing a Trainium2 kernel written in Bass

```python
import math
import jax, jax.numpy as jnp
from jax import lax
import numpy as np

D_MODEL = 2048
BATCH = 2
SEQ = 16384
DEPTH = 2

D_ATTN = D_MODEL
D_SSM = D_MODEL
D_MIX = D_ATTN + D_SSM
HEAD_DIM = 128
N_HEADS_ATTN = D_ATTN // HEAD_DIM
N_KV_GROUPS = 2
HEADS_PER_GROUP = N_HEADS_ATTN // N_KV_GROUPS
KV_WIDTH = N_KV_GROUPS * HEAD_DIM
CMP_BLOCK = 32
CMP_STRIDE = 16
CMP_RATIO = CMP_BLOCK // CMP_STRIDE
CMP_HIDDEN = 256
SLC_BLOCK = 64
N_SELECT = 16
WINDOW = 512
Q_BLOCK = 128
ROPE_THETA = 10000.0
NEG_INF = -1e30
SEL_FORCED = 1e4
SSM_HEAD_DIM = 64
N_HEADS_SSM = D_SSM // SSM_HEAD_DIM
N_SSM_GROUPS = 8
SSM_HEADS_PER_GROUP = N_HEADS_SSM // N_SSM_GROUPS
D_STATE = 128
CONV_WIDTH = 4
CONV_DIM = D_SSM + 2 * N_SSM_GROUPS * D_STATE
SSD_CHUNK = 256
PROJ_WIDTHS = (D_ATTN, KV_WIDTH, KV_WIDTH, KV_WIDTH, KV_WIDTH, KV_WIDTH, KV_WIDTH,
               3 * N_HEADS_ATTN, D_ATTN, CONV_DIM, N_HEADS_SSM, D_SSM)
D_PROJ = sum(PROJ_WIDTHS)
POS_OFFSET_MAX = 4096

kernel_name = "hymba_nsa_mamba2_hybrid"


def rmsnorm(x, w, eps=1e-6):
    xf = x.astype(jnp.float32)
    y = xf * lax.rsqrt(jnp.mean(xf * xf, axis=-1, keepdims=True) + eps)
    return y.astype(x.dtype) * w


def grouped_rmsnorm(x, w, n_groups, eps=1e-6):
    shp = x.shape
    xg = x.reshape(shp[:-1] + (n_groups, shp[-1] // n_groups)).astype(jnp.float32)
    y = xg * lax.rsqrt(jnp.mean(xg * xg, axis=-1, keepdims=True) + eps)
    return y.reshape(shp).astype(x.dtype) * w


def rope(u, pos):
    half = u.shape[-1] // 2
    inv_freq = ROPE_THETA ** (-jnp.arange(half, dtype=jnp.float32) / half)
    ang = pos.astype(jnp.float32)[..., None] * inv_freq
    cos = jnp.cos(ang)[:, :, None, :]
    sin = jnp.sin(ang)[:, :, None, :]
    u1 = u[..., :half].astype(jnp.float32)
    u2 = u[..., half:].astype(jnp.float32)
    return jnp.concatenate([u1 * cos - u2 * sin, u2 * cos + u1 * sin], axis=-1).astype(u.dtype)


def split_columns(proj):
    offsets = np.cumsum(PROJ_WIDTHS)[:-1].tolist()
    return jnp.split(proj, offsets, axis=-1)


def compress_blocks(u, pe, w1, w2):
    b, s, g, d = u.shape
    n_chunks = s // CMP_STRIDE
    n_cmp = n_chunks - CMP_RATIO + 1
    chunks = u.reshape(b, n_chunks, CMP_STRIDE, g, d)
    pe = pe.reshape(CMP_RATIO, CMP_STRIDE, 1, d)
    w1 = w1.reshape(CMP_RATIO, CMP_STRIDE, d, CMP_HIDDEN)
    hid = sum(jnp.einsum("bcigd,idh->bcgh", chunks[:, r:r + n_cmp] + pe[r], w1[r])
              for r in range(CMP_RATIO))
    return jnp.einsum("bcgh,hd->bcgd", jax.nn.silu(hid), w2)


def nsa_attention(q, k_cmp, v_cmp, k_slc, v_slc, k_win, v_win, gates):
    b, s = q.shape[:2]
    g, r, dk = N_KV_GROUPS, HEADS_PER_GROUP, HEAD_DIM
    n_cmp = k_cmp.shape[1]
    n_slc = s // SLC_BLOCK
    n_sel = min(N_SELECT, n_slc)
    slc_ratio = SLC_BLOCK // CMP_STRIDE
    n_overlap = slc_ratio + CMP_RATIO - 1
    pad_left = CMP_RATIO - 1
    pad_right = slc_ratio * (n_slc - 1) + n_overlap - (n_cmp + pad_left)
    scale = dk ** -0.5
    qg = q.reshape(b, s, g, r, dk)
    gates = gates.reshape(b, s, g, r, 3)
    kb = jnp.moveaxis(k_slc.reshape(b, n_slc, SLC_BLOCK, g, dk), 3, 1)
    vb = jnp.moveaxis(v_slc.reshape(b, n_slc, SLC_BLOCK, g, dk), 3, 1)
    k_wp = jnp.pad(k_win, ((0, 0), (WINDOW, 0), (0, 0), (0, 0)))
    v_wp = jnp.pad(v_win, ((0, 0), (WINDOW, 0), (0, 0), (0, 0)))
    cmp_end = jnp.arange(n_cmp) * CMP_STRIDE + CMP_BLOCK - 1
    blk_ids = jnp.arange(n_slc)
    b_ix = jnp.arange(b)[:, None, None, None]
    g_ix = jnp.arange(g)[None, :, None, None]

    def query_block(qi):
        t0 = qi * Q_BLOCK
        qb = lax.dynamic_slice_in_dim(qg, t0, Q_BLOCK, axis=1)
        t = t0 + jnp.arange(Q_BLOCK)
        s_c = jnp.einsum("bqgrd,bngd->bgrqn", qb, k_cmp).astype(jnp.float32) * scale
        m_c = cmp_end[None, :] <= t[:, None]
        p_c = jax.nn.softmax(jnp.where(m_c, s_c, NEG_INF), axis=-1) * m_c
        o_c = jnp.einsum("bgrqn,bngd->bqgrd", p_c.astype(v_cmp.dtype), v_cmp)
        imp = jnp.pad(p_c.sum(axis=2), ((0, 0), (0, 0), (0, 0), (pad_left, pad_right)))
        imp_s = sum(imp[..., m:m + slc_ratio * (n_slc - 1) + 1:slc_ratio] for m in range(n_overlap))
        cur = t // SLC_BLOCK
        forced = (blk_ids[None] == 0) | (blk_ids[None] == cur[:, None]) | (blk_ids[None] == cur[:, None] - 1)
        visible = blk_ids[None] <= cur[:, None]
        score = jnp.where(forced, SEL_FORCED, jnp.where(visible, imp_s, -SEL_FORCED))
        _, idx = lax.top_k(score, n_sel)
        ks = kb[b_ix, g_ix, idx]
        vs = vb[b_ix, g_ix, idx]
        s_s = jnp.einsum("bqgrd,bgqnld->bgrqnl", qb, ks).astype(jnp.float32) * scale
        tok = idx[..., None] * SLC_BLOCK + jnp.arange(SLC_BLOCK)
        m_s = (tok <= t[None, None, :, None, None])[:, :, None]
        s_s = jnp.where(m_s, s_s, NEG_INF).reshape(b, g, r, Q_BLOCK, n_sel * SLC_BLOCK)
        p_s = jax.nn.softmax(s_s, axis=-1)
        o_s = jnp.einsum("bgrqm,bgqmd->bqgrd", p_s.astype(vs.dtype),
                         vs.reshape(b, g, Q_BLOCK, n_sel * SLC_BLOCK, dk))
        kw = lax.dynamic_slice_in_dim(k_wp, t0, WINDOW + Q_BLOCK, axis=1)
        vw = lax.dynamic_slice_in_dim(v_wp, t0, WINDOW + Q_BLOCK, axis=1)
        pos_k = t0 - WINDOW + jnp.arange(WINDOW + Q_BLOCK)
        m_w = ((pos_k[None] <= t[:, None]) & (pos_k[None] > t[:, None] - WINDOW) & (pos_k[None] >= 0))
        s_w = jnp.einsum("bqgrd,bkgd->bgrqk", qb, kw).astype(jnp.float32) * scale
        p_w = jax.nn.softmax(jnp.where(m_w, s_w, NEG_INF), axis=-1)
        o_w = jnp.einsum("bgrqk,bkgd->bqgrd", p_w.astype(vw.dtype), vw)
        gb = lax.dynamic_slice_in_dim(gates, t0, Q_BLOCK, axis=1)
        o = gb[..., 0:1] * o_c + gb[..., 1:2] * o_s + gb[..., 2:3] * o_w
        return o.astype(q.dtype)

    out = lax.map(query_block, jnp.arange(s // Q_BLOCK))
    return jnp.moveaxis(out, 0, 1).reshape(b, s, N_HEADS_ATTN * dk)


def causal_depthwise_conv(u, w, bias):
    y = lax.conv_general_dilated(u, w[:, None, :], window_strides=(1,), padding=[(CONV_WIDTH - 1, 0)],
                                 dimension_numbers=("NWC", "WIO", "NWC"), feature_group_count=u.shape[-1])
    return y + bias


def ssd_chunked(x, dt, a, bmat, cmat):
    b, s = x.shape[:2]
    chunk = math.gcd(s, SSD_CHUNK)
    nc = s // chunk
    gs, r = N_SSM_GROUPS, SSM_HEADS_PER_GROUP
    xdt = (x * dt[..., None]).reshape(b, nc, chunk, gs, r, SSM_HEAD_DIM)
    da = (dt * a).reshape(b, nc, chunk, gs, r)
    bc = bmat.reshape(b, nc, chunk, gs, D_STATE)
    cc = cmat.reshape(b, nc, chunk, gs, D_STATE)
    causal = jnp.tril(jnp.ones((chunk, chunk), bool))[None, :, :, None, None]

    def step(state, inp):
        xk, dak, bk, ck = inp
        cum = jnp.cumsum(dak, axis=1)
        decay = jnp.exp(jnp.where(causal, cum[:, :, None] - cum[:, None], -jnp.inf))
        w_diag = jnp.einsum("blgn,bsgn->blsg", ck, bk)[..., None] * decay
        y = jnp.einsum("blsgr,bsgrp->blgrp", w_diag, xk)
        y = y + jnp.einsum("blgn,bgrpn->blgrp", ck, state) * jnp.exp(cum)[..., None]
        last = cum[:, -1]
        x_end = jnp.exp(last[:, None] - cum)[..., None] * xk
        state = state * jnp.exp(last)[..., None, None] + jnp.einsum("blgn,blgrp->bgrpn", bk, x_end)
        return state, y

    state0 = jnp.zeros((b, gs, r, SSM_HEAD_DIM, D_STATE), jnp.float32)
    xs = (jnp.moveaxis(xdt, 1, 0), jnp.moveaxis(da, 1, 0), jnp.moveaxis(bc, 1, 0), jnp.moveaxis(cc, 1, 0))
    _, y = lax.scan(step, state0, xs)
    return jnp.moveaxis(y, 0, 1).reshape(b, s, N_HEADS_SSM, SSM_HEAD_DIM)


def hybrid_layer(x, positions, pos_cmp, norm_w, w_in, pe_k, w1_k, w2_k, pe_v, w1_v, w2_v,
                 conv_w, conv_b, dt_bias, a_log, d_skip, norm_attn_w, norm_ssm_w, w_out):
    b, s, _ = x.shape
    h = rmsnorm(x, norm_w)
    proj = jnp.einsum("bsd,dp->bsp", h, w_in)
    (q, kc, vc, ks, vs, kw, vw, gate, z_attn, xbc, dt, z_ssm) = split_columns(proj)
    kv_shape = (b, s, N_KV_GROUPS, HEAD_DIM)
    q = rope(q.reshape(b, s, N_HEADS_ATTN, HEAD_DIM), positions)
    k_slc = rope(ks.reshape(kv_shape), positions)
    k_win = rope(kw.reshape(kv_shape), positions)
    k_cmp = rope(compress_blocks(kc.reshape(kv_shape), pe_k, w1_k, w2_k), pos_cmp)
    v_cmp = compress_blocks(vc.reshape(kv_shape), pe_v, w1_v, w2_v)
    gates = jax.nn.sigmoid(gate.astype(jnp.float32)).reshape(b, s, N_HEADS_ATTN, 3)
    o_attn = nsa_attention(q, k_cmp, v_cmp, k_slc, vs.reshape(kv_shape), k_win, vw.reshape(kv_shape), gates)
    y_attn = rmsnorm(o_attn * jax.nn.silu(z_attn), norm_attn_w)
    xbc = jax.nn.silu(causal_depthwise_conv(xbc, conv_w, conv_b))
    xs_, bm, cm = jnp.split(xbc, [D_SSM, D_SSM + N_SSM_GROUPS * D_STATE], axis=-1)
    xs_ = xs_.reshape(b, s, N_HEADS_SSM, SSM_HEAD_DIM).astype(jnp.float32)
    dt = jax.nn.softplus(dt.astype(jnp.float32) + dt_bias.astype(jnp.float32))
    a = -jnp.exp(a_log.astype(jnp.float32))
    y = ssd_chunked(xs_, dt, a,
                    bm.reshape(b, s, N_SSM_GROUPS, D_STATE).astype(jnp.float32),
                    cm.reshape(b, s, N_SSM_GROUPS, D_STATE).astype(jnp.float32))
    y = (y + d_skip.astype(jnp.float32)[:, None] * xs_).reshape(b, s, D_SSM).astype(x.dtype)
    y_ssm = grouped_rmsnorm(y * jax.nn.silu(z_ssm), norm_ssm_w, N_SSM_GROUPS)
    mixed = jnp.concatenate([y_attn, y_ssm], axis=-1)
    return x + jnp.einsum("bsm,md->bsd", mixed, w_out)


def setup_inputs(seed: int = 0) -> dict:
    key = jax.random.key(seed)
    ks = jax.random.split(key, 24)
    f32 = jnp.float32

    def nrm(k, shape, scale):
        return jax.random.normal(k, shape, f32) * scale

    x = nrm(ks[0], (BATCH, SEQ, D_MODEL), 1.0)
    positions = (jax.random.randint(ks[1], (BATCH, 1), 0, POS_OFFSET_MAX, dtype=jnp.int32)
                 + jnp.arange(SEQ, dtype=jnp.int32)[None])
    norm_w = 1.0 + nrm(ks[2], (DEPTH, D_MODEL), 0.02)
    w_in = nrm(ks[3], (DEPTH, D_MODEL, D_PROJ), D_MODEL ** -0.5)
    cmp_pe_k = nrm(ks[4], (DEPTH, CMP_BLOCK, HEAD_DIM), 0.1)
    cmp_w1_k = nrm(ks[5], (DEPTH, CMP_BLOCK, HEAD_DIM, CMP_HIDDEN), (CMP_BLOCK * HEAD_DIM) ** -0.5)
    cmp_w2_k = nrm(ks[6], (DEPTH, CMP_HIDDEN, HEAD_DIM), CMP_HIDDEN ** -0.5)
    cmp_pe_v = nrm(ks[7], (DEPTH, CMP_BLOCK, HEAD_DIM), 0.1)
    cmp_w1_v = nrm(ks[8], (DEPTH, CMP_BLOCK, HEAD_DIM, CMP_HIDDEN), (CMP_BLOCK * HEAD_DIM) ** -0.5)
    cmp_w2_v = nrm(ks[9], (DEPTH, CMP_HIDDEN, HEAD_DIM), CMP_HIDDEN ** -0.5)
    conv_w = nrm(ks[10], (DEPTH, CONV_WIDTH, CONV_DIM), CONV_WIDTH ** -0.5)
    conv_b = nrm(ks[11], (DEPTH, CONV_DIM), 0.02)
    dt0 = jnp.exp(jax.random.uniform(ks[12], (DEPTH, N_HEADS_SSM), f32, math.log(1e-3), math.log(1e-1)))
    dt_bias = dt0 + jnp.log(-jnp.expm1(-dt0))
    a_log = jnp.log(jax.random.uniform(ks[13], (DEPTH, N_HEADS_SSM), f32, 1.0, 16.0))
    d_skip = 1.0 + nrm(ks[14], (DEPTH, N_HEADS_SSM), 0.02)
    norm_attn_w = 1.0 + nrm(ks[15], (DEPTH, D_ATTN), 0.02)
    norm_ssm_w = 1.0 + nrm(ks[16], (DEPTH, D_SSM), 0.02)
    w_out = nrm(ks[17], (DEPTH, D_MIX, D_MODEL), D_MIX ** -0.5)
    final_norm_w = 1.0 + nrm(ks[18], (D_MODEL,), 0.02)
    return {"x": x, "positions": positions, "norm_w": norm_w, "w_in": w_in,
            "cmp_pe_k": cmp_pe_k, "cmp_w1_k": cmp_w1_k, "cmp_w2_k": cmp_w2_k,
            "cmp_pe_v": cmp_pe_v, "cmp_w1_v": cmp_w1_v, "cmp_w2_v": cmp_w2_v,
            "conv_w": conv_w, "conv_b": conv_b, "dt_bias": dt_bias, "a_log": a_log,
            "d_skip": d_skip, "norm_attn_w": norm_attn_w, "norm_ssm_w": norm_ssm_w,
            "w_out": w_out, "final_norm_w": final_norm_w}


def reference(x, positions, norm_w, w_in, cmp_pe_k, cmp_w1_k, cmp_w2_k, cmp_pe_v, cmp_w1_v, cmp_w2_v,
              conv_w, conv_b, dt_bias, a_log, d_skip, norm_attn_w, norm_ssm_w, w_out, final_norm_w):
    pos_cmp = positions[:, CMP_BLOCK - 1::CMP_STRIDE]
    for l in range(DEPTH):
        x = hybrid_layer(x, positions, pos_cmp, norm_w[l], w_in[l],
                         cmp_pe_k[l], cmp_w1_k[l], cmp_w2_k[l], cmp_pe_v[l], cmp_w1_v[l], cmp_w2_v[l],
                         conv_w[l], conv_b[l], dt_bias[l], a_log[l], d_skip[l],
                         norm_attn_w[l], norm_ssm_w[l], w_out[l])
    return rmsnorm(x, final_norm_w)
```

```python
import math, os
from contextlib import ExitStack
import numpy as np
import ml_dtypes
import concourse.bass as bass
import concourse.mybir as mybir
from concourse.bass_utils import run_bass_kernel_spmd

F32 = mybir.dt.float32
BF16 = mybir.dt.bfloat16
I32 = mybir.dt.int32
AF = mybir.ActivationFunctionType
ALU = mybir.AluOpType
AX = mybir.AxisListType
NPBF = ml_dtypes.bfloat16

D_MODEL = 2048
DEPTH = 2
HEAD_DIM = 128
D_PROJ = 11856
EPS = 1e-6
OFF_Q, OFF_KC, OFF_VC, OFF_KS, OFF_VS, OFF_KW, OFF_VW = 0, 2048, 2304, 2560, 2816, 3072, 3328
OFF_GATE, OFF_ZA, OFF_X, OFF_B, OFF_C, OFF_DT, OFF_ZS = 3584, 3632, 5680, 7728, 8752, 9776, 9808
KC = D_MODEL // 128


class Prog:
    STREAMS = ("pe", "act", "dve", "pool", "sp")

    def __init__(self, nc, es, n_dma_sems=10):
        self.nc, self.es = nc, es
        self.ops = {s: [] for s in self.STREAMS}
        self.sems = []
        self.csem = {}
        for s in ("pe", "act", "dve", "pool"):
            self.csem[s] = self._new_sem("c_" + s)
        self.ccnt = {s: 0 for s in self.csem}
        self.dsem = {q: [[self._new_sem(f"d_{q}{i}"), 0] for i in range(n_dma_sems)]
                     for q in ("sp", "act", "pool")}
        self.drr = {q: 0 for q in self.dsem}
        self.lastw = {}
        self.readers = {}
        self.known = {s: {} for s in self.STREAMS}
        self.nsb = 0

    def _new_sem(self, name):
        h = self.es.enter_context(self.nc.semaphore(name))
        self.sems.append(h)
        return len(self.sems) - 1

    def sb(self, shape, dt, name=None):
        self.nsb += 1
        return self.es.enter_context(self.nc.sbuf_tensor(name or f"sb{self.nsb}", list(shape), dt))

    def ps(self, shape, dt, name=None):
        self.nsb += 1
        return self.es.enter_context(self.nc.psum_tensor(name or f"ps{self.nsb}", list(shape), dt))

    def _emit(self, stream, fn, reads, writes, dma):
        deps = []
        for b in reads:
            w = self.lastw.get(b)
            if w:
                deps.append(w)
        for b in writes:
            w = self.lastw.get(b)
            if w:
                deps.append(w)
            deps.extend(self.readers.get(b, {}).items())
        if dma:
            pool = self.dsem[stream]
            slot = pool[self.drr[stream] % len(pool)]
            self.drr[stream] += 1
            if slot[1] > 0:
                deps.append((slot[0], slot[1]))
            slot[1] += 16
            tok = (slot[0], slot[1])
            inc = 16
        else:
            self.ccnt[stream] += 1
            tok = (self.csem[stream], self.ccnt[stream])
            inc = 1
        waits = {}
        kn = self.known[stream]
        for sid, v in deps:
            if stream == "pe" and not dma and sid == self.csem["pe"]:
                continue
            if kn.get(sid, 0) >= v:
                continue
            if waits.get(sid, 0) < v:
                waits[sid] = v
        for sid, v in waits.items():
            kn[sid] = v
        self.ops[stream].append((sorted(waits.items()), fn, tok[0], inc))
        for b in reads:
            r = self.readers.setdefault(b, {})
            if r.get(tok[0], 0) < tok[1]:
                r[tok[0]] = tok[1]
        for b in writes:
            self.lastw[b] = tok
            self.readers[b] = {}
        return tok

    def op(self, stream, fn, reads=(), writes=()):
        return self._emit(stream, fn, reads, writes, False)

    def dma(self, queue, out, in_, reads=(), writes=(), **kw):
        return self._emit(queue, lambda e: e.dma_start(out=out, in_=in_, **kw), reads, writes, True)

    def check(self):
        val = [0] * len(self.sems)
        pc = {s: 0 for s in self.STREAMS}
        prog = True
        while prog:
            prog = False
            for s in self.STREAMS:
                ops = self.ops[s]
                while pc[s] < len(ops):
                    waits, fn, sid, inc = ops[pc[s]]
                    if all(val[ws] >= wv for ws, wv in waits):
                        val[sid] += inc
                        pc[s] += 1
                        prog = True
                    else:
                        break
        stuck = {s: (pc[s], len(self.ops[s])) for s in self.STREAMS if pc[s] < len(self.ops[s])}
        if stuck:
            msg = []
            for s, (i, n) in stuck.items():
                waits = self.ops[s][i][0]
                msg.append(f"{s} stuck at {i}/{n}: waits {[(ws, wv, val[ws]) for ws, wv in waits]}")
            raise RuntimeError("DEADLOCK: " + "; ".join(msg))

    def finish(self):
        self.check()
        finals = []
        for q in self.dsem:
            for sid, v in self.dsem[q]:
                if v > 0:
                    finals.append((sid, v))
        for s in self.csem:
            if self.ccnt[s] > 0:
                finals.append((self.csem[s], self.ccnt[s]))
        nc = self.nc
        ops, sems = self.ops, self.sems
        with nc.Block() as blk:
            def run(stream, e):
                for waits, fn, sid, inc in ops[stream]:
                    for ws, wv in waits:
                        e.wait_ge(sems[ws], wv)
                    fn(e).then_inc(sems[sid], inc)

            @blk.tensor
            def _(e):
                run("pe", e)

            @blk.scalar
            def _(e):
                run("act", e)

            @blk.vector
            def _(e):
                run("dve", e)

            @blk.gpsimd
            def _(e):
                run("pool", e)

            @blk.sync
            def _(e):
                run("sp", e)
                for ws, wv in finals:
                    e.wait_ge(sems[ws], wv)


def _bcast_rows(ap_1d, n=128):
    return ap_1d.partition_broadcast(n)


def rms_rstd(P, ss, rstd, inv_n, kss, krs):
    P.op("dve", lambda e: e.tensor_scalar(out=ss, in0=ss, scalar1=inv_n, scalar2=EPS,
                                          op0=ALU.mult, op1=ALU.add), reads=[kss], writes=[kss])
    P.op("act", lambda e: e.activation(out=ss, in_=ss, func=AF.Sqrt), reads=[kss], writes=[kss])
    P.op("dve", lambda e: e.reciprocal(out=rstd, in_=ss), reads=[kss], writes=[krs])


def norm_to_hT(P, xt, kx, hbt, khb, sst, kss, rst, krs, nw_bc, knw, tpt, ktp, idn, hT_dst, khT):
    P.op("act", lambda e: e.activation(out=hbt, in_=xt, func=AF.Square, accum_out=sst),
         reads=[kx], writes=[khb, kss])
    rms_rstd(P, sst, rst, 1.0 / D_MODEL, kss, krs)
    P.op("dve", lambda e: e.scalar_tensor_tensor(out=hbt, in0=xt, scalar=rst, in1=nw_bc,
                                                 op0=ALU.mult, op1=ALU.mult),
         reads=[kx, krs, knw], writes=[khb])
    for k in range(KC):
        P.op("pe", lambda e, k=k: e.transpose(out=tpt[:, k * 128:(k + 1) * 128],
                                              in_=hbt[:, k * 128:(k + 1) * 128], identity=idn),
             reads=[khb, "idn"], writes=[ktp])
    P.op("act", lambda e: e.activation(out=hT_dst, in_=tpt.rearrange("p (k t) -> p k t", k=KC), func=AF.Copy),
         reads=[ktp], writes=[khT])


def build_out(TPC, last):
    nc = bass.Bass("TRN2", target_bir_lowering=False)
    dt = nc.dram_tensor
    x = dt("x", [TPC, D_MODEL], F32, kind="ExternalInput").ap()
    oa = dt("oa", [TPC, 2048], BF16, kind="ExternalInput").ap()
    ys = dt("ys", [TPC, 2048], BF16, kind="ExternalInput").ap()
    wz = dt("wz", [D_MODEL, 4096], F32, kind="ExternalInput").ap()
    wo = dt("wo", [4096, D_MODEL], F32, kind="ExternalInput").ap()
    nw = dt("nw", [D_MODEL], F32, kind="ExternalInput").ap()
    nwa = dt("nwa", [2048], F32, kind="ExternalInput").ap()
    nws = dt("nws", [2048], F32, kind="ExternalInput").ap()
    fnw = dt("fnw", [D_MODEL], F32, kind="ExternalInput").ap()
    ident = dt("ident", [128, 128], BF16, kind="ExternalInput").ap()
    y = dt("y", [TPC, D_MODEL], F32, kind="ExternalOutput").ap()
    TB = min(512, TPC)
    NT = TB // 128
    NB = TPC // TB
    wz_v = wz.rearrange("(kc p) c -> p kc c", p=128)
    wo_v = wo.rearrange("(kc p) c -> p kc c", p=128)
    with ExitStack() as es:
        P = Prog(nc, es)
        idn = P.sb([128, 128], BF16)
        nw_bc = P.sb([128, 2048], F32)
        nwa_bc = P.sb([128, 2048], BF16)
        nws_bc = P.sb([128, 2048], BF16)
        fnw_bc = P.sb([128, 2048], F32)
        P.dma("sp", idn[:], ident[:, :], writes=["idn"])
        P.dma("sp", nw_bc[:], _bcast_rows(nw), writes=["nw"])
        P.dma("pool", nwa_bc[:], _bcast_rows(nwa), writes=["nwa"])
        P.dma("pool", nws_bc[:], _bcast_rows(nws), writes=["nws"])
        P.dma("sp", fnw_bc[:], _bcast_rows(fnw), writes=["fnw"])
        res = [P.sb([128, 2048], F32) for _ in range(NT)]
        hb = [P.sb([128, 2048], BF16) for _ in range(2)]
        hT = P.sb([128, KC, TB], BF16)
        wblk = [P.sb([128, 8192], BF16) for _ in range(2)]
        ob = [P.sb([128, 512], BF16) for _ in range(2)]
        zs = [P.sb([128, 512], F32) for _ in range(2)]
        u = [P.sb([128, 4096], BF16) for _ in range(NT)]
        mT = P.sb([128, 32, TB], BF16)
        ss = [P.sb([128, 16], F32) for _ in range(NT)]
        rs = [P.sb([128, 16], F32) for _ in range(NT)]
        tp = [P.ps([128, 2048], BF16) for _ in range(2)]
        mm = [P.ps([128, 512], F32) for _ in range(2)]
        cnt = {"hb": 0, "w": 0, "mm": 0, "tp": 0, "ob": 0}

        def nxt(k, n=2):
            v = cnt[k] % n
            cnt[k] += 1
            return v

        for blk_i in range(NB):
            t0 = blk_i * TB
            for t in range(NT):
                rows = slice(t0 + t * 128, t0 + (t + 1) * 128)
                P.dma("sp", res[t][:], x[rows, :], writes=[("res", t)])
                b = nxt("hb")
                pb = nxt("tp")
                norm_to_hT(P, res[t][:], ("res", t), hb[b][:], ("hb", b), ss[t][:, 15:16], ("ssn", t),
                           rs[t][:, 15:16], ("rsn", t), nw_bc[:], "nw", tp[pb][:], ("tp", pb), idn[:],
                           hT[:, :, t * 128:(t + 1) * 128], ("hT", t))
            for t in range(NT):
                P.op("pool", lambda e, t=t: e.memset(ss[t][:, 0:13], 0.0), writes=[("ss2", t)])
            for cb in range(8):
                wbi = nxt("w")
                wv = wblk[wbi][:].rearrange("p (k c) -> p k c", k=KC)
                P.dma("pool", wv, wz_v[:, :, cb * 512:(cb + 1) * 512], writes=[("w", wbi)])
                for t in range(NT):
                    rows = slice(t0 + t * 128, t0 + (t + 1) * 128)
                    mi = nxt("mm")
                    for k in range(KC):
                        P.op("pe", lambda e, k=k, t=t, mi=mi, wv=wv: e.matmul(
                            mm[mi][:], lhsT=hT[:, k, t * 128:(t + 1) * 128], rhs=wv[:, k, :],
                            start=(k == 0), stop=(k == KC - 1)),
                            reads=[("hT", t), ("w", wbi)], writes=[("mm", mi)])
                    oi = nxt("ob")
                    src = oa if cb < 4 else ys
                    c0 = (cb % 4) * 512
                    P.dma("sp", ob[oi][:], src[rows, c0:c0 + 512], writes=[("ob", oi)])
                    P.op("act", lambda e, mi=mi, oi=oi: e.activation(out=zs[oi][:], in_=mm[mi][:], func=AF.Silu),
                         reads=[("mm", mi)], writes=[("zs", oi)])
                    ucol = cb * 512
                    P.op("dve", lambda e, oi=oi, t=t, ucol=ucol: e.tensor_tensor(
                        out=u[t][:, ucol:ucol + 512], in0=zs[oi][:], in1=ob[oi][:], op=ALU.mult),
                        reads=[("zs", oi), ("ob", oi)], writes=[("u", t)])
                    if cb < 4:
                        P.op("act", lambda e, oi=oi, t=t, ucol=ucol, cb=cb: e.activation(
                            out=zs[oi][:], in_=u[t][:, ucol:ucol + 512], func=AF.Square,
                            accum_out=ss[t][:, cb:cb + 1]),
                            reads=[("u", t), ("ss2", t)], writes=[("zs", oi), ("ss2", t)])
                    else:
                        for hh in range(2):
                            g = (cb - 4) * 2 + hh
                            P.op("act", lambda e, oi=oi, t=t, ucol=ucol, g=g, hh=hh: e.activation(
                                out=zs[oi][:, hh * 256:hh * 256 + 256],
                                in_=u[t][:, ucol + hh * 256:ucol + hh * 256 + 256], func=AF.Square,
                                accum_out=ss[t][:, 4 + g:5 + g]),
                                reads=[("u", t), ("ss2", t)], writes=[("zs", oi), ("ss2", t)])
            for t in range(NT):
                k2 = ("ss2", t)
                P.op("dve", lambda e, t=t: e.tensor_reduce(out=ss[t][:, 12:13], in_=ss[t][:, 0:4], axis=AX.X,
                                                          op=ALU.add), reads=[k2], writes=[k2])
                P.op("dve", lambda e, t=t: e.tensor_scalar(out=ss[t][:, 12:13], in0=ss[t][:, 12:13],
                                                          scalar1=1.0 / 2048, scalar2=EPS, op0=ALU.mult, op1=ALU.add),
                     reads=[k2], writes=[k2])
                P.op("dve", lambda e, t=t: e.tensor_scalar(out=ss[t][:, 4:12], in0=ss[t][:, 4:12],
                                                          scalar1=1.0 / 256, scalar2=EPS, op0=ALU.mult, op1=ALU.add),
                     reads=[k2], writes=[k2])
                P.op("act", lambda e, t=t: e.activation(out=ss[t][:, 4:13], in_=ss[t][:, 4:13], func=AF.Sqrt),
                     reads=[k2], writes=[k2])
                P.op("dve", lambda e, t=t: e.reciprocal(out=rs[t][:, 4:13], in_=ss[t][:, 4:13]),
                     reads=[k2], writes=[("rs2", t)])
                P.op("dve", lambda e, t=t: e.scalar_tensor_tensor(
                    out=u[t][:, 0:2048], in0=u[t][:, 0:2048], scalar=rs[t][:, 12:13], in1=nwa_bc[:],
                    op0=ALU.mult, op1=ALU.mult), reads=[("u", t), ("rs2", t), "nwa"], writes=[("u", t)])
                for g in range(8):
                    cs = slice(2048 + g * 256, 2048 + (g + 1) * 256)
                    P.op("dve", lambda e, t=t, g=g, cs=cs: e.scalar_tensor_tensor(
                        out=u[t][:, cs], in0=u[t][:, cs], scalar=rs[t][:, 4 + g:5 + g],
                        in1=nws_bc[:, g * 256:(g + 1) * 256], op0=ALU.mult, op1=ALU.mult),
                        reads=[("u", t), ("rs2", t), "nws"], writes=[("u", t)])
                for half in range(2):
                    pb = nxt("tp")
                    for k in range(16):
                        kk = half * 16 + k
                        P.op("pe", lambda e, t=t, k=k, kk=kk, pb=pb: e.transpose(
                            out=tp[pb][:, k * 128:(k + 1) * 128], in_=u[t][:, kk * 128:(kk + 1) * 128],
                            identity=idn[:]), reads=[("u", t), "idn"], writes=[("tp", pb)])
                    P.op("act", lambda e, pb=pb, t=t, half=half: e.activation(
                        out=mT[:, half * 16:(half + 1) * 16, t * 128:(t + 1) * 128],
                        in_=tp[pb][:].rearrange("p (k t) -> p k t", k=16), func=AF.Copy),
                        reads=[("tp", pb)], writes=[("mT", t)])
            for cb in range(8):
                wbi = nxt("w")
                wv = wblk[wbi][:].rearrange("p (k c) -> p k c", k=32)
                P.dma("pool", wv, wo_v[:, :, cb * 256:(cb + 1) * 256], writes=[("w", wbi)])
                for t in range(NT):
                    mi = nxt("mm")
                    for k in range(32):
                        P.op("pe", lambda e, k=k, t=t, mi=mi, wv=wv: e.matmul(
                            mm[mi][:, 0:256], lhsT=mT[:, k, t * 128:(t + 1) * 128], rhs=wv[:, k, :],
                            start=(k == 0), stop=(k == 31)),
                            reads=[("mT", t), ("w", wbi)], writes=[("mm", mi)])
                    P.op("dve", lambda e, mi=mi, t=t, cb=cb: e.tensor_tensor(
                        out=res[t][:, cb * 256:(cb + 1) * 256], in0=mm[mi][:, 0:256],
                        in1=res[t][:, cb * 256:(cb + 1) * 256], op=ALU.add),
                        reads=[("mm", mi), ("res", t)], writes=[("res", t)])
            for t in range(NT):
                rows = slice(t0 + t * 128, t0 + (t + 1) * 128)
                if last:
                    b = nxt("hb")
                    P.op("act", lambda e, t=t, b=b: e.activation(out=hb[b][:], in_=res[t][:], func=AF.Square,
                                                               accum_out=ss[t][:, 14:15]),
                         reads=[("res", t)], writes=[("hb", b), ("ssf", t)])
                    rms_rstd(P, ss[t][:, 14:15], rs[t][:, 14:15], 1.0 / D_MODEL, ("ssf", t), ("rsf", t))
                    P.op("dve", lambda e, t=t: e.scalar_tensor_tensor(
                        out=res[t][:], in0=res[t][:], scalar=rs[t][:, 14:15], in1=fnw_bc[:],
                        op0=ALU.mult, op1=ALU.mult), reads=[("res", t), ("rsf", t), "fnw"], writes=[("res", t)])
                P.dma("sp", y[rows, :], res[t][:], reads=[("res", t)])
        P.finish()
    return nc


NFB = 31
TWO_PI = 2.0 * math.pi
CW1 = 6.28125
CW2 = TWO_PI - 6.28125
PI_LO = 3.141592


def proj_decl(nc, S, kind_out):
    dt = nc.dram_tensor
    D = {}
    D["x"] = dt("x", [S, D_MODEL], F32, kind="ExternalInput").ap()
    D["pos"] = dt("pos", [S], I32, kind="ExternalInput").ap()
    D["xq"] = dt("xq", [S // 2, D_MODEL], F32, kind="ExternalInput").ap()
    D["posq"] = dt("posq", [S // 2], I32, kind="ExternalInput").ap()
    D["nw"] = dt("nw", [D_MODEL], F32, kind="ExternalInput").ap()
    D["wf"] = dt("wf", [NFB, 128, KC * 128], F32, kind="ExternalInput").ap()
    D["wt"] = dt("wt", [128, KC * 288], F32, kind="ExternalInput").ap()
    D["ident"] = dt("ident", [128, 128], BF16, kind="ExternalInput").ap()
    D["invf"] = dt("invf", [128, 2], F32, kind="ExternalInput").ap()
    D["QT"] = dt("QT", [8, 128, S // 2], BF16, kind=kind_out).ap()
    for n in ("KST", "KWT", "KCT", "VCT"):
        D[n] = dt(n, [128, S], BF16, kind=kind_out).ap()
    D["VG"] = dt("VG", [S, 288], BF16, kind=kind_out).ap()
    D["XBCT"] = dt("XBCT", [1024, S], BF16, kind=kind_out).ap()
    D["DTT"] = dt("DTT", [8, S], F32, kind=kind_out).ap()
    return D


def rope_tables(P, posf, kpos, invf, ang, kf, tmp, cosT, sinT, n, tag):
    ka, kk, kt = (tag, "ang"), (tag, "kf"), (tag, "tmp")
    kfi = kf.bitcast(I32)
    P.op("dve", lambda e: e.tensor_scalar(out=ang, in0=posf, scalar1=invf[:, 0:1], scalar2=None, op0=ALU.mult),
         reads=[kpos, "invf"], writes=[ka])
    P.op("dve", lambda e: e.tensor_scalar(out=tmp, in0=ang, scalar1=1.0 / TWO_PI, scalar2=None, op0=ALU.mult),
         reads=[ka], writes=[kt])
    P.op("dve", lambda e: e.tensor_copy(out=kfi, in_=tmp), reads=[kt], writes=[kk])
    P.op("dve", lambda e: e.tensor_copy(out=tmp, in_=kfi), reads=[kk], writes=[kt])
    P.op("dve", lambda e: e.scalar_tensor_tensor(out=ang, in0=tmp, scalar=-CW1, in1=ang, op0=ALU.mult, op1=ALU.add),
         reads=[kt, ka], writes=[ka])
    P.op("dve", lambda e: e.scalar_tensor_tensor(out=ang, in0=tmp, scalar=-CW2, in1=ang, op0=ALU.mult, op1=ALU.add),
         reads=[kt, ka], writes=[ka])

    def wrap(buf, kb):
        P.op("dve", lambda e: e.tensor_scalar(out=tmp, in0=buf, scalar1=math.pi, scalar2=-TWO_PI,
                                              op0=ALU.is_gt, op1=ALU.mult), reads=[kb], writes=[kt])
        P.op("dve", lambda e: e.tensor_tensor(out=buf, in0=buf, in1=tmp, op=ALU.add), reads=[kb, kt], writes=[kb])
        P.op("dve", lambda e: e.tensor_scalar(out=tmp, in0=buf, scalar1=-math.pi, scalar2=TWO_PI,
                                              op0=ALU.is_lt, op1=ALU.mult), reads=[kb], writes=[kt])
        P.op("dve", lambda e: e.tensor_tensor(out=buf, in0=buf, in1=tmp, op=ALU.add), reads=[kb, kt], writes=[kb])
        P.op("dve", lambda e: e.tensor_scalar(out=buf, in0=buf, scalar1=PI_LO, scalar2=-PI_LO,
                                              op0=ALU.min, op1=ALU.max), reads=[kb], writes=[kb])

    wrap(ang, ka)
    kc_, ks_ = (tag, "cos"), (tag, "sin")
    P.op("act", lambda e: e.activation(out=sinT, in_=ang, func=AF.Sin, scale=invf[:, 1:2]),
         reads=[ka, "invf"], writes=[ks_])
    P.op("dve", lambda e: e.tensor_scalar(out=kf, in0=ang, scalar1=math.pi / 2, scalar2=None, op0=ALU.add),
         reads=[ka], writes=[kk])
    wrap(kf, kk)
    P.op("act", lambda e: e.activation(out=cosT, in_=kf, func=AF.Sin), reads=[kk], writes=[kc_])
    return kc_, ks_


def emit_proj(P, S, D):
    SBT = min(2048, S)
    NSB = S // SBT
    NTS = SBT // 128
    x, pos = D["x"], D["pos"]
    idn = P.sb([128, 128], BF16)
    nw_bc = P.sb([128, 2048], F32)
    invf = P.sb([128, 2], F32)
    P.dma("sp", idn[:], D["ident"][:, :], writes=["idn"])
    P.dma("sp", nw_bc[:], _bcast_rows(D["nw"]), writes=["nw"])
    P.dma("sp", invf[:], D["invf"][:, :], writes=["invf"])
    xb = [P.sb([128, 2048], F32) for _ in range(2)]
    hb = [P.sb([128, 2048], BF16) for _ in range(2)]
    ssn = P.sb([128, 4], F32)
    hT = P.sb([128, KC, SBT], BF16)
    posi = P.sb([128, SBT], I32)
    posf = P.sb([128, SBT], F32)
    ang = P.sb([128, SBT], F32)
    kf = P.sb([128, SBT], F32)
    tmp = P.sb([128, SBT], F32)
    cosT = P.sb([128, SBT], F32)
    sinT = P.sb([128, SBT], F32)
    wb = [P.sb([128, KC * 128], BF16) for _ in range(3)]
    wt = P.sb([128, KC * 288], BF16)
    stg = [P.sb([128, 512], BF16) for _ in range(4)]
    stf = [P.sb([128, 512], F32) for _ in range(2)]
    t1 = [P.sb([128, 512], F32) for _ in range(2)]
    t2 = [P.sb([128, 512], F32) for _ in range(2)]
    vst = [P.sb([128, 288], BF16) for _ in range(2)]
    tp = [P.ps([128, 2048], BF16) for _ in range(2)]
    mm = [P.ps([128, 512], F32) for _ in range(4)]
    cnt = {}

    def nxt(k, n):
        v = cnt.get(k, 0)
        cnt[k] = v + 1
        return v % n

    P.dma("pool", wt[:].rearrange("p (a c) -> p a c", c=1152), D["wt"].rearrange("p (a c) -> p a c", c=1152), writes=["wt"])
    wtv = wt[:].rearrange("p (k c) -> p k c", k=KC)
    kcos, ksin = ("rp", "cos"), ("rp", "sin")

    def prep_block(xsrc, psrc, r0, n):
        SK = os.environ.get('PROJ_SKIP', '')
        if 'r' not in SK:
            P.dma("sp", posi[:, 0:n], psrc[r0:r0 + n].partition_broadcast(128), writes=["posi"])
            P.op("dve", lambda e: e.tensor_copy(out=posf[:, 0:n], in_=posi[:, 0:n]), reads=["posi"], writes=["posf"])
        if 'r' not in SK and 'R' not in SK:
            rope_tables(P, posf[:, 0:n], "posf", invf, ang[:, 0:n], kf[:, 0:n], tmp[:, 0:n], cosT[:, 0:n],
                        sinT[:, 0:n], n, "rp")
        for t in range(n // 128):
            rows = slice(r0 + t * 128, r0 + (t + 1) * 128)
            b = nxt("xb", 2)
            pb = nxt("tp", 2)
            P.dma("sp", xb[b][:], xsrc[rows, :], writes=[("xb", b)])
            norm_to_hT(P, xb[b][:], ("xb", b), hb[b][:], ("hb", b), ssn[:, b:b + 1], ("ssn", b),
                       ssn[:, 2 + b:3 + b], ("rsn", b), nw_bc[:], "nw", tp[pb][:], ("tp", pb), idn[:],
                       hT[:, :, t * 128:(t + 1) * 128], ("hT", t))

    def load_w(fb):
        wi = nxt("wb", 3)
        P.dma("pool", wb[wi][:].rearrange("p (a c) -> p a c", c=1024),
              D["wf"][fb, :, :].rearrange("p (a c) -> p a c", c=1024), writes=[("wb", wi)])
        return wi, wb[wi][:].rearrange("p (k c) -> p k c", k=KC)

    def mm_block(wi, wv, rhs_fn, n, reads_h):
        mi = nxt("mm", 4)
        for k in range(KC):
            P.op("pe", lambda e, k=k, mi=mi, wv=wv: e.matmul(
                mm[mi][:, 0:n], lhsT=wv[:, k, :], rhs=rhs_fn(k), start=(k == 0), stop=(k == KC - 1)),
                reads=reads_h + [("wb", wi)], writes=[("mm", mi)])
        return mi

    def rope_store(ma, mb, cs, sn, dst, n):
        ti = nxt("t12", 2)
        si = nxt("stg", 4)
        P.op("dve", lambda e: e.tensor_tensor(out=t1[ti][:, 0:n], in0=mm[ma][:, 0:n], in1=cs, op=ALU.mult),
             reads=[("mm", ma), kcos], writes=[("t1", ti)])
        P.op("dve", lambda e: e.tensor_tensor(out=t2[ti][:, 0:n], in0=mm[mb][:, 0:n], in1=sn, op=ALU.mult),
             reads=[("mm", mb), ksin], writes=[("t2", ti)])
        P.op("pool", lambda e: e.tensor_tensor(out=stg[si][:, 0:n], in0=t1[ti][:, 0:n], in1=t2[ti][:, 0:n],
                                               op=ALU.add),
             reads=[("t1", ti), ("t2", ti)], writes=[("stg", si)])
        P.dma("sp", dst, stg[si][:, 0:n], reads=[("stg", si)])

    SQ = S // 2
    SBQ = min(2048, SQ)
    SKIP = os.environ.get('PROJ_SKIP', '')
    for qb in range(0 if 'q' in SKIP else SQ // SBQ):
        q0 = qb * SBQ
        prep_block(D["xq"], D["posq"], q0, SBQ)
        hq_all = [("hT", t) for t in range(SBQ // 128)]
        CHQ = min(512, SBQ)
        for r in range(8):
            wa, wva = load_w(r)
            wb_, wvb = load_w(8 + r)
            for c in range(SBQ // CHQ):
                cs_ = slice(c * CHQ, (c + 1) * CHQ)
                rf = lambda k, cs_=cs_: hT[:, k, cs_]
                ma = mm_block(wa, wva, rf, CHQ, hq_all)
                mb = mm_block(wb_, wvb, rf, CHQ, hq_all)
                rope_store(ma, mb, cosT[:, cs_], sinT[:, cs_], D["QT"][r, :, q0 + c * CHQ:q0 + (c + 1) * CHQ], CHQ)

    for sbi in range(NSB):
        s0 = sbi * SBT
        prep_block(x, pos, s0, SBT)
        hT_all = [("hT", t) for t in range(NTS)]
        for t in range(0 if 't' in SKIP else NTS):
            rows = slice(s0 + t * 128, s0 + (t + 1) * 128)
            mi = nxt("mm", 4)
            for k in range(KC):
                P.op("pe", lambda e, k=k, t=t, mi=mi: e.matmul(
                    mm[mi][:, 0:288], lhsT=hT[:, k, t * 128:(t + 1) * 128], rhs=wtv[:, k, :],
                    start=(k == 0), stop=(k == KC - 1)), reads=[("hT", t), "wt"], writes=[("mm", mi)])
            vi = nxt("vst", 2)
            P.op("act", lambda e, mi=mi, vi=vi: e.activation(out=vst[vi][:, 0:256], in_=mm[mi][:, 0:256],
                                                           func=AF.Copy),
                 reads=[("mm", mi)], writes=[("vstA", vi)])
            P.op("act", lambda e, mi=mi, vi=vi: e.activation(out=vst[vi][:, 256:288], in_=mm[mi][:, 256:288],
                                                           func=AF.Sigmoid),
                 reads=[("mm", mi)], writes=[("vstB", vi)])
            P.dma("sp", D["VG"][rows, :], vst[vi][:], reads=[("vstA", vi), ("vstB", vi)])

        CH = min(512, SBT)
        for (fa, name) in (() if 'k' in SKIP else ((16, "KST"), (18, "KWT"))):
            wa, wva = load_w(fa)
            wb_, wvb = load_w(fa + 1)
            for c in range(SBT // CH):
                cs_ = slice(c * CH, (c + 1) * CH)
                rf = lambda k, cs_=cs_: hT[:, k, cs_]
                ma = mm_block(wa, wva, rf, CH, hT_all)
                mb = mm_block(wb_, wvb, rf, CH, hT_all)
                rope_store(ma, mb, cosT[:, cs_], sinT[:, cs_], D[name][:, s0 + c * CH:s0 + (c + 1) * CH], CH)
        plain = [(20, "KCT", 0), (21, "VCT", 0)] + [(22 + i, "XBCT", i * 128) for i in range(8)] + [(30, "DTT", 0)]
        for (fb, name, r0) in ([] if 'p' in SKIP else plain):
            wa, wva = load_w(fb)
            for c in range(SBT // CH):
                cs_ = slice(c * CH, (c + 1) * CH)
                rf = lambda k, cs_=cs_: hT[:, k, cs_]
                ma = mm_block(wa, wva, rf, CH, hT_all)
                tok = slice(s0 + c * CH, s0 + (c + 1) * CH)
                if name == "DTT":
                    fi = nxt("stf", 2)
                    P.op("act", lambda e, ma=ma, fi=fi: e.activation(out=stf[fi][0:8, 0:CH], in_=mm[ma][0:8, 0:CH],
                                                                   func=AF.Copy),
                         reads=[("mm", ma)], writes=[("stf", fi)])
                    P.dma("sp", D["DTT"][:, tok], stf[fi][0:8, 0:CH], reads=[("stf", fi)])
                else:
                    si = nxt("stg", 4)
                    eng = "act" if (cnt["stg"] % 2) else "dve"
                    if eng == "act":
                        P.op("act", lambda e, ma=ma, si=si: e.activation(out=stg[si][:, 0:CH], in_=mm[ma][:, 0:CH],
                                                                       func=AF.Copy),
                             reads=[("mm", ma)], writes=[("stg", si)])
                    else:
                        P.op("dve", lambda e, ma=ma, si=si: e.tensor_copy(out=stg[si][:, 0:CH], in_=mm[ma][:, 0:CH]),
                             reads=[("mm", ma)], writes=[("stg", si)])
                    P.dma("sp", D[name][r0:r0 + 128, tok], stg[si][:, 0:CH], reads=[("stg", si)])


def build_proj(S):
    nc = bass.Bass("TRN2", target_bir_lowering=False)
    D = proj_decl(nc, S, "ExternalOutput")
    with ExitStack() as es:
        P = Prog(nc, es)
        emit_proj(P, S, D)
        P.finish()
    return nc, D


def core_coords(c):
    return c // 4, (c % 4) // 2, c % 2, c % 4


def _swap(a):
    return np.concatenate([a[64:], a[:64]])


def proj_weight_layout(w, g, j):
    cols = []
    for r in range(8):
        h = g * 8 + r
        cols.append(np.arange(h * 128, (h + 1) * 128))
    for r in range(8):
        h = g * 8 + r
        cols.append(_swap(np.arange(h * 128, (h + 1) * 128)))
    kv = lambda off: np.arange(off + g * 128, off + (g + 1) * 128)
    cols += [kv(OFF_KS), _swap(kv(OFF_KS)), kv(OFF_KW), _swap(kv(OFF_KW)), kv(OFF_KC), kv(OFF_VC)]
    for i in range(4):
        cols.append(np.arange(OFF_X + 512 * j + i * 128, OFF_X + 512 * j + (i + 1) * 128))
    for i in range(2):
        cols.append(np.arange(OFF_B + 256 * j + i * 128, OFF_B + 256 * j + (i + 1) * 128))
    for i in range(2):
        cols.append(np.arange(OFF_C + 256 * j + i * 128, OFF_C + 256 * j + (i + 1) * 128))
    wf = np.zeros((NFB, 128, KC, 128), np.float32)
    for fb, cc in enumerate(cols):
        wf[fb] = w[:, cc].reshape(KC, 128, 128).transpose(1, 0, 2)
    wf[30, :, :, 0:8] = w[:, OFF_DT + 8 * j:OFF_DT + 8 * j + 8].reshape(KC, 128, 8).transpose(1, 0, 2)
    tc = np.concatenate([kv(OFF_VS), kv(OFF_VW), np.arange(OFF_GATE + g * 24, OFF_GATE + (g + 1) * 24)])
    wt = np.zeros((128, KC, 288), np.float32)
    wt[:, :, 0:280] = w[:, tc].reshape(KC, 128, 280).transpose(1, 0, 2)
    return wf.reshape(NFB, 128, KC * 128), wt.reshape(128, KC * 288)


def rope_consts():
    half = 64
    inv = (10000.0 ** (-np.arange(half, dtype=np.float32) / half)).astype(np.float32)
    c = np.zeros((128, 2), np.float32)
    c[:, 0] = np.concatenate([inv, inv])
    c[:64, 1] = -1.0
    c[64:, 1] = 1.0
    return c


def my_tiles(a, par):
    s = a.shape[0]
    v = a.reshape((s // 256, 2, 128) + a.shape[1:])
    return np.ascontiguousarray(v[:, par].reshape((s // 2,) + a.shape[1:]))


IDENT_BF = np.eye(128, dtype=np.float32).astype(NPBF)


def ssd_decl(nc, S, kind_in, kind_out, D=None):
    dt = nc.dram_tensor
    D = {} if D is None else D
    if "XBCT" not in D:
        D["XBCT"] = dt("XBCT", [1024, S], BF16, kind=kind_in).ap()
        D["DTT"] = dt("DTT", [8, S], F32, kind=kind_in).ap()
    D["convp"] = dt("convp", [128, 8, 5], F32, kind="ExternalInput").ap()
    D["ssmp"] = dt("ssmp", [8, 2], F32, kind="ExternalInput").ap()
    D["dskip"] = dt("dskip", [8], F32, kind="ExternalInput").ap()
    D["negm"] = dt("negm", [128, 128], F32, kind="ExternalInput").ap()
    D["onehot"] = dt("onehot", [8, 1024], F32, kind="ExternalInput").ap()
    D["id8"] = dt("id8", [8, 8], F32, kind="ExternalInput").ap()
    if "ident" not in D:
        D["ident"] = dt("ident", [128, 128], BF16, kind="ExternalInput").ap()
    D["YS"] = dt("YS", [S, 512], BF16, kind=kind_out).ap()
    return D


def emit_ssd(P, S, D, pfx="s"):
    SC = min(2048, S)
    NSC = S // SC
    NCH = SC // 128
    K = lambda *a: (pfx,) + a
    idn = P.sb([128, 128], BF16)
    id8 = P.sb([8, 8], F32)
    convp = P.sb([128, 8, 5], F32)
    ssmp = P.sb([8, 2], F32)
    negm = P.sb([128, 128], F32)
    onehot = P.sb([8, 1024], F32)
    dsk = P.sb([128, 8], F32)
    P.dma("sp", idn[:], D["ident"][:, :], writes=[K("idn")])
    P.dma("sp", id8[:], D["id8"][:, :], writes=[K("id8")])
    P.dma("sp", convp[:], D["convp"][:, :, :], writes=[K("convp")])
    P.dma("sp", ssmp[:], D["ssmp"][:, :], writes=[K("ssmp")])
    P.dma("sp", negm[:], D["negm"][:, :], writes=[K("negm")])
    P.dma("sp", onehot[:], D["onehot"][:, :], writes=[K("onehot")])
    P.dma("sp", dsk[:], D["dskip"].partition_broadcast(128), writes=[K("dsk")])
    aneg = P.sb([8, 1], F32)
    P.op("act", lambda e: e.activation(out=aneg[:], in_=ssmp[:, 1:2], func=AF.Exp), reads=[K("ssmp")],
         writes=[K("aneg")])
    P.op("dve", lambda e: e.tensor_scalar(out=aneg[:], in0=aneg[:], scalar1=-1.0, scalar2=None, op0=ALU.mult),
         reads=[K("aneg")], writes=[K("aneg")])
    raw = P.sb([128, 8, SC + 4], BF16)
    acc = [P.sb([128, SC], F32) for _ in range(2)]
    cv = P.sb([128, 8, SC], BF16)
    dtf = P.sb([8, SC], F32)
    daf = P.sb([8, SC], F32)
    cum = P.sb([8, SC], F32)
    ones8 = P.sb([8, 128], F32)
    P.op("pool", lambda e: e.memset(ones8[:], 1.0), writes=[K("ones8")])
    P.op("pool", lambda e: e.memset(raw[:, :, 0:4], 0.0), writes=[K("raw")])
    state = [P.sb([128, 64], F32) for _ in range(8)]
    stbf = [P.sb([128, 64], BF16) for _ in range(8)]
    for h in range(8):
        P.op("pool", lambda e, h=h: e.memset(state[h][:], 0.0), writes=[K("state", h)])
        P.op("pool", lambda e, h=h: e.memset(stbf[h][:], 0.0), writes=[K("stbf", h)])
    xb_tok = [P.sb([128, 768], BF16) for _ in range(2)]
    dtc = [P.sb([128, 24], F32) for _ in range(2)]
    lastbc = [P.sb([128, 8], F32) for _ in range(2)]
    elast = [P.sb([128, 8], F32) for _ in range(2)]
    eend = [P.sb([128, 8], F32) for _ in range(2)]
    ecum = [P.sb([128, 8], F32) for _ in range(2)]
    tmpd = [P.sb([128, 128], F32) for _ in range(2)]
    dec = [P.sb([128, 128], F32) for _ in range(2)]
    WT = [P.sb([128, 128], BF16) for _ in range(2)]
    xdt = [P.sb([128, 64], BF16) for _ in range(2)]
    xend = [P.sb([128, 64], BF16) for _ in range(2)]
    t1 = [P.sb([128, 64], F32) for _ in range(2)]
    yt = [P.sb([128, 512], BF16) for _ in range(2)]
    pA = [P.ps([128, 128], F32) for _ in range(2)]
    pCB = P.ps([128, 256], F32)
    pC = [P.ps([128, 128], F32) for _ in range(2)]
    pD = P.ps([128, 64], F32)
    pT = P.ps([128, 768], BF16)
    pT2 = P.ps([128, 16], F32)
    cnt = {}

    def nxt(k, n=2):
        v = cnt.get(k, 0)
        cnt[k] = v + 1
        return v % n

    for sc in range(NSC):
        s0 = sc * SC
        if sc > 0:
            P.op("pool", lambda e: e.tensor_copy(out=raw[:, :, 1:4], in_=raw[:, :, SC + 1:SC + 4]),
                 reads=[K("raw")], writes=[K("raw")])
        P.dma("sp", raw[:, :, 4:SC + 4], D["XBCT"][:, s0:s0 + SC].rearrange("(b p) t -> p b t", p=128),
              writes=[K("raw")])
        for b in range(8):
            ai = nxt("acc")
            P.op("dve", lambda e, b=b, ai=ai: e.tensor_scalar(
                out=acc[ai][:], in0=raw[:, b, 1:SC + 1], scalar1=convp[:, b, 0:1], scalar2=convp[:, b, 4:5],
                op0=ALU.mult, op1=ALU.add), reads=[K("raw"), K("convp")], writes=[K("acc", ai)])
            for k in range(1, 4):
                P.op("dve", lambda e, b=b, ai=ai, k=k: e.scalar_tensor_tensor(
                    out=acc[ai][:], in0=raw[:, b, 1 + k:SC + 1 + k], scalar=convp[:, b, k:k + 1], in1=acc[ai][:],
                    op0=ALU.mult, op1=ALU.add), reads=[K("raw"), K("convp"), K("acc", ai)], writes=[K("acc", ai)])
            P.op("act", lambda e, b=b, ai=ai: e.activation(out=cv[:, b, :], in_=acc[ai][:], func=AF.Silu),
                 reads=[K("acc", ai)], writes=[K("cv", b)])
        P.dma("sp", dtf[:], D["DTT"][:, s0:s0 + SC], writes=[K("dtf")])
        P.op("act", lambda e: e.activation(out=dtf[:], in_=dtf[:], func=AF.Exp, bias=ssmp[:, 0:1]),
             reads=[K("dtf"), K("ssmp")], writes=[K("dtf")])
        P.op("act", lambda e: e.activation(out=dtf[:], in_=dtf[:], func=AF.Ln, bias=1.0),
             reads=[K("dtf")], writes=[K("dtf")])
        P.op("dve", lambda e: e.tensor_scalar(out=daf[:], in0=dtf[:], scalar1=aneg[:, 0:1], scalar2=None,
                                              op0=ALU.mult), reads=[K("dtf"), K("aneg")], writes=[K("daf")])
        for c in range(NCH):
            cs = slice(c * 128, (c + 1) * 128)
            P.op("dve", lambda e, cs=cs: e.tensor_tensor_scan(out=cum[:, cs], data0=ones8[:], data1=daf[:, cs],
                                                              initial=0.0, op0=ALU.mult, op1=ALU.add),
                 reads=[K("daf"), K("ones8")], writes=[K("cum")])
        cv_all = [K("cv", b) for b in range(8)]
        for c in range(NCH):
            cs = slice(c * 128, (c + 1) * 128)
            rows = slice(s0 + c * 128, s0 + (c + 1) * 128)
            for b in range(6):
                P.op("pe", lambda e, b=b, cs=cs: e.transpose(out=pT[:, b * 128:(b + 1) * 128], in_=cv[:, b, cs],
                                                            identity=idn[:]),
                     reads=[K("cv", b), K("idn")], writes=[K("pT")])
            xi = nxt("xb")
            P.op("act", lambda e, xi=xi: e.activation(out=xb_tok[xi][:], in_=pT[:], func=AF.Copy),
                 reads=[K("pT")], writes=[K("xb", xi)])
            P.op("pe", lambda e, cs=cs: e.transpose(out=pT2[:, 0:8], in_=dtf[:, cs], identity=id8[:]),
                 reads=[K("dtf"), K("id8")], writes=[K("pT2")])
            P.op("pe", lambda e, cs=cs: e.transpose(out=pT2[:, 8:16], in_=cum[:, cs], identity=id8[:]),
                 reads=[K("cum"), K("id8")], writes=[K("pT2")])
            di = nxt("dtc")
            P.op("dve", lambda e, di=di: e.tensor_copy(out=dtc[di][:, 0:16], in_=pT2[:]),
                 reads=[K("pT2")], writes=[K("dtc", di)])
            P.op("dve", lambda e, di=di: e.tensor_scalar(out=dtc[di][:, 16:24], in0=dtc[di][:, 8:16], scalar1=-1.0,
                                                        scalar2=None, op0=ALU.mult),
                 reads=[K("dtc", di)], writes=[K("dtc", di)])
            P.op("act", lambda e, di=di: e.activation(out=ecum[di][:], in_=dtc[di][:, 8:16], func=AF.Exp),
                 reads=[K("dtc", di)], writes=[K("ecum", di)])
            for g in range(2):
                P.op("pe", lambda e, g=g, cs=cs: e.matmul(pCB[:, g * 128:(g + 1) * 128], lhsT=cv[:, 4 + g, cs],
                                                         rhs=cv[:, 6 + g, cs], start=True, stop=True),
                     reads=[K("cv", 4 + g), K("cv", 6 + g)], writes=[K("pCB", g)])
            yi = nxt("yt")
            for h in range(8):
                g = h // 4
                ai = nxt("pA")
                P.op("pe", lambda e, h=h, ai=ai, cs=cs: e.matmul(pA[ai][:], lhsT=onehot[:, h * 128:(h + 1) * 128],
                                                               rhs=cum[:, cs], start=True, stop=True),
                     reads=[K("onehot"), K("cum")], writes=[K("pA", ai)])
                ti = nxt("tmpd")
                P.op("dve", lambda e, ai=ai, ti=ti: e.tensor_tensor(out=tmpd[ti][:], in0=pA[ai][:], in1=negm[:],
                                                                    op=ALU.add),
                     reads=[K("pA", ai), K("negm")], writes=[K("tmpd", ti)])
                P.op("dve", lambda e, ai=ai, di=di, h=h: e.tensor_copy(out=lastbc[di][:, h:h + 1],
                                                                      in_=pA[ai][:, 127:128]),
                     reads=[K("pA", ai)], writes=[K("lastbc", di, h)])
                P.op("act", lambda e, ti=ti, di=di, h=h: e.activation(out=dec[ti][:], in_=tmpd[ti][:], func=AF.Exp,
                                                                     bias=dtc[di][:, 16 + h:17 + h]),
                     reads=[K("tmpd", ti), K("dtc", di)], writes=[K("dec", ti)])
                P.op("dve", lambda e, ti=ti, g=g: e.tensor_tensor(out=WT[ti][:], in0=dec[ti][:],
                                                                 in1=pCB[:, g * 128:(g + 1) * 128], op=ALU.mult),
                     reads=[K("dec", ti), K("pCB", g)], writes=[K("WT", ti)])
                xi2 = nxt("xdt")
                P.op("dve", lambda e, xi=xi, xi2=xi2, di=di, h=h: e.tensor_scalar(
                    out=xdt[xi2][:], in0=xb_tok[xi][:, h * 64:(h + 1) * 64], scalar1=dtc[di][:, h:h + 1],
                    scalar2=None, op0=ALU.mult), reads=[K("xb", xi), K("dtc", di)], writes=[K("xdt", xi2)])
                ci = nxt("pC")
                P.op("pe", lambda e, ti=ti, xi2=xi2, ci=ci: e.matmul(pC[ci][:, 0:64], lhsT=WT[ti][:], rhs=xdt[xi2][:],
                                                                    start=True, stop=True),
                     reads=[K("WT", ti), K("xdt", xi2)], writes=[K("pC", ci)])
                P.op("pe", lambda e, g=g, h=h, ci=ci, cs=cs: e.matmul(pC[ci][:, 64:128], lhsT=cv[:, 6 + g, cs],
                                                                     rhs=stbf[h][:], start=True, stop=True),
                     reads=[K("cv", 6 + g), K("stbf", h)], writes=[K("pC", ci)])
                P.op("dve", lambda e, ci=ci, ti=ti, di=di, h=h: e.scalar_tensor_tensor(
                    out=t1[ti][:], in0=pC[ci][:, 64:128], scalar=ecum[di][:, h:h + 1], in1=pC[ci][:, 0:64],
                    op0=ALU.mult, op1=ALU.add) if False else e.tensor_scalar(
                    out=t1[ti][:], in0=pC[ci][:, 64:128], scalar1=ecum[di][:, h:h + 1], scalar2=None, op0=ALU.mult),
                    reads=[K("pC", ci), K("ecum", di)], writes=[K("t1", ti)])
                P.op("dve", lambda e, ci=ci, ti=ti: e.tensor_tensor(out=t1[ti][:], in0=t1[ti][:], in1=pC[ci][:, 0:64],
                                                                    op=ALU.add),
                     reads=[K("pC", ci), K("t1", ti)], writes=[K("t1", ti)])
                P.op("dve", lambda e, xi=xi, ti=ti, yi=yi, h=h: e.scalar_tensor_tensor(
                    out=yt[yi][:, h * 64:(h + 1) * 64], in0=xb_tok[xi][:, h * 64:(h + 1) * 64],
                    scalar=dsk[:, h:h + 1], in1=t1[ti][:], op0=ALU.mult, op1=ALU.add),
                    reads=[K("xb", xi), K("dsk"), K("t1", ti)], writes=[K("yt", yi)])
                P.op("act", lambda e, di=di, h=h: e.activation(out=eend[di][:, h:h + 1], in_=dtc[di][:, 16 + h:17 + h],
                                                              func=AF.Exp, bias=lastbc[di][:, h:h + 1]),
                     reads=[K("dtc", di), K("lastbc", di, h)], writes=[K("eend", di, h)])
                P.op("act", lambda e, di=di, h=h: e.activation(out=elast[di][:, h:h + 1], in_=lastbc[di][:, h:h + 1],
                                                              func=AF.Exp),
                     reads=[K("lastbc", di, h)], writes=[K("elast", di, h)])
                P.op("dve", lambda e, xi2=xi2, di=di, h=h: e.tensor_scalar(
                    out=xend[xi2][:], in0=xdt[xi2][:], scalar1=eend[di][:, h:h + 1], scalar2=None, op0=ALU.mult),
                    reads=[K("xdt", xi2), K("eend", di, h)], writes=[K("xend", xi2)])
                P.op("pe", lambda e, xi=xi, xi2=xi2, g=g: e.matmul(pD[:], lhsT=xb_tok[xi][:, 512 + g * 128:640 + g * 128],
                                                                  rhs=xend[xi2][:], start=True, stop=True),
                     reads=[K("xb", xi), K("xend", xi2)], writes=[K("pD")])
                P.op("dve", lambda e, di=di, h=h: e.scalar_tensor_tensor(
                    out=state[h][:], in0=state[h][:], scalar=elast[di][:, h:h + 1], in1=pD[:],
                    op0=ALU.mult, op1=ALU.add), reads=[K("state", h), K("elast", di, h), K("pD")],
                    writes=[K("state", h)])
                P.op("pool", lambda e, h=h: e.tensor_copy(out=stbf[h][:], in_=state[h][:]),
                     reads=[K("state", h)], writes=[K("stbf", h)])
            P.dma("sp", D["YS"][rows, :], yt[yi][:], reads=[K("yt", yi)])


def build_ssd(S):
    nc = bass.Bass("TRN2", target_bir_lowering=False)
    D = ssd_decl(nc, S, "ExternalInput", "ExternalOutput")
    with ExitStack() as es:
        P = Prog(nc, es)
        emit_ssd(P, S, D)
        P.finish()
    return nc, D


def ssd_consts():
    l = np.arange(128)
    negm = np.where(l[None, :] >= l[:, None], 0.0, -1e30).astype(np.float32)
    onehot = np.zeros((8, 1024), np.float32)
    for h in range(8):
        onehot[h, h * 128:(h + 1) * 128] = 1.0
    return dict(negm=negm, onehot=onehot, id8=np.eye(8, dtype=np.float32))


def ssd_param_layout(conv_w, conv_b, dt_bias, a_log, d_skip, j):
    ch = np.concatenate([np.arange(512 * j, 512 * (j + 1)), 2048 + np.arange(256 * j, 256 * (j + 1)),
                         3072 + np.arange(256 * j, 256 * (j + 1))])
    cp = np.zeros((128, 8, 5), np.float32)
    cp[:, :, 0:4] = conv_w[:, ch].T.reshape(8, 128, 4).transpose(1, 0, 2)
    cp[:, :, 4] = conv_b[ch].reshape(8, 128).T
    hs = slice(8 * j, 8 * j + 8)
    ssmp = np.stack([dt_bias[hs], a_log[hs]], 1).astype(np.float32)
    return dict(convp=cp, ssmp=np.ascontiguousarray(ssmp), dskip=np.ascontiguousarray(d_skip[hs]))


def build_cmp(S):
    NCP = S // 16
    NC = NCP - 1
    nc = bass.Bass("TRN2", target_bir_lowering=False)
    dt = nc.dram_tensor
    KCT = dt("KCT", [128, S], BF16, kind="ExternalInput").ap()
    VCT = dt("VCT", [128, S], BF16, kind="ExternalInput").ap()
    w1 = {n: dt("w1" + n, [32, 128, 256], F32, kind="ExternalInput").ap() for n in "kv"}
    w2 = {n: dt("w2" + n, [256, 128], F32, kind="ExternalInput").ap() for n in "kv"}
    w2s = dt("w2ks", [256, 128], F32, kind="ExternalInput").ap()
    peT = {n: dt("peT" + n, [128, 32], F32, kind="ExternalInput").ap() for n in "kv"}
    posc = dt("posc", [NCP], I32, kind="ExternalInput").ap()
    invf_d = dt("invf", [128, 2], F32, kind="ExternalInput").ap()
    KCMPT = dt("KCMPT", [128, NCP], BF16, kind="ExternalOutput").ap()
    VCMP = dt("VCMP", [NCP, 128], BF16, kind="ExternalOutput").ap()
    with ExitStack() as es:
        P = Prog(nc, es)
        invf = P.sb([128, 2], F32)
        P.dma("sp", invf[:], invf_d[:, :], writes=["invf"])
        posi = P.sb([128, NCP], I32)
        posf = P.sb([128, NCP], F32)
        ang = P.sb([128, NCP], F32)
        kf = P.sb([128, NCP], F32)
        tmp = P.sb([128, NCP], F32)
        cosT = P.sb([128, NCP], F32)
        sinT = P.sb([128, NCP], F32)
        P.dma("sp", posi[:], posc.partition_broadcast(128), writes=["posi"])
        P.op("dve", lambda e: e.tensor_copy(out=posf[:], in_=posi[:]), reads=["posi"], writes=["posf"])
        rope_tables(P, posf[:], "posf", invf, ang[:], kf[:], tmp[:], cosT[:], sinT[:], NCP, "rp")
        raw = {n: P.sb([128, S], BF16) for n in "kv"}
        P.dma("sp", raw["k"][:], KCT[:, :], writes=[("raw", "k")])
        P.dma("sp", raw["v"][:], VCT[:, :], writes=[("raw", "v")])
        w1s = {n: P.sb([128, 32, 256], BF16) for n in "kv"}
        w2sb = {n: P.sb([128, 2, 128], BF16) for n in ("k", "v", "ks")}
        pes = {n: P.sb([128, 32], BF16) for n in "kv"}
        for n in "kv":
            for half in range(2):
                P.dma("pool", w1s[n][:, half * 16:(half + 1) * 16, :],
                      w1[n][half * 16:(half + 1) * 16].rearrange("p d h -> d p h"), writes=[("w1", n)])
            P.dma("pool", w2sb[n][:], w2[n].rearrange("(c p) d -> p c d", p=128), writes=[("w2", n)])
            P.dma("pool", pes[n][:], peT[n][:, :], writes=[("pe", n)])
        P.dma("pool", w2sb["ks"][:], w2s.rearrange("(c p) d -> p c d", p=128), writes=[("w2", "ks")])
        sil = {n: P.sb([128, 2, NCP], BF16) for n in "kv"}
        bias = P.sb([128, 4], F32)
        pb = P.ps([128, 4], F32)
        ph = [P.ps([128, 512], F32) for _ in range(2)]
        pk = [P.ps([128, 512], F32) for _ in range(2)]
        t1 = P.sb([128, 512], F32)
        t2 = P.sb([128, 512], F32)
        ko = P.sb([128, NCP], BF16)
        vo = P.sb([128, 128], BF16)
        P.op("pool", lambda e: e.memset(ko[:], 0.0), writes=["ko"])
        for n in "kv":
            P.op("pool", lambda e, n=n: e.memset(sil[n][:], 0.0), writes=[("sil", n)])
        halves = [(a, min(a + 512, NC)) for a in range(0, NC, 512)]
        cnt = [0]
        for ni, n in enumerate("kv"):
            for hh in range(2):
                col = ni * 2 + hh
                for p in range(32):
                    P.op("pe", lambda e, n=n, hh=hh, p=p, col=col: e.matmul(
                        pb[:, col:col + 1], lhsT=w1s[n][:, p, hh * 128:(hh + 1) * 128], rhs=pes[n][:, p:p + 1],
                        start=(p == 0), stop=(p == 31)), reads=[("w1", n), ("pe", n)], writes=["pb"])
                P.op("dve", lambda e, col=col: e.tensor_copy(out=bias[:, col:col + 1], in_=pb[:, col:col + 1]),
                     reads=["pb"], writes=[("bias", col)])
                for (a, b) in halves:
                    pi = cnt[0] % 2
                    cnt[0] += 1
                    nn = b - a
                    for p in range(32):
                        P.op("pe", lambda e, n=n, hh=hh, p=p, a=a, nn=nn, pi=pi: e.matmul(
                            ph[pi][:, 0:nn], lhsT=w1s[n][:, p, hh * 128:(hh + 1) * 128],
                            rhs=raw[n][:, 16 * a + p:16 * a + p + 16 * (nn - 1) + 1:16],
                            start=(p == 0), stop=(p == 31)), reads=[("w1", n), ("raw", n)], writes=[("ph", pi)])
                    P.op("act", lambda e, n=n, hh=hh, a=a, b=b, nn=nn, pi=pi, col=col: e.activation(
                        out=sil[n][:, hh, a:b], in_=ph[pi][:, 0:nn], func=AF.Silu, bias=bias[:, col:col + 1]),
                        reads=[("ph", pi), ("bias", col)], writes=[("sil", n)])
        for (a, b) in halves:
            nn = b - a
            for vi, wn in enumerate(("k", "ks")):
                for hh in range(2):
                    P.op("pe", lambda e, wn=wn, hh=hh, a=a, b=b, nn=nn, vi=vi: e.matmul(
                        pk[vi][:, 0:nn], lhsT=w2sb[wn][:, hh, :], rhs=sil["k"][:, hh, a:b],
                        start=(hh == 0), stop=(hh == 1)), reads=[("w2", wn), ("sil", "k")], writes=[("pk", vi)])
            P.op("dve", lambda e, a=a, b=b, nn=nn: e.tensor_tensor(out=t1[:, 0:nn], in0=pk[0][:, 0:nn],
                                                                   in1=cosT[:, a:b], op=ALU.mult),
                 reads=[("pk", 0), ("rp", "cos")], writes=["t1"])
            P.op("dve", lambda e, a=a, b=b, nn=nn: e.tensor_tensor(out=t2[:, 0:nn], in0=pk[1][:, 0:nn],
                                                                   in1=sinT[:, a:b], op=ALU.mult),
                 reads=[("pk", 1), ("rp", "sin")], writes=["t2"])
            P.op("dve", lambda e, a=a, b=b, nn=nn: e.tensor_tensor(out=ko[:, a:b], in0=t1[:, 0:nn], in1=t2[:, 0:nn],
                                                                   op=ALU.add),
                 reads=["t1", "t2"], writes=["ko"])
        P.dma("sp", KCMPT[:, :], ko[:], reads=["ko"])
        for ct in range(NCP // 128):
            for hh in range(2):
                P.op("pe", lambda e, hh=hh, ct=ct: e.matmul(
                    pk[0][:, 0:128], lhsT=sil["v"][:, hh, ct * 128:(ct + 1) * 128], rhs=w2sb["v"][:, hh, :],
                    start=(hh == 0), stop=(hh == 1)), reads=[("w2", "v"), ("sil", "v")], writes=[("pk", 0)])
            P.op("dve", lambda e: e.tensor_copy(out=vo[:], in_=pk[0][:, 0:128]), reads=[("pk", 0)], writes=["vo"])
            P.dma("sp", VCMP[ct * 128:(ct + 1) * 128, :], vo[:], reads=["vo"])
        P.finish()
    return nc


SCALE = 128 ** -0.5
BIGV = 1.0e4


def attn_consts(par):
    p = np.arange(128)[:, None]
    q = np.arange(128)[None, :]
    mc = np.zeros((128, 8, 128), np.float32)
    for r in range(8):
        mc[:, r, :] = (16 * (p - 16 * r - 8 * par) + 31 <= q)
    mcp = np.ones((128, 128), np.float32)
    mcp[127, :] = (15 - 128 * par <= np.arange(128))
    A = np.zeros((128, 8, 256), np.float32)
    blk = np.arange(256)[None, :]
    for jc in range(8):
        c = 128 * jc + p
        A[:, jc, :] = (4 * blk - 1 <= c) & (c <= 4 * blk + 3)
    qq = np.arange(128)[:, None]
    d = np.arange(512)[None, :] - 256
    curd = 2 * par + (qq >= 64)
    bc = np.where((d == curd) | (d == curd - 1), BIGV, np.where(d > curd, -BIGV, 0.0)).astype(np.float32)
    tri = (p <= q).astype(np.float32)
    gtr = (p > q).astype(np.float32)
    one = np.ones((128, 128), np.float32)
    zero = np.zeros((128, 128), np.float32)
    cms = np.stack([tri, zero] if par == 0 else [one, tri], 1)
    cmw = np.stack([gtr, one, one, one, tri, zero] if par == 0 else [zero, gtr, one, one, one, tri], 1)
    y = np.arange(8192)[None, :]
    G = (y // 64 == np.arange(128)[:, None]).astype(np.float32)
    f = lambda a: np.ascontiguousarray(a).astype(NPBF)
    return dict(mc=f(mc), mcp=f(mcp), amat=f(A), bcst=bc, cms=f(cms), cmw=f(cmw), gexp=f(G),
                zeros=np.zeros((128, 512), NPBF))


def build_attn(S):
    SQ = S // 2
    NQ = SQ // 128
    NKT = S // 128
    NCP = S // 16
    NCT = max(1, NCP // 128)
    nc = bass.Bass("TRN2", target_bir_lowering=False)
    dt = nc.dram_tensor
    inp = lambda n, sh, d: dt(n, sh, d, kind="ExternalInput").ap()
    QT = inp("QT", [8, 128, SQ], BF16)
    KST = inp("KST", [128, S], BF16)
    KWT = inp("KWT", [128, S], BF16)
    VG = inp("VG", [S, 288], BF16)
    GQ = inp("GQ", [SQ, 32], BF16)
    KCMPT = inp("KCMPT", [128, NCP], BF16)
    VCMP = inp("VCMP", [NCP, 128], BF16)
    mc_d = inp("mc", [128, 8, 128], BF16)
    mcp_d = inp("mcp", [128, 128], BF16)
    a_d = inp("amat", [128, 8, 256], BF16)
    bc_d = inp("bcst", [128, 512], F32)
    cms_d = inp("cms", [128, 2, 128], BF16)
    cmw_d = inp("cmw", [128, 6, 128], BF16)
    g_d = inp("gexp", [128, 8192], BF16)
    z_d = inp("zeros", [128, 512], BF16)
    id_d = inp("ident", [128, 128], BF16)
    OA = dt("OA", [SQ, 1024], BF16, kind="ExternalOutput").ap()
    with ExitStack() as es:
        P = Prog(nc, es)
        kst = P.sb([128, S], BF16)
        kwt = P.sb([128, S], BF16)
        vs1 = P.sb([128, NKT, 129], BF16)
        vw1 = P.sb([128, NKT, 129], BF16)
        kcm = P.sb([128, NCP], BF16)
        vc1 = P.sb([128, NCT, 129], BF16)
        mc = P.sb([128, 8, 128], BF16)
        mcp = P.sb([128, 128], BF16)
        amat = P.sb([128, 8, 256], BF16)
        bcst = P.sb([128, 512], F32)
        cms = P.sb([128, 2, 128], BF16)
        cmw = P.sb([128, 6, 128], BF16)
        gexp = P.sb([128, 8192], BF16)
        zeros = P.sb([128, 512], BF16)
        idn = P.sb([128, 128], BF16)
        for t_, d_, k_ in ((kst, KST, "kst"), (kwt, KWT, "kwt"), (kcm, KCMPT, "kcm"), (mcp, mcp_d, "mcp"),
                           (bcst, bc_d, "bcst"), (gexp, g_d, "gexp"), (zeros, z_d, "zeros"), (idn, id_d, "idn")):
            P.dma("sp", t_[:], d_[:, :], writes=[k_])
        for t_, d_, k_ in ((mc, mc_d, "mc"), (amat, a_d, "amat"), (cms, cms_d, "cms"), (cmw, cmw_d, "cmw")):
            P.dma("sp", t_[:], d_[:, :, :], writes=[k_])
        for t_, k_ in ((vs1, "vs1"), (vw1, "vw1"), (vc1, "vc1")):
            P.op("pool", lambda e, t_=t_: e.memset(t_[:], 1.0), writes=[k_])
        P.dma("sp", vs1[:, :, 0:128], VG[:, 0:128].rearrange("(t p) d -> p t d", p=128), writes=["vs1"])
        P.dma("sp", vw1[:, :, 0:128], VG[:, 128:256].rearrange("(t p) d -> p t d", p=128), writes=["vw1"])
        if NCP >= 128:
            P.dma("sp", vc1[:, :, 0:128], VCMP.rearrange("(t p) d -> p t d", p=128), writes=["vc1"])
        qT = [P.sb([128, 8, 128], BF16) for _ in range(2)]
        gq = [P.sb([128, 32], BF16) for _ in range(2)]
        et = P.sb([128, NCT, 1024], BF16)
        pt = [P.sb([128, 1024], BF16) for _ in range(2)]
        mk = [P.sb([128, 128], BF16) for _ in range(2)]
        imp = P.sb([128, 256], F32)
        sc2 = P.sb([128, 256], F32)
        m8 = P.sb([128, 16], F32)
        sel = P.sb([128, 256], BF16)
        selT = P.sb([128, 2, 128], BF16)
        zall = P.sb([128, 8], F32)
        coef = P.sb([128, 8], F32)
        oacc = P.sb([128, 1024], F32)
        obf = [P.sb([128, 1024], BF16) for _ in range(2)]
        ST = [P.ps([128, 1024], F32) for _ in range(2)]
        OB = [P.ps([128, 512], F32) for _ in range(3)]
        MS = P.ps([128, 512], F32)
        cnt = {}

        def nxt(k, n=2):
            v = cnt.get(k, 0)
            cnt[k] = v + 1
            return v % n

        def oreg(h):
            return OB[h // 3][:, (h % 3) * 129:(h % 3) * 129 + 129]

        def zero_acc():
            for b in range(3):
                P.op("pe", lambda e, b=b: e.matmul(OB[b][:], lhsT=zeros[:, 0:128], rhs=zeros[:], start=True,
                                                  stop=False, skip_group_check=True),
                     reads=["zeros"], writes=[("OB", b)])

        def scores(ktile_ap, qi, kkey):
            si = nxt("ST")
            for hf in range(2):
                P.op("pe", lambda e, hf=hf, si=si: e.matmul(
                    ST[si][:, hf * 512:(hf + 1) * 512], lhsT=ktile_ap, rhs=qT[qi][:, hf * 4:(hf + 1) * 4, :],
                    start=True, stop=True), reads=[kkey, ("qT", qi)], writes=[("ST", si)])
            return si

        def pv(p_ap_fn, pkey, v_ap, vkey, last):
            for h in range(8):
                P.op("pe", lambda e, h=h: e.matmul(oreg(h), lhsT=p_ap_fn(h), rhs=v_ap, start=False, stop=last,
                                                   skip_group_check=True),
                     reads=[pkey, vkey], writes=[("OB", h // 3)])

        def combine(br, qi, first):
            for b in range(3):
                nh = 3 if b < 2 else 2
                P.op("dve", lambda e, b=b, nh=nh: e.tensor_copy(
                    out=zall[:, b * 3:b * 3 + nh],
                    in_=OB[b][:, 0:nh * 129].rearrange("p (h c) -> p h c", c=129)[:, :, 128]),
                    reads=[("OB", b)], writes=["zall"])
            P.op("dve", lambda e: e.tensor_scalar(out=zall[:], in0=zall[:], scalar1=1e-30, scalar2=None,
                                                  op0=ALU.max), reads=["zall"], writes=["zall"])
            P.op("dve", lambda e: e.reciprocal(out=zall[:], in_=zall[:]), reads=["zall"], writes=["zall"])
            P.op("dve", lambda e: e.tensor_tensor(
                out=coef[:], in0=zall[:], in1=gq[qi][:, 0:24].rearrange("p (h c) -> p h c", c=3)[:, :, br],
                op=ALU.mult), reads=["zall", ("gq", qi)], writes=["coef"])
            for h in range(8):
                osl = oacc[:, h * 128:(h + 1) * 128]
                if first:
                    P.op("dve", lambda e, h=h, osl=osl: e.tensor_scalar(
                        out=osl, in0=oreg(h)[:, 0:128], scalar1=coef[:, h:h + 1], scalar2=None, op0=ALU.mult),
                        reads=[("OB", h // 3), "coef"], writes=["oacc"])
                else:
                    P.op("dve", lambda e, h=h, osl=osl: e.scalar_tensor_tensor(
                        out=osl, in0=oreg(h)[:, 0:128], scalar=coef[:, h:h + 1], in1=osl, op0=ALU.mult,
                        op1=ALU.add), reads=[("OB", h // 3), "coef", "oacc"], writes=["oacc"])

        def bmul(buf_ap, m_ap):
            return lambda e: e.tensor_tensor(out=buf_ap.rearrange("p (h q) -> p h q", h=8),
                                             in0=buf_ap.rearrange("p (h q) -> p h q", h=8),
                                             in1=m_ap.unsqueeze(1).to_broadcast([128, 8, 128]), op=ALU.mult)

        for i in range(NQ):
            qi = nxt("qT")
            P.dma("sp", qT[qi][:], QT[:, :, i * 128:(i + 1) * 128].rearrange("h d q -> d h q"), writes=[("qT", qi)])
            P.dma("sp", gq[qi][:], GQ[i * 128:(i + 1) * 128, :], writes=[("gq", qi)])
            jl = i // 8
            r = i % 8
            zero_acc()
            for jc in range(jl + 1):
                si = scores(kcm[:, jc * 128:(jc + 1) * 128], qi, "kcm")
                P.op("act", lambda e, si=si, jc=jc: e.activation(out=et[:, jc, :], in_=ST[si][:], func=AF.Exp,
                                                               scale=SCALE),
                     reads=[("ST", si)], writes=[("et", jc)])
                if jc == jl:
                    P.op("dve", bmul(et[:, jc, :], mc[:, r, :]), reads=[("et", jc), "mc"], writes=[("et", jc)])
                elif jc == jl - 1 and r == 0:
                    P.op("dve", bmul(et[:, jc, :], mcp[:]), reads=[("et", jc), "mcp"], writes=[("et", jc)])
            for jc in range(jl + 1):
                pv(lambda h, jc=jc: et[:, jc, h * 128:(h + 1) * 128], ("et", jc), vc1[:, jc, :], "vc1", jc == jl)
            combine(0, qi, True)
            P.op("dve", lambda e: e.tensor_scalar(out=coef[:], in0=zall[:], scalar1=1.0, scalar2=None, op0=ALU.mult),
                 reads=["zall"], writes=["coef2"]) if False else None
            for h in range(8):
                for jc in range(jl + 1):
                    P.op("pe", lambda e, h=h, jc=jc: e.matmul(MS[:, 0:256], lhsT=et[:, jc, h * 128:(h + 1) * 128],
                                                             rhs=amat[:, jc, :], start=(jc == 0), stop=(jc == jl)),
                         reads=[("et", jc), "amat"], writes=["MS"])
                if h == 0:
                    P.op("dve", lambda e: e.tensor_scalar(out=imp[:], in0=MS[:, 0:256], scalar1=zall[:, 0:1],
                                                          scalar2=None, op0=ALU.mult),
                         reads=["MS", "zall"], writes=["imp"])
                else:
                    P.op("dve", lambda e, h=h: e.scalar_tensor_tensor(
                        out=imp[:], in0=MS[:, 0:256], scalar=zall[:, h:h + 1], in1=imp[:], op0=ALU.mult,
                        op1=ALU.add), reads=["MS", "zall", "imp"], writes=["imp"])
            P.op("dve", lambda e, i=i: e.tensor_tensor(out=imp[:], in0=imp[:], in1=bcst[:, 256 - 4 * i:512 - 4 * i],
                                                      op=ALU.add), reads=["imp", "bcst"], writes=["imp"])
            P.op("dve", lambda e: e.memset(imp[:, 0:1], BIGV), reads=["imp"], writes=["imp"])
            P.op("dve", lambda e: e.max(out=m8[:, 0:8], in_=imp[:]), reads=["imp"], writes=["m8"])
            P.op("dve", lambda e: e.match_replace(out=sc2[:], in_to_replace=m8[:, 0:8], in_values=imp[:],
                                                  imm_value=-3.0e4), reads=["imp", "m8"], writes=["sc2"])
            P.op("dve", lambda e: e.max(out=m8[:, 8:16], in_=sc2[:]), reads=["sc2"], writes=["m8"])
            P.op("dve", lambda e: e.tensor_scalar(out=sel[:], in0=imp[:], scalar1=m8[:, 15:16], scalar2=None,
                                                  op0=ALU.is_ge), reads=["imp", "m8"], writes=["sel"])
            nchunk = 1 if (4 * i + 3) < 128 else 2
            MSb = MS[:].bitcast(BF16)
            for c in range(nchunk):
                P.op("pe", lambda e, c=c: e.transpose(out=MSb[:, c * 128:(c + 1) * 128],
                                                      in_=sel[:, c * 128:(c + 1) * 128], identity=idn[:]),
                     reads=["sel", "idn"], writes=["MS"])
            P.op("dve", lambda e, nchunk=nchunk: e.tensor_copy(
                out=selT[:, 0:nchunk, :], in_=MSb[:, 0:nchunk * 128].rearrange("p (c q) -> p c q", c=nchunk)),
                reads=["MS"], writes=["selT"])
            zero_acc()
            nkt = 2 * i + 2
            for kt in range(nkt):
                si = scores(kst[:, kt * 128:(kt + 1) * 128], qi, "kst")
                pi = nxt("pt")
                P.op("act", lambda e, si=si, pi=pi: e.activation(out=pt[pi][:], in_=ST[si][:], func=AF.Exp,
                                                               scale=SCALE),
                     reads=[("ST", si)], writes=[("pt", pi)])
                P.op("pe", lambda e, kt=kt: e.matmul(MS[:, 0:128], lhsT=gexp[:, (kt % 64) * 128:(kt % 64 + 1) * 128],
                                                    rhs=selT[:, kt // 64, :], start=True, stop=True),
                     reads=["gexp", "selT"], writes=["MS"])
                mi = nxt("mk")
                if kt >= 2 * i:
                    P.op("dve", lambda e, mi=mi, kt=kt, i=i: e.tensor_tensor(out=mk[mi][:], in0=MS[:, 0:128],
                                                                          in1=cms[:, kt - 2 * i, :], op=ALU.mult),
                         reads=["MS", "cms"], writes=[("mk", mi)])
                else:
                    P.op("dve", lambda e, mi=mi: e.tensor_copy(out=mk[mi][:], in_=MS[:, 0:128]),
                         reads=["MS"], writes=[("mk", mi)])
                P.op("dve", bmul(pt[pi][:], mk[mi][:]), reads=[("pt", pi), ("mk", mi)], writes=[("pt", pi)])
                pv(lambda h, pi=pi: pt[pi][:, h * 128:(h + 1) * 128], ("pt", pi), vs1[:, kt, :], "vs1",
                   kt == nkt - 1)
            combine(1, qi, False)
            zero_acc()
            wt_list = [(kt, w) for w, kt in enumerate(range(2 * i - 4, 2 * i + 2)) if kt >= 0]
            for n_, (kt, w) in enumerate(wt_list):
                si = scores(kwt[:, kt * 128:(kt + 1) * 128], qi, "kwt")
                pi = nxt("pt")
                P.op("act", lambda e, si=si, pi=pi: e.activation(out=pt[pi][:], in_=ST[si][:], func=AF.Exp,
                                                               scale=SCALE),
                     reads=[("ST", si)], writes=[("pt", pi)])
                P.op("dve", bmul(pt[pi][:], cmw[:, w, :]), reads=[("pt", pi), "cmw"], writes=[("pt", pi)])
                pv(lambda h, pi=pi: pt[pi][:, h * 128:(h + 1) * 128], ("pt", pi), vw1[:, kt, :], "vw1",
                   n_ == len(wt_list) - 1)
            combine(2, qi, False)
            oi = nxt("obf")
            P.op("act", lambda e, oi=oi: e.activation(out=obf[oi][:], in_=oacc[:], func=AF.Copy),
                 reads=["oacc"], writes=[("obf", oi)])
            P.dma("sp", OA[i * 128:(i + 1) * 128, :], obf[oi][:], reads=[("obf", oi)])
        P.finish()
    return nc


_SW = np.concatenate([np.arange(64, 128), np.arange(64)])


def kernel(x, positions, norm_w, w_in, cmp_pe_k, cmp_w1_k, cmp_w2_k, cmp_pe_v, cmp_w1_v, cmp_w2_v,
           conv_w, conv_b, dt_bias, a_log, d_skip, norm_attn_w, norm_ssm_w, w_out, final_norm_w):
    f32 = lambda a: np.ascontiguousarray(np.asarray(a), dtype=np.float32)
    x = f32(x)
    positions = np.ascontiguousarray(np.asarray(positions), dtype=np.int32)
    norm_w, w_in, w_out = f32(norm_w), f32(w_in), f32(w_out)
    cmp_pe_k, cmp_w1_k, cmp_w2_k = f32(cmp_pe_k), f32(cmp_w1_k), f32(cmp_w2_k)
    cmp_pe_v, cmp_w1_v, cmp_w2_v = f32(cmp_pe_v), f32(cmp_w1_v), f32(cmp_w2_v)
    conv_w, conv_b, dt_bias, a_log, d_skip = f32(conv_w), f32(conv_b), f32(dt_bias), f32(a_log), f32(d_skip)
    norm_attn_w, norm_ssm_w, final_norm_w = f32(norm_attn_w), f32(norm_ssm_w), f32(final_norm_w)
    B, S, _ = x.shape
    cores = list(range(8))
    TPC = B * S // 8
    rc = rope_consts()
    ac = [attn_consts(0), attn_consts(1)]
    sc = ssd_consts()
    nc_proj, _ = build_proj(S)
    nc_cmp = build_cmp(S)
    nc_attn = build_attn(S)
    nc_ssd, _ = build_ssd(S)
    run = lambda nc, maps: run_bass_kernel_spmd(nc, maps, core_ids=cores).results
    for l in range(DEPTH):
        w = w_in[l]
        maps = []
        for c in cores:
            b, g, par, j = core_coords(c)
            wf, wt = proj_weight_layout(w, g, j)
            maps.append(dict(x=x[b], pos=positions[b], xq=my_tiles(x[b], par), posq=my_tiles(positions[b], par),
                             nw=norm_w[l], wf=wf, wt=wt, ident=IDENT_BF, invf=rc))
        rp = run(nc_proj, maps)
        del maps
        maps = []
        for c in cores:
            b, g, par, j = core_coords(c)
            pc = positions[b, 31::16]
            maps.append(dict(KCT=rp[c]["KCT"], VCT=rp[c]["VCT"], w1k=cmp_w1_k[l], w1v=cmp_w1_v[l],
                             w2k=cmp_w2_k[l], w2v=cmp_w2_v[l], w2ks=np.ascontiguousarray(cmp_w2_k[l][:, _SW]),
                             peTk=np.ascontiguousarray(cmp_pe_k[l].T), peTv=np.ascontiguousarray(cmp_pe_v[l].T),
                             posc=np.ascontiguousarray(np.concatenate([pc, pc[-1:]]), dtype=np.int32), invf=rc))
        rcm = run(nc_cmp, maps)
        maps = []
        for c in cores:
            b, g, par, j = core_coords(c)
            vg = np.asarray(rp[c]["VG"])
            m = dict(QT=rp[c]["QT"], KST=rp[c]["KST"], KWT=rp[c]["KWT"], VG=vg,
                     GQ=my_tiles(np.ascontiguousarray(vg[:, 256:288]), par),
                     KCMPT=rcm[c]["KCMPT"], VCMP=rcm[c]["VCMP"], ident=IDENT_BF)
            m.update(ac[par])
            maps.append(m)
        ra = run(nc_attn, maps)
        maps = []
        for c in cores:
            b, g, par, j = core_coords(c)
            m = dict(XBCT=rp[c]["XBCT"], DTT=rp[c]["DTT"], ident=IDENT_BF)
            m.update(sc)
            m.update(ssd_param_layout(conv_w[l], conv_b[l], dt_bias[l], a_log[l], d_skip[l], j))
            maps.append(m)
        rs = run(nc_ssd, maps)
        del rp
        oa = np.zeros((B, S, 2048), NPBF)
        ys = np.zeros((B, S, 2048), NPBF)
        for c in cores:
            b, g, par, j = core_coords(c)
            oa[b].reshape(S // 256, 2, 128, 2048)[:, par, :, g * 1024:(g + 1) * 1024] = \
                np.asarray(ra[c]["OA"]).reshape(S // 256, 128, 1024)
            ys[b, :, 512 * j:512 * (j + 1)] = np.asarray(rs[c]["YS"])
        xf = x.reshape(B * S, D_MODEL)
        oaf = oa.reshape(B * S, 2048)
        ysf = ys.reshape(B * S, 2048)
        wz = np.ascontiguousarray(np.concatenate([w[:, OFF_ZA:OFF_ZA + 2048], w[:, OFF_ZS:OFF_ZS + 2048]], 1))
        nc_out = build_out(TPC, l == DEPTH - 1)
        maps = []
        for c in cores:
            r = slice(c * TPC, (c + 1) * TPC)
            maps.append(dict(x=xf[r], oa=oaf[r], ys=ysf[r], wz=wz, wo=w_out[l], nw=norm_w[l], nwa=norm_attn_w[l],
                             nws=norm_ssm_w[l], fnw=final_norm_w, ident=IDENT_BF))
        ro = run(nc_out, maps)
        x = np.concatenate([np.asarray(ro[c]["y"]) for c in cores], 0).reshape(B, S, D_MODEL).astype(np.float32)
    return x
```

```python
import math, os
from contextlib import ExitStack
import numpy as np
import ml_dtypes
import concourse.bass as bass
import concourse.mybir as mybir
from concourse.bass_utils import run_bass_kernel_spmd

F32 = mybir.dt.float32
BF16 = mybir.dt.bfloat16
I32 = mybir.dt.int32
AF = mybir.ActivationFunctionType
ALU = mybir.AluOpType
AX = mybir.AxisListType
NPBF = ml_dtypes.bfloat16

D_MODEL = 2048
DEPTH = 2
HEAD_DIM = 128
D_PROJ = 11856
EPS = 1e-6
OFF_Q, OFF_KC, OFF_VC, OFF_KS, OFF_VS, OFF_KW, OFF_VW = 0, 2048, 2304, 2560, 2816, 3072, 3328
OFF_GATE, OFF_ZA, OFF_X, OFF_B, OFF_C, OFF_DT, OFF_ZS = 3584, 3632, 5680, 7728, 8752, 9776, 9808
KC = D_MODEL // 128


class Prog:
    STREAMS = ("pe", "act", "dve", "pool", "sp")

    def __init__(self, nc, es, n_dma_sems=10):
        self.nc, self.es = nc, es
        self.ops = {s: [] for s in self.STREAMS}
        self.sems = []
        self.csem = {}
        for s in ("pe", "act", "dve", "pool"):
            self.csem[s] = self._new_sem("c_" + s)
        self.ccnt = {s: 0 for s in self.csem}
        self.dsem = {q: [[self._new_sem(f"d_{q}{i}"), 0] for i in range(n_dma_sems)]
                     for q in ("sp", "act", "pool")}
        self.drr = {q: 0 for q in self.dsem}
        self.lastw = {}
        self.readers = {}
        self.known = {s: {} for s in self.STREAMS}
        self.nsb = 0

    def _new_sem(self, name):
        h = self.es.enter_context(self.nc.semaphore(name))
        self.sems.append(h)
        return len(self.sems) - 1

    def sb(self, shape, dt, name=None):
        self.nsb += 1
        return self.es.enter_context(self.nc.sbuf_tensor(name or f"sb{self.nsb}", list(shape), dt))

    def ps(self, shape, dt, name=None):
        self.nsb += 1
        return self.es.enter_context(self.nc.psum_tensor(name or f"ps{self.nsb}", list(shape), dt))

    def _emit(self, stream, fn, reads, writes, dma):
        deps = []
        for b in reads:
            w = self.lastw.get(b)
            if w:
                deps.append(w)
        for b in writes:
            w = self.lastw.get(b)
            if w:
                deps.append(w)
            deps.extend(self.readers.get(b, {}).items())
        if dma:
            pool = self.dsem[stream]
            slot = pool[self.drr[stream] % len(pool)]
            self.drr[stream] += 1
            if slot[1] > 0:
                deps.append((slot[0], slot[1]))
            slot[1] += 16
            tok = (slot[0], slot[1])
            inc = 16
        else:
            self.ccnt[stream] += 1
            tok = (self.csem[stream], self.ccnt[stream])
            inc = 1
        waits = {}
        kn = self.known[stream]
        for sid, v in deps:
            if stream == "pe" and not dma and sid == self.csem["pe"]:
                continue
            if kn.get(sid, 0) >= v:
                continue
            if waits.get(sid, 0) < v:
                waits[sid] = v
        for sid, v in waits.items():
            kn[sid] = v
        self.ops[stream].append((sorted(waits.items()), fn, tok[0], inc))
        for b in reads:
            r = self.readers.setdefault(b, {})
            if r.get(tok[0], 0) < tok[1]:
                r[tok[0]] = tok[1]
        for b in writes:
            self.lastw[b] = tok
            self.readers[b] = {}
        return tok

    def op(self, stream, fn, reads=(), writes=()):
        return self._emit(stream, fn, reads, writes, False)

    def dma(self, queue, out, in_, reads=(), writes=(), **kw):
        return self._emit(queue, lambda e: e.dma_start(out=out, in_=in_, **kw), reads, writes, True)

    def check(self):
        val = [0] * len(self.sems)
        pc = {s: 0 for s in self.STREAMS}
        prog = True
        while prog:
            prog = False
            for s in self.STREAMS:
                ops = self.ops[s]
                while pc[s] < len(ops):
                    waits, fn, sid, inc = ops[pc[s]]
                    if all(val[ws] >= wv for ws, wv in waits):
                        val[sid] += inc
                        pc[s] += 1
                        prog = True
                    else:
                        break
        stuck = {s: (pc[s], len(self.ops[s])) for s in self.STREAMS if pc[s] < len(self.ops[s])}
        if stuck:
            msg = []
            for s, (i, n) in stuck.items():
                waits = self.ops[s][i][0]
                msg.append(f"{s} stuck at {i}/{n}: waits {[(ws, wv, val[ws]) for ws, wv in waits]}")
            raise RuntimeError("DEADLOCK: " + "; ".join(msg))

    def finish(self):
        self.check()
        finals = []
        for q in self.dsem:
            for sid, v in self.dsem[q]:
                if v > 0:
                    finals.append((sid, v))
        for s in self.csem:
            if self.ccnt[s] > 0:
                finals.append((self.csem[s], self.ccnt[s]))
        nc = self.nc
        ops, sems = self.ops, self.sems
        with nc.Block() as blk:
            def run(stream, e):
                for waits, fn, sid, inc in ops[stream]:
                    for ws, wv in waits:
                        e.wait_ge(sems[ws], wv)
                    fn(e).then_inc(sems[sid], inc)

            @blk.tensor
            def _(e):
                run("pe", e)

            @blk.scalar
            def _(e):
                run("act", e)

            @blk.vector
            def _(e):
                run("dve", e)

            @blk.gpsimd
            def _(e):
                run("pool", e)

            @blk.sync
            def _(e):
                run("sp", e)
                for ws, wv in finals:
                    e.wait_ge(sems[ws], wv)


def _bcast_rows(ap_1d, n=128):
    return ap_1d.partition_broadcast(n)


def rms_rstd(P, ss, rstd, inv_n, kss, krs):
    P.op("dve", lambda e: e.tensor_scalar(out=ss, in0=ss, scalar1=inv_n, scalar2=EPS,
                                          op0=ALU.mult, op1=ALU.add), reads=[kss], writes=[kss])
    P.op("act", lambda e: e.activation(out=ss, in_=ss, func=AF.Sqrt), reads=[kss], writes=[kss])
    P.op("dve", lambda e: e.reciprocal(out=rstd, in_=ss), reads=[kss], writes=[krs])


def norm_to_hT(P, xt, kx, hbt, khb, sst, kss, rst, krs, nw_bc, knw, tpt, ktp, idn, hT_dst, khT):
    P.op("act", lambda e: e.activation(out=hbt, in_=xt, func=AF.Square, accum_out=sst),
         reads=[kx], writes=[khb, kss])
    rms_rstd(P, sst, rst, 1.0 / D_MODEL, kss, krs)
    P.op("dve", lambda e: e.scalar_tensor_tensor(out=hbt, in0=xt, scalar=rst, in1=nw_bc,
                                                 op0=ALU.mult, op1=ALU.mult),
         reads=[kx, krs, knw], writes=[khb])
    for k in range(KC):
        P.op("pe", lambda e, k=k: e.transpose(out=tpt[:, k * 128:(k + 1) * 128],
                                              in_=hbt[:, k * 128:(k + 1) * 128], identity=idn),
             reads=[khb, "idn"], writes=[ktp])
    P.op("act", lambda e: e.activation(out=hT_dst, in_=tpt.rearrange("p (k t) -> p k t", k=KC), func=AF.Copy),
         reads=[ktp], writes=[khT])


def build_out(TPC, last):
    nc = bass.Bass("TRN2", target_bir_lowering=False)
    dt = nc.dram_tensor
    x = dt("x", [TPC, D_MODEL], F32, kind="ExternalInput").ap()
    oa = dt("oa", [TPC, 2048], BF16, kind="ExternalInput").ap()
    ys = dt("ys", [TPC, 2048], BF16, kind="ExternalInput").ap()
    wz = dt("wz", [D_MODEL, 4096], F32, kind="ExternalInput").ap()
    wo = dt("wo", [4096, D_MODEL], F32, kind="ExternalInput").ap()
    nw = dt("nw", [D_MODEL], F32, kind="ExternalInput").ap()
    nwa = dt("nwa", [2048], F32, kind="ExternalInput").ap()
    nws = dt("nws", [2048], F32, kind="ExternalInput").ap()
    fnw = dt("fnw", [D_MODEL], F32, kind="ExternalInput").ap()
    ident = dt("ident", [128, 128], BF16, kind="ExternalInput").ap()
    y = dt("y", [TPC, D_MODEL], F32, kind="ExternalOutput").ap()
    TB = min(512, TPC)
    NT = TB // 128
    NB = TPC // TB
    wz_v = wz.rearrange("(kc p) c -> p kc c", p=128)
    wo_v = wo.rearrange("(kc p) c -> p kc c", p=128)
    with ExitStack() as es:
        P = Prog(nc, es)
        idn = P.sb([128, 128], BF16)
        nw_bc = P.sb([128, 2048], F32)
        nwa_bc = P.sb([128, 2048], BF16)
        nws_bc = P.sb([128, 2048], BF16)
        fnw_bc = P.sb([128, 2048], F32)
        P.dma("sp", idn[:], ident[:, :], writes=["idn"])
        P.dma("sp", nw_bc[:], _bcast_rows(nw), writes=["nw"])
        P.dma("pool", nwa_bc[:], _bcast_rows(nwa), writes=["nwa"])
        P.dma("pool", nws_bc[:], _bcast_rows(nws), writes=["nws"])
        P.dma("sp", fnw_bc[:], _bcast_rows(fnw), writes=["fnw"])
        res = [P.sb([128, 2048], F32) for _ in range(NT)]
        hb = [P.sb([128, 2048], BF16) for _ in range(2)]
        hT = P.sb([128, KC, TB], BF16)
        wblk = [P.sb([128, 8192], BF16) for _ in range(2)]
        ob = [P.sb([128, 512], BF16) for _ in range(2)]
        zs = [P.sb([128, 512], F32) for _ in range(2)]
        u = [P.sb([128, 4096], BF16) for _ in range(NT)]
        mT = P.sb([128, 32, TB], BF16)
        ss = [P.sb([128, 16], F32) for _ in range(NT)]
        rs = [P.sb([128, 16], F32) for _ in range(NT)]
        tp = [P.ps([128, 2048], BF16) for _ in range(2)]
        mm = [P.ps([128, 512], F32) for _ in range(2)]
        cnt = {"hb": 0, "w": 0, "mm": 0, "tp": 0, "ob": 0}

        def nxt(k, n=2):
            v = cnt[k] % n
            cnt[k] += 1
            return v

        for blk_i in range(NB):
            t0 = blk_i * TB
            for t in range(NT):
                rows = slice(t0 + t * 128, t0 + (t + 1) * 128)
                P.dma("sp", res[t][:], x[rows, :], writes=[("res", t)])
                b = nxt("hb")
                pb = nxt("tp")
                norm_to_hT(P, res[t][:], ("res", t), hb[b][:], ("hb", b), ss[t][:, 15:16], ("ssn", t),
                           rs[t][:, 15:16], ("rsn", t), nw_bc[:], "nw", tp[pb][:], ("tp", pb), idn[:],
                           hT[:, :, t * 128:(t + 1) * 128], ("hT", t))
            for t in range(NT):
                P.op("pool", lambda e, t=t: e.memset(ss[t][:, 0:13], 0.0), writes=[("ss2", t)])
            for cb in range(8):
                wbi = nxt("w")
                wv = wblk[wbi][:].rearrange("p (k c) -> p k c", k=KC)
                P.dma("pool", wv, wz_v[:, :, cb * 512:(cb + 1) * 512], writes=[("w", wbi)])
                for t in range(NT):
                    rows = slice(t0 + t * 128, t0 + (t + 1) * 128)
                    mi = nxt("mm")
                    for k in range(KC):
                        P.op("pe", lambda e, k=k, t=t, mi=mi, wv=wv: e.matmul(
                            mm[mi][:], lhsT=hT[:, k, t * 128:(t + 1) * 128], rhs=wv[:, k, :],
                            start=(k == 0), stop=(k == KC - 1)),
                            reads=[("hT", t), ("w", wbi)], writes=[("mm", mi)])
                    oi = nxt("ob")
                    src = oa if cb < 4 else ys
                    c0 = (cb % 4) * 512
                    P.dma("sp", ob[oi][:], src[rows, c0:c0 + 512], writes=[("ob", oi)])
                    P.op("act", lambda e, mi=mi, oi=oi: e.activation(out=zs[oi][:], in_=mm[mi][:], func=AF.Silu),
                         reads=[("mm", mi)], writes=[("zs", oi)])
                    ucol = cb * 512
                    P.op("dve", lambda e, oi=oi, t=t, ucol=ucol: e.tensor_tensor(
                        out=u[t][:, ucol:ucol + 512], in0=zs[oi][:], in1=ob[oi][:], op=ALU.mult),
                        reads=[("zs", oi), ("ob", oi)], writes=[("u", t)])
                    if cb < 4:
                        P.op("act", lambda e, oi=oi, t=t, ucol=ucol, cb=cb: e.activation(
                            out=zs[oi][:], in_=u[t][:, ucol:ucol + 512], func=AF.Square,
                            accum_out=ss[t][:, cb:cb + 1]),
                            reads=[("u", t), ("ss2", t)], writes=[("zs", oi), ("ss2", t)])
                    else:
                        for hh in range(2):
                            g = (cb - 4) * 2 + hh
                            P.op("act", lambda e, oi=oi, t=t, ucol=ucol, g=g, hh=hh: e.activation(
                                out=zs[oi][:, hh * 256:hh * 256 + 256],
                                in_=u[t][:, ucol + hh * 256:ucol + hh * 256 + 256], func=AF.Square,
                                accum_out=ss[t][:, 4 + g:5 + g]),
                                reads=[("u", t), ("ss2", t)], writes=[("zs", oi), ("ss2", t)])
            for t in range(NT):
                k2 = ("ss2", t)
                P.op("dve", lambda e, t=t: e.tensor_reduce(out=ss[t][:, 12:13], in_=ss[t][:, 0:4], axis=AX.X,
                                                          op=ALU.add), reads=[k2], writes=[k2])
                P.op("dve", lambda e, t=t: e.tensor_scalar(out=ss[t][:, 12:13], in0=ss[t][:, 12:13],
                                                          scalar1=1.0 / 2048, scalar2=EPS, op0=ALU.mult, op1=ALU.add),
                     reads=[k2], writes=[k2])
                P.op("dve", lambda e, t=t: e.tensor_scalar(out=ss[t][:, 4:12], in0=ss[t][:, 4:12],
                                                          scalar1=1.0 / 256, scalar2=EPS, op0=ALU.mult, op1=ALU.add),
                     reads=[k2], writes=[k2])
                P.op("act", lambda e, t=t: e.activation(out=ss[t][:, 4:13], in_=ss[t][:, 4:13], func=AF.Sqrt),
                     reads=[k2], writes=[k2])
                P.op("dve", lambda e, t=t: e.reciprocal(out=rs[t][:, 4:13], in_=ss[t][:, 4:13]),
                     reads=[k2], writes=[("rs2", t)])
                P.op("dve", lambda e, t=t: e.scalar_tensor_tensor(
                    out=u[t][:, 0:2048], in0=u[t][:, 0:2048], scalar=rs[t][:, 12:13], in1=nwa_bc[:],
                    op0=ALU.mult, op1=ALU.mult), reads=[("u", t), ("rs2", t), "nwa"], writes=[("u", t)])
                for g in range(8):
                    cs = slice(2048 + g * 256, 2048 + (g + 1) * 256)
                    P.op("dve", lambda e, t=t, g=g, cs=cs: e.scalar_tensor_tensor(
                        out=u[t][:, cs], in0=u[t][:, cs], scalar=rs[t][:, 4 + g:5 + g],
                        in1=nws_bc[:, g * 256:(g + 1) * 256], op0=ALU.mult, op1=ALU.mult),
                        reads=[("u", t), ("rs2", t), "nws"], writes=[("u", t)])
                for half in range(2):
                    pb = nxt("tp")
                    for k in range(16):
                        kk = half * 16 + k
                        P.op("pe", lambda e, t=t, k=k, kk=kk, pb=pb: e.transpose(
                            out=tp[pb][:, k * 128:(k + 1) * 128], in_=u[t][:, kk * 128:(kk + 1) * 128],
                            identity=idn[:]), reads=[("u", t), "idn"], writes=[("tp", pb)])
                    P.op("act", lambda e, pb=pb, t=t, half=half: e.activation(
                        out=mT[:, half * 16:(half + 1) * 16, t * 128:(t + 1) * 128],
                        in_=tp[pb][:].rearrange("p (k t) -> p k t", k=16), func=AF.Copy),
                        reads=[("tp", pb)], writes=[("mT", t)])
            for cb in range(8):
                wbi = nxt("w")
                wv = wblk[wbi][:].rearrange("p (k c) -> p k c", k=32)
                P.dma("pool", wv, wo_v[:, :, cb * 256:(cb + 1) * 256], writes=[("w", wbi)])
                for t in range(NT):
                    mi = nxt("mm")
                    for k in range(32):
                        P.op("pe", lambda e, k=k, t=t, mi=mi, wv=wv: e.matmul(
                            mm[mi][:, 0:256], lhsT=mT[:, k, t * 128:(t + 1) * 128], rhs=wv[:, k, :],
                            start=(k == 0), stop=(k == 31)),
                            reads=[("mT", t), ("w", wbi)], writes=[("mm", mi)])
                    P.op("dve", lambda e, mi=mi, t=t, cb=cb: e.tensor_tensor(
                        out=res[t][:, cb * 256:(cb + 1) * 256], in0=mm[mi][:, 0:256],
                        in1=res[t][:, cb * 256:(cb + 1) * 256], op=ALU.add),
                        reads=[("mm", mi), ("res", t)], writes=[("res", t)])
            for t in range(NT):
                rows = slice(t0 + t * 128, t0 + (t + 1) * 128)
                if last:
                    b = nxt("hb")
                    P.op("act", lambda e, t=t, b=b: e.activation(out=hb[b][:], in_=res[t][:], func=AF.Square,
                                                               accum_out=ss[t][:, 14:15]),
                         reads=[("res", t)], writes=[("hb", b), ("ssf", t)])
                    rms_rstd(P, ss[t][:, 14:15], rs[t][:, 14:15], 1.0 / D_MODEL, ("ssf", t), ("rsf", t))
                    P.op("dve", lambda e, t=t: e.scalar_tensor_tensor(
                        out=res[t][:], in0=res[t][:], scalar=rs[t][:, 14:15], in1=fnw_bc[:],
                        op0=ALU.mult, op1=ALU.mult), reads=[("res", t), ("rsf", t), "fnw"], writes=[("res", t)])
                P.dma("sp", y[rows, :], res[t][:], reads=[("res", t)])
        P.finish()
    return nc


NFB = 31
TWO_PI = 2.0 * math.pi
CW1 = 6.28125
CW2 = TWO_PI - 6.28125
PI_LO = 3.141592


def proj_decl(nc, S, kind_out):
    dt = nc.dram_tensor
    D = {}
    D["x"] = dt("x", [S, D_MODEL], F32, kind="ExternalInput").ap()
    D["pos"] = dt("pos", [S], I32, kind="ExternalInput").ap()
    D["xq"] = dt("xq", [S // 2, D_MODEL], F32, kind="ExternalInput").ap()
    D["posq"] = dt("posq", [S // 2], I32, kind="ExternalInput").ap()
    D["nw"] = dt("nw", [D_MODEL], F32, kind="ExternalInput").ap()
    D["wf"] = dt("wf", [NFB, 128, KC * 128], F32, kind="ExternalInput").ap()
    D["wt"] = dt("wt", [128, KC * 288], F32, kind="ExternalInput").ap()
    D["ident"] = dt("ident", [128, 128], BF16, kind="ExternalInput").ap()
    D["invf"] = dt("invf", [128, 2], F32, kind="ExternalInput").ap()
    D["QT"] = dt("QT", [8, 128, S // 2], BF16, kind=kind_out).ap()
    for n in ("KST", "KWT", "KCT", "VCT"):
        D[n] = dt(n, [128, S], BF16, kind=kind_out).ap()
    D["VG"] = dt("VG", [S, 288], BF16, kind=kind_out).ap()
    D["XBCT"] = dt("XBCT", [1024, S], BF16, kind=kind_out).ap()
    D["DTT"] = dt("DTT", [8, S], F32, kind=kind_out).ap()
    return D


def rope_tables(P, posf, kpos, invf, ang, kf, tmp, cosT, sinT, n, tag):
    ka, kk, kt = (tag, "ang"), (tag, "kf"), (tag, "tmp")
    kfi = kf.bitcast(I32)
    P.op("dve", lambda e: e.tensor_scalar(out=ang, in0=posf, scalar1=invf[:, 0:1], scalar2=None, op0=ALU.mult),
         reads=[kpos, "invf"], writes=[ka])
    P.op("dve", lambda e: e.tensor_scalar(out=tmp, in0=ang, scalar1=1.0 / TWO_PI, scalar2=None, op0=ALU.mult),
         reads=[ka], writes=[kt])
    P.op("dve", lambda e: e.tensor_copy(out=kfi, in_=tmp), reads=[kt], writes=[kk])
    P.op("dve", lambda e: e.tensor_copy(out=tmp, in_=kfi), reads=[kk], writes=[kt])
    P.op("dve", lambda e: e.scalar_tensor_tensor(out=ang, in0=tmp, scalar=-CW1, in1=ang, op0=ALU.mult, op1=ALU.add),
         reads=[kt, ka], writes=[ka])
    P.op("dve", lambda e: e.scalar_tensor_tensor(out=ang, in0=tmp, scalar=-CW2, in1=ang, op0=ALU.mult, op1=ALU.add),
         reads=[kt, ka], writes=[ka])

    def wrap(buf, kb):
        P.op("dve", lambda e: e.tensor_scalar(out=tmp, in0=buf, scalar1=math.pi, scalar2=-TWO_PI,
                                              op0=ALU.is_gt, op1=ALU.mult), reads=[kb], writes=[kt])
        P.op("dve", lambda e: e.tensor_tensor(out=buf, in0=buf, in1=tmp, op=ALU.add), reads=[kb, kt], writes=[kb])
        P.op("dve", lambda e: e.tensor_scalar(out=tmp, in0=buf, scalar1=-math.pi, scalar2=TWO_PI,
                                              op0=ALU.is_lt, op1=ALU.mult), reads=[kb], writes=[kt])
        P.op("dve", lambda e: e.tensor_tensor(out=buf, in0=buf, in1=tmp, op=ALU.add), reads=[kb, kt], writes=[kb])
        P.op("dve", lambda e: e.tensor_scalar(out=buf, in0=buf, scalar1=PI_LO, scalar2=-PI_LO,
                                              op0=ALU.min, op1=ALU.max), reads=[kb], writes=[kb])

    wrap(ang, ka)
    kc_, ks_ = (tag, "cos"), (tag, "sin")
    P.op("act", lambda e: e.activation(out=sinT, in_=ang, func=AF.Sin, scale=invf[:, 1:2]),
         reads=[ka, "invf"], writes=[ks_])
    P.op("dve", lambda e: e.tensor_scalar(out=kf, in0=ang, scalar1=math.pi / 2, scalar2=None, op0=ALU.add),
         reads=[ka], writes=[kk])
    wrap(kf, kk)
    P.op("act", lambda e: e.activation(out=cosT, in_=kf, func=AF.Sin), reads=[kk], writes=[kc_])
    return kc_, ks_


def emit_proj(P, S, D):
    SBT = min(2048, S)
    NSB = S // SBT
    NTS = SBT // 128
    x, pos = D["x"], D["pos"]
    idn = P.sb([128, 128], BF16)
    nw_bc = P.sb([128, 2048], F32)
    invf = P.sb([128, 2], F32)
    P.dma("sp", idn[:], D["ident"][:, :], writes=["idn"])
    P.dma("sp", nw_bc[:], _bcast_rows(D["nw"]), writes=["nw"])
    P.dma("sp", invf[:], D["invf"][:, :], writes=["invf"])
    xb = [P.sb([128, 2048], F32) for _ in range(2)]
    hb = [P.sb([128, 2048], BF16) for _ in range(2)]
    ssn = P.sb([128, 4], F32)
    hT = P.sb([128, KC, SBT], BF16)
    posi = P.sb([128, SBT], I32)
    posf = P.sb([128, SBT], F32)
    ang = P.sb([128, SBT], F32)
    kf = P.sb([128, SBT], F32)
    tmp = P.sb([128, SBT], F32)
    cosT = P.sb([128, SBT], F32)
    sinT = P.sb([128, SBT], F32)
    wb = [P.sb([128, KC * 128], BF16) for _ in range(3)]
    wt = P.sb([128, KC * 288], BF16)
    stg = [P.sb([128, 512], BF16) for _ in range(4)]
    stf = [P.sb([128, 512], F32) for _ in range(2)]
    t1 = [P.sb([128, 512], F32) for _ in range(2)]
    t2 = [P.sb([128, 512], F32) for _ in range(2)]
    vst = [P.sb([128, 288], BF16) for _ in range(2)]
    tp = [P.ps([128, 2048], BF16) for _ in range(2)]
    mm = [P.ps([128, 512], F32) for _ in range(4)]
    cnt = {}

    def nxt(k, n):
        v = cnt.get(k, 0)
        cnt[k] = v + 1
        return v % n

    P.dma("pool", wt[:].rearrange("p (a c) -> p a c", c=1152), D["wt"].rearrange("p (a c) -> p a c", c=1152), writes=["wt"])
    wtv = wt[:].rearrange("p (k c) -> p k c", k=KC)
    kcos, ksin = ("rp", "cos"), ("rp", "sin")

    def prep_block(xsrc, psrc, r0, n):
        SK = os.environ.get('PROJ_SKIP', '')
        if 'r' not in SK:
            P.dma("sp", posi[:, 0:n], psrc[r0:r0 + n].partition_broadcast(128), writes=["posi"])
            P.op("dve", lambda e: e.tensor_copy(out=posf[:, 0:n], in_=posi[:, 0:n]), reads=["posi"], writes=["posf"])
        if 'r' not in SK and 'R' not in SK:
            rope_tables(P, posf[:, 0:n], "posf", invf, ang[:, 0:n], kf[:, 0:n], tmp[:, 0:n], cosT[:, 0:n],
                        sinT[:, 0:n], n, "rp")
        for t in range(n // 128):
            rows = slice(r0 + t * 128, r0 + (t + 1) * 128)
            b = nxt("xb", 2)
            pb = nxt("tp", 2)
            P.dma("sp", xb[b][:], xsrc[rows, :], writes=[("xb", b)])
            norm_to_hT(P, xb[b][:], ("xb", b), hb[b][:], ("hb", b), ssn[:, b:b + 1], ("ssn", b),
                       ssn[:, 2 + b:3 + b], ("rsn", b), nw_bc[:], "nw", tp[pb][:], ("tp", pb), idn[:],
                       hT[:, :, t * 128:(t + 1) * 128], ("hT", t))

    def load_w(fb):
        wi = nxt("wb", 3)
        P.dma("pool", wb[wi][:].rearrange("p (a c) -> p a c", c=1024),
              D["wf"][fb, :, :].rearrange("p (a c) -> p a c", c=1024), writes=[("wb", wi)])
        return wi, wb[wi][:].rearrange("p (k c) -> p k c", k=KC)

    def mm_block(wi, wv, rhs_fn, n, reads_h):
        mi = nxt("mm", 4)
        for k in range(KC):
            P.op("pe", lambda e, k=k, mi=mi, wv=wv: e.matmul(
                mm[mi][:, 0:n], lhsT=wv[:, k, :], rhs=rhs_fn(k), start=(k == 0), stop=(k == KC - 1)),
                reads=reads_h + [("wb", wi)], writes=[("mm", mi)])
        return mi

    def rope_store(ma, mb, cs, sn, dst, n):
        ti = nxt("t12", 2)
        si = nxt("stg", 4)
        P.op("dve", lambda e: e.tensor_tensor(out=t1[ti][:, 0:n], in0=mm[ma][:, 0:n], in1=cs, op=ALU.mult),
             reads=[("mm", ma), kcos], writes=[("t1", ti)])
        P.op("dve", lambda e: e.tensor_tensor(out=t2[ti][:, 0:n], in0=mm[mb][:, 0:n], in1=sn, op=ALU.mult),
             reads=[("mm", mb), ksin], writes=[("t2", ti)])
        P.op("pool", lambda e: e.tensor_tensor(out=stg[si][:, 0:n], in0=t1[ti][:, 0:n], in1=t2[ti][:, 0:n],
                                               op=ALU.add),
             reads=[("t1", ti), ("t2", ti)], writes=[("stg", si)])
        P.dma("sp", dst, stg[si][:, 0:n], reads=[("stg", si)])

    SQ = S // 2
    SBQ = min(2048, SQ)
    SKIP = os.environ.get('PROJ_SKIP', '')
    for qb in range(0 if 'q' in SKIP else SQ // SBQ):
        q0 = qb * SBQ
        prep_block(D["xq"], D["posq"], q0, SBQ)
        hq_all = [("hT", t) for t in range(SBQ // 128)]
        CHQ = min(512, SBQ)
        for r in range(8):
            wa, wva = load_w(r)
            wb_, wvb = load_w(8 + r)
            for c in range(SBQ // CHQ):
                cs_ = slice(c * CHQ, (c + 1) * CHQ)
                rf = lambda k, cs_=cs_: hT[:, k, cs_]
                ma = mm_block(wa, wva, rf, CHQ, hq_all)
                mb = mm_block(wb_, wvb, rf, CHQ, hq_all)
                rope_store(ma, mb, cosT[:, cs_], sinT[:, cs_], D["QT"][r, :, q0 + c * CHQ:q0 + (c + 1) * CHQ], CHQ)

    for sbi in range(NSB):
        s0 = sbi * SBT
        prep_block(x, pos, s0, SBT)
        hT_all = [("hT", t) for t in range(NTS)]
        for t in range(0 if 't' in SKIP else NTS):
            rows = slice(s0 + t * 128, s0 + (t + 1) * 128)
            mi = nxt("mm", 4)
            for k in range(KC):
                P.op("pe", lambda e, k=k, t=t, mi=mi: e.matmul(
                    mm[mi][:, 0:288], lhsT=hT[:, k, t * 128:(t + 1) * 128], rhs=wtv[:, k, :],
                    start=(k == 0), stop=(k == KC - 1)), reads=[("hT", t), "wt"], writes=[("mm", mi)])
            vi = nxt("vst", 2)
            P.op("act", lambda e, mi=mi, vi=vi: e.activation(out=vst[vi][:, 0:256], in_=mm[mi][:, 0:256],
                                                           func=AF.Copy),
                 reads=[("mm", mi)], writes=[("vstA", vi)])
            P.op("act", lambda e, mi=mi, vi=vi: e.activation(out=vst[vi][:, 256:288], in_=mm[mi][:, 256:288],
                                                           func=AF.Sigmoid),
                 reads=[("mm", mi)], writes=[("vstB", vi)])
            P.dma("sp", D["VG"][rows, :], vst[vi][:], reads=[("vstA", vi), ("vstB", vi)])

        CH = min(512, SBT)
        for (fa, name) in (() if 'k' in SKIP else ((16, "KST"), (18, "KWT"))):
            wa, wva = load_w(fa)
            wb_, wvb = load_w(fa + 1)
            for c in range(SBT // CH):
                cs_ = slice(c * CH, (c + 1) * CH)
                rf = lambda k, cs_=cs_: hT[:, k, cs_]
                ma = mm_block(wa, wva, rf, CH, hT_all)
                mb = mm_block(wb_, wvb, rf, CH, hT_all)
                rope_store(ma, mb, cosT[:, cs_], sinT[:, cs_], D[name][:, s0 + c * CH:s0 + (c + 1) * CH], CH)
        plain = [(20, "KCT", 0), (21, "VCT", 0)] + [(22 + i, "XBCT", i * 128) for i in range(8)] + [(30, "DTT", 0)]
        for (fb, name, r0) in ([] if 'p' in SKIP else plain):
            wa, wva = load_w(fb)
            for c in range(SBT // CH):
                cs_ = slice(c * CH, (c + 1) * CH)
                rf = lambda k, cs_=cs_: hT[:, k, cs_]
                ma = mm_block(wa, wva, rf, CH, hT_all)
                tok = slice(s0 + c * CH, s0 + (c + 1) * CH)
                if name == "DTT":
                    fi = nxt("stf", 2)
                    P.op("act", lambda e, ma=ma, fi=fi: e.activation(out=stf[fi][0:8, 0:CH], in_=mm[ma][0:8, 0:CH],
                                                                   func=AF.Copy),
                         reads=[("mm", ma)], writes=[("stf", fi)])
                    P.dma("sp", D["DTT"][:, tok], stf[fi][0:8, 0:CH], reads=[("stf", fi)])
                else:
                    si = nxt("stg", 4)
                    eng = "act" if (cnt["stg"] % 2) else "dve"
                    if eng == "act":
                        P.op("act", lambda e, ma=ma, si=si: e.activation(out=stg[si][:, 0:CH], in_=mm[ma][:, 0:CH],
                                                                       func=AF.Copy),
                             reads=[("mm", ma)], writes=[("stg", si)])
                    else:
                        P.op("dve", lambda e, ma=ma, si=si: e.tensor_copy(out=stg[si][:, 0:CH], in_=mm[ma][:, 0:CH]),
                             reads=[("mm", ma)], writes=[("stg", si)])
                    P.dma("sp", D[name][r0:r0 + 128, tok], stg[si][:, 0:CH], reads=[("stg", si)])


def build_proj(S):
    nc = bass.Bass("TRN2", target_bir_lowering=False)
    D = proj_decl(nc, S, "ExternalOutput")
    with ExitStack() as es:
        P = Prog(nc, es)
        emit_proj(P, S, D)
        P.finish()
    return nc, D


def core_coords(c):
    return c // 4, (c % 4) // 2, c % 2, c % 4


def _swap(a):
    return np.concatenate([a[64:], a[:64]])


def proj_weight_layout(w, g, j):
    cols = []
    for r in range(8):
        h = g * 8 + r
        cols.append(np.arange(h * 128, (h + 1) * 128))
    for r in range(8):
        h = g * 8 + r
        cols.append(_swap(np.arange(h * 128, (h + 1) * 128)))
    kv = lambda off: np.arange(off + g * 128, off + (g + 1) * 128)
    cols += [kv(OFF_KS), _swap(kv(OFF_KS)), kv(OFF_KW), _swap(kv(OFF_KW)), kv(OFF_KC), kv(OFF_VC)]
    for i in range(4):
        cols.append(np.arange(OFF_X + 512 * j + i * 128, OFF_X + 512 * j + (i + 1) * 128))
    for i in range(2):
        cols.append(np.arange(OFF_B + 256 * j + i * 128, OFF_B + 256 * j + (i + 1) * 128))
    for i in range(2):
        cols.append(np.arange(OFF_C + 256 * j + i * 128, OFF_C + 256 * j + (i + 1) * 128))
    wf = np.zeros((NFB, 128, KC, 128), np.float32)
    for fb, cc in enumerate(cols):
        wf[fb] = w[:, cc].reshape(KC, 128, 128).transpose(1, 0, 2)
    wf[30, :, :, 0:8] = w[:, OFF_DT + 8 * j:OFF_DT + 8 * j + 8].reshape(KC, 128, 8).transpose(1, 0, 2)
    tc = np.concatenate([kv(OFF_VS), kv(OFF_VW), np.arange(OFF_GATE + g * 24, OFF_GATE + (g + 1) * 24)])
    wt = np.zeros((128, KC, 288), np.float32)
    wt[:, :, 0:280] = w[:, tc].reshape(KC, 128, 280).transpose(1, 0, 2)
    return wf.reshape(NFB, 128, KC * 128), wt.reshape(128, KC * 288)


def rope_consts():
    half = 64
    inv = (10000.0 ** (-np.arange(half, dtype=np.float32) / half)).astype(np.float32)
    c = np.zeros((128, 2), np.float32)
    c[:, 0] = np.concatenate([inv, inv])
    c[:64, 1] = -1.0
    c[64:, 1] = 1.0
    return c


def my_tiles(a, par):
    s = a.shape[0]
    v = a.reshape((s // 256, 2, 128) + a.shape[1:])
    return np.ascontiguousarray(v[:, par].reshape((s // 2,) + a.shape[1:]))


IDENT_BF = np.eye(128, dtype=np.float32).astype(NPBF)


def ssd_decl(nc, S, kind_in, kind_out, D=None):
    dt = nc.dram_tensor
    D = {} if D is None else D
    if "XBCT" not in D:
        D["XBCT"] = dt("XBCT", [1024, S], BF16, kind=kind_in).ap()
        D["DTT"] = dt("DTT", [8, S], F32, kind=kind_in).ap()
    D["convp"] = dt("convp", [128, 8, 5], F32, kind="ExternalInput").ap()
    D["ssmp"] = dt("ssmp", [8, 2], F32, kind="ExternalInput").ap()
    D["dskip"] = dt("dskip", [8], F32, kind="ExternalInput").ap()
    D["negm"] = dt("negm", [128, 128], F32, kind="ExternalInput").ap()
    D["onehot"] = dt("onehot", [8, 1024], F32, kind="ExternalInput").ap()
    D["id8"] = dt("id8", [8, 8], F32, kind="ExternalInput").ap()
    if "ident" not in D:
        D["ident"] = dt("ident", [128, 128], BF16, kind="ExternalInput").ap()
    D["YS"] = dt("YS", [S, 512], BF16, kind=kind_out).ap()
    return D


def emit_ssd(P, S, D, pfx="s"):
    SC = min(2048, S)
    NSC = S // SC
    NCH = SC // 128
    K = lambda *a: (pfx,) + a
    idn = P.sb([128, 128], BF16)
    id8 = P.sb([8, 8], F32)
    convp = P.sb([128, 8, 5], F32)
    ssmp = P.sb([8, 2], F32)
    negm = P.sb([128, 128], F32)
    onehot = P.sb([8, 1024], F32)
    dsk = P.sb([128, 8], F32)
    P.dma("sp", idn[:], D["ident"][:, :], writes=[K("idn")])
    P.dma("sp", id8[:], D["id8"][:, :], writes=[K("id8")])
    P.dma("sp", convp[:], D["convp"][:, :, :], writes=[K("convp")])
    P.dma("sp", ssmp[:], D["ssmp"][:, :], writes=[K("ssmp")])
    P.dma("sp", negm[:], D["negm"][:, :], writes=[K("negm")])
    P.dma("sp", onehot[:], D["onehot"][:, :], writes=[K("onehot")])
    P.dma("sp", dsk[:], D["dskip"].partition_broadcast(128), writes=[K("dsk")])
    aneg = P.sb([8, 1], F32)
    P.op("act", lambda e: e.activation(out=aneg[:], in_=ssmp[:, 1:2], func=AF.Exp), reads=[K("ssmp")],
         writes=[K("aneg")])
    P.op("dve", lambda e: e.tensor_scalar(out=aneg[:], in0=aneg[:], scalar1=-1.0, scalar2=None, op0=ALU.mult),
         reads=[K("aneg")], writes=[K("aneg")])
    raw = P.sb([128, 8, SC + 4], BF16)
    acc = [P.sb([128, SC], F32) for _ in range(2)]
    cv = P.sb([128, 8, SC], BF16)
    dtf = P.sb([8, SC], F32)
    daf = P.sb([8, SC], F32)
    cum = P.sb([8, SC], F32)
    ones8 = P.sb([8, 128], F32)
    P.op("pool", lambda e: e.memset(ones8[:], 1.0), writes=[K("ones8")])
    P.op("pool", lambda e: e.memset(raw[:, :, 0:4], 0.0), writes=[K("raw")])
    state = [P.sb([128, 64], F32) for _ in range(8)]
    stbf = [P.sb([128, 64], BF16) for _ in range(8)]
    for h in range(8):
        P.op("pool", lambda e, h=h: e.memset(state[h][:], 0.0), writes=[K("state", h)])
        P.op("pool", lambda e, h=h: e.memset(stbf[h][:], 0.0), writes=[K("stbf", h)])
    xb_tok = [P.sb([128, 768], BF16) for _ in range(2)]
    dtc = [P.sb([128, 24], F32) for _ in range(2)]
    lastbc = [P.sb([128, 8], F32) for _ in range(2)]
    elast = [P.sb([128, 8], F32) for _ in range(2)]
    eend = [P.sb([128, 8], F32) for _ in range(2)]
    ecum = [P.sb([128, 8], F32) for _ in range(2)]
    tmpd = [P.sb([128, 128], F32) for _ in range(2)]
    dec = [P.sb([128, 128], F32) for _ in range(2)]
    WT = [P.sb([128, 128], BF16) for _ in range(2)]
    xdt = [P.sb([128, 64], BF16) for _ in range(2)]
    xend = [P.sb([128, 64], BF16) for _ in range(2)]
    t1 = [P.sb([128, 64], F32) for _ in range(2)]
    yt = [P.sb([128, 512], BF16) for _ in range(2)]
    pA = [P.ps([128, 128], F32) for _ in range(2)]
    pCB = P.ps([128, 256], F32)
    pC = [P.ps([128, 128], F32) for _ in range(2)]
    pD = P.ps([128, 64], F32)
    pT = P.ps([128, 768], BF16)
    pT2 = P.ps([128, 16], F32)
    cnt = {}

    def nxt(k, n=2):
        v = cnt.get(k, 0)
        cnt[k] = v + 1
        return v % n

    for sc in range(NSC):
        s0 = sc * SC
        if sc > 0:
            P.op("pool", lambda e: e.tensor_copy(out=raw[:, :, 1:4], in_=raw[:, :, SC + 1:SC + 4]),
                 reads=[K("raw")], writes=[K("raw")])
        P.dma("sp", raw[:, :, 4:SC + 4], D["XBCT"][:, s0:s0 + SC].rearrange("(b p) t -> p b t", p=128),
              writes=[K("raw")])
        for b in range(8):
            ai = nxt("acc")
            P.op("dve", lambda e, b=b, ai=ai: e.tensor_scalar(
                out=acc[ai][:], in0=raw[:, b, 1:SC + 1], scalar1=convp[:, b, 0:1], scalar2=convp[:, b, 4:5],
                op0=ALU.mult, op1=ALU.add), reads=[K("raw"), K("convp")], writes=[K("acc", ai)])
            for k in range(1, 4):
                P.op("dve", lambda e, b=b, ai=ai, k=k: e.scalar_tensor_tensor(
                    out=acc[ai][:], in0=raw[:, b, 1 + k:SC + 1 + k], scalar=convp[:, b, k:k + 1], in1=acc[ai][:],
                    op0=ALU.mult, op1=ALU.add), reads=[K("raw"), K("convp"), K("acc", ai)], writes=[K("acc", ai)])
            P.op("act", lambda e, b=b, ai=ai: e.activation(out=cv[:, b, :], in_=acc[ai][:], func=AF.Silu),
                 reads=[K("acc", ai)], writes=[K("cv", b)])
        P.dma("sp", dtf[:], D["DTT"][:, s0:s0 + SC], writes=[K("dtf")])
        P.op("act", lambda e: e.activation(out=dtf[:], in_=dtf[:], func=AF.Exp, bias=ssmp[:, 0:1]),
             reads=[K("dtf"), K("ssmp")], writes=[K("dtf")])
        P.op("act", lambda e: e.activation(out=dtf[:], in_=dtf[:], func=AF.Ln, bias=1.0),
             reads=[K("dtf")], writes=[K("dtf")])
        P.op("dve", lambda e: e.tensor_scalar(out=daf[:], in0=dtf[:], scalar1=aneg[:, 0:1], scalar2=None,
                                              op0=ALU.mult), reads=[K("dtf"), K("aneg")], writes=[K("daf")])
        for c in range(NCH):
            cs = slice(c * 128, (c + 1) * 128)
            P.op("dve", lambda e, cs=cs: e.tensor_tensor_scan(out=cum[:, cs], data0=ones8[:], data1=daf[:, cs],
                                                              initial=0.0, op0=ALU.mult, op1=ALU.add),
                 reads=[K("daf"), K("ones8")], writes=[K("cum")])
        cv_all = [K("cv", b) for b in range(8)]
        for c in range(NCH):
            cs = slice(c * 128, (c + 1) * 128)
            rows = slice(s0 + c * 128, s0 + (c + 1) * 128)
            for b in range(6):
                P.op("pe", lambda e, b=b, cs=cs: e.transpose(out=pT[:, b * 128:(b + 1) * 128], in_=cv[:, b, cs],
                                                            identity=idn[:]),
                     reads=[K("cv", b), K("idn")], writes=[K("pT")])
            xi = nxt("xb")
            P.op("act", lambda e, xi=xi: e.activation(out=xb_tok[xi][:], in_=pT[:], func=AF.Copy),
                 reads=[K("pT")], writes=[K("xb", xi)])
            P.op("pe", lambda e, cs=cs: e.transpose(out=pT2[:, 0:8], in_=dtf[:, cs], identity=id8[:]),
                 reads=[K("dtf"), K("id8")], writes=[K("pT2")])
            P.op("pe", lambda e, cs=cs: e.transpose(out=pT2[:, 8:16], in_=cum[:, cs], identity=id8[:]),
                 reads=[K("cum"), K("id8")], writes=[K("pT2")])
            di = nxt("dtc")
            P.op("dve", lambda e, di=di: e.tensor_copy(out=dtc[di][:, 0:16], in_=pT2[:]),
                 reads=[K("pT2")], writes=[K("dtc", di)])
            P.op("dve", lambda e, di=di: e.tensor_scalar(out=dtc[di][:, 16:24], in0=dtc[di][:, 8:16], scalar1=-1.0,
                                                        scalar2=None, op0=ALU.mult),
                 reads=[K("dtc", di)], writes=[K("dtc", di)])
            P.op("act", lambda e, di=di: e.activation(out=ecum[di][:], in_=dtc[di][:, 8:16], func=AF.Exp),
                 reads=[K("dtc", di)], writes=[K("ecum", di)])
            for g in range(2):
                P.op("pe", lambda e, g=g, cs=cs: e.matmul(pCB[:, g * 128:(g + 1) * 128], lhsT=cv[:, 4 + g, cs],
                                                         rhs=cv[:, 6 + g, cs], start=True, stop=True),
                     reads=[K("cv", 4 + g), K("cv", 6 + g)], writes=[K("pCB", g)])
            yi = nxt("yt")
            for h in range(8):
                g = h // 4
                ai = nxt("pA")
                P.op("pe", lambda e, h=h, ai=ai, cs=cs: e.matmul(pA[ai][:], lhsT=onehot[:, h * 128:(h + 1) * 128],
                                                               rhs=cum[:, cs], start=True, stop=True),
                     reads=[K("onehot"), K("cum")], writes=[K("pA", ai)])
                ti = nxt("tmpd")
                P.op("dve", lambda e, ai=ai, ti=ti: e.tensor_tensor(out=tmpd[ti][:], in0=pA[ai][:], in1=negm[:],
                                                                    op=ALU.add),
                     reads=[K("pA", ai), K("negm")], writes=[K("tmpd", ti)])
                P.op("dve", lambda e, ai=ai, di=di, h=h: e.tensor_copy(out=lastbc[di][:, h:h + 1],
                                                                      in_=pA[ai][:, 127:128]),
                     reads=[K("pA", ai)], writes=[K("lastbc", di, h)])
                P.op("act", lambda e, ti=ti, di=di, h=h: e.activation(out=dec[ti][:], in_=tmpd[ti][:], func=AF.Exp,
                                                                     bias=dtc[di][:, 16 + h:17 + h]),
                     reads=[K("tmpd", ti), K("dtc", di)], writes=[K("dec", ti)])
                P.op("dve", lambda e, ti=ti, g=g: e.tensor_tensor(out=WT[ti][:], in0=dec[ti][:],
                                                                 in1=pCB[:, g * 128:(g + 1) * 128], op=ALU.mult),
                     reads=[K("dec", ti), K("pCB", g)], writes=[K("WT", ti)])
                xi2 = nxt("xdt")
                P.op("dve", lambda e, xi=xi, xi2=xi2, di=di, h=h: e.tensor_scalar(
                    out=xdt[xi2][:], in0=xb_tok[xi][:, h * 64:(h + 1) * 64], scalar1=dtc[di][:, h:h + 1],
                    scalar2=None, op0=ALU.mult), reads=[K("xb", xi), K("dtc", di)], writes=[K("xdt", xi2)])
                ci = nxt("pC")
                P.op("pe", lambda e, ti=ti, xi2=xi2, ci=ci: e.matmul(pC[ci][:, 0:64], lhsT=WT[ti][:], rhs=xdt[xi2][:],
                                                                    start=True, stop=True),
                     reads=[K("WT", ti), K("xdt", xi2)], writes=[K("pC", ci)])
                P.op("pe", lambda e, g=g, h=h, ci=ci, cs=cs: e.matmul(pC[ci][:, 64:128], lhsT=cv[:, 6 + g, cs],
                                                                     rhs=stbf[h][:], start=True, stop=True),
                     reads=[K("cv", 6 + g), K("stbf", h)], writes=[K("pC", ci)])
                P.op("dve", lambda e, ci=ci, ti=ti, di=di, h=h: e.scalar_tensor_tensor(
                    out=t1[ti][:], in0=pC[ci][:, 64:128], scalar=ecum[di][:, h:h + 1], in1=pC[ci][:, 0:64],
                    op0=ALU.mult, op1=ALU.add) if False else e.tensor_scalar(
                    out=t1[ti][:], in0=pC[ci][:, 64:128], scalar1=ecum[di][:, h:h + 1], scalar2=None, op0=ALU.mult),
                    reads=[K("pC", ci), K("ecum", di)], writes=[K("t1", ti)])
                P.op("dve", lambda e, ci=ci, ti=ti: e.tensor_tensor(out=t1[ti][:], in0=t1[ti][:], in1=pC[ci][:, 0:64],
                                                                    op=ALU.add),
                     reads=[K("pC", ci), K("t1", ti)], writes=[K("t1", ti)])
                P.op("dve", lambda e, xi=xi, ti=ti, yi=yi, h=h: e.scalar_tensor_tensor(
                    out=yt[yi][:, h * 64:(h + 1) * 64], in0=xb_tok[xi][:, h * 64:(h + 1) * 64],
                    scalar=dsk[:, h:h + 1], in1=t1[ti][:], op0=ALU.mult, op1=ALU.add),
                    reads=[K("xb", xi), K("dsk"), K("t1", ti)], writes=[K("yt", yi)])
                P.op("act", lambda e, di=di, h=h: e.activation(out=eend[di][:, h:h + 1], in_=dtc[di][:, 16 + h:17 + h],
                                                              func=AF.Exp, bias=lastbc[di][:, h:h + 1]),
                     reads=[K("dtc", di), K("lastbc", di, h)], writes=[K("eend", di, h)])
                P.op("act", lambda e, di=di, h=h: e.activation(out=elast[di][:, h:h + 1], in_=lastbc[di][:, h:h + 1],
                                                              func=AF.Exp),
                     reads=[K("lastbc", di, h)], writes=[K("elast", di, h)])
                P.op("dve", lambda e, xi2=xi2, di=di, h=h: e.tensor_scalar(
                    out=xend[xi2][:], in0=xdt[xi2][:], scalar1=eend[di][:, h:h + 1], scalar2=None, op0=ALU.mult),
                    reads=[K("xdt", xi2), K("eend", di, h)], writes=[K("xend", xi2)])
                P.op("pe", lambda e, xi=xi, xi2=xi2, g=g: e.matmul(pD[:], lhsT=xb_tok[xi][:, 512 + g * 128:640 + g * 128],
                                                                  rhs=xend[xi2][:], start=True, stop=True),
                     reads=[K("xb", xi), K("xend", xi2)], writes=[K("pD")])
                P.op("dve", lambda e, di=di, h=h: e.scalar_tensor_tensor(
                    out=state[h][:], in0=state[h][:], scalar=elast[di][:, h:h + 1], in1=pD[:],
                    op0=ALU.mult, op1=ALU.add), reads=[K("state", h), K("elast", di, h), K("pD")],
                    writes=[K("state", h)])
                P.op("pool", lambda e, h=h: e.tensor_copy(out=stbf[h][:], in_=state[h][:]),
                     reads=[K("state", h)], writes=[K("stbf", h)])
            P.dma("sp", D["YS"][rows, :], yt[yi][:], reads=[K("yt", yi)])


def build_ssd(S):
    nc = bass.Bass("TRN2", target_bir_lowering=False)
    D = ssd_decl(nc, S, "ExternalInput", "ExternalOutput")
    with ExitStack() as es:
        P = Prog(nc, es)
        emit_ssd(P, S, D)
        P.finish()
    return nc, D


def ssd_consts():
    l = np.arange(128)
    negm = np.where(l[None, :] >= l[:, None], 0.0, -1e30).astype(np.float32)
    onehot = np.zeros((8, 1024), np.float32)
    for h in range(8):
        onehot[h, h * 128:(h + 1) * 128] = 1.0
    return dict(negm=negm, onehot=onehot, id8=np.eye(8, dtype=np.float32))


def ssd_param_layout(conv_w, conv_b, dt_bias, a_log, d_skip, j):
    ch = np.concatenate([np.arange(512 * j, 512 * (j + 1)), 2048 + np.arange(256 * j, 256 * (j + 1)),
                         3072 + np.arange(256 * j, 256 * (j + 1))])
    cp = np.zeros((128, 8, 5), np.float32)
    cp[:, :, 0:4] = conv_w[:, ch].T.reshape(8, 128, 4).transpose(1, 0, 2)
    cp[:, :, 4] = conv_b[ch].reshape(8, 128).T
    hs = slice(8 * j, 8 * j + 8)
    ssmp = np.stack([dt_bias[hs], a_log[hs]], 1).astype(np.float32)
    return dict(convp=cp, ssmp=np.ascontiguousarray(ssmp), dskip=np.ascontiguousarray(d_skip[hs]))


def build_cmp(S):
    NCP = S // 16
    NC = NCP - 1
    nc = bass.Bass("TRN2", target_bir_lowering=False)
    dt = nc.dram_tensor
    KCT = dt("KCT", [128, S], BF16, kind="ExternalInput").ap()
    VCT = dt("VCT", [128, S], BF16, kind="ExternalInput").ap()
    w1 = {n: dt("w1" + n, [32, 128, 256], F32, kind="ExternalInput").ap() for n in "kv"}
    w2 = {n: dt("w2" + n, [256, 128], F32, kind="ExternalInput").ap() for n in "kv"}
    w2s = dt("w2ks", [256, 128], F32, kind="ExternalInput").ap()
    peT = {n: dt("peT" + n, [128, 32], F32, kind="ExternalInput").ap() for n in "kv"}
    posc = dt("posc", [NCP], I32, kind="ExternalInput").ap()
    invf_d = dt("invf", [128, 2], F32, kind="ExternalInput").ap()
    KCMPT = dt("KCMPT", [128, NCP], BF16, kind="ExternalOutput").ap()
    VCMP = dt("VCMP", [NCP, 128], BF16, kind="ExternalOutput").ap()
    with ExitStack() as es:
        P = Prog(nc, es)
        invf = P.sb([128, 2], F32)
        P.dma("sp", invf[:], invf_d[:, :], writes=["invf"])
        posi = P.sb([128, NCP], I32)
        posf = P.sb([128, NCP], F32)
        ang = P.sb([128, NCP], F32)
        kf = P.sb([128, NCP], F32)
        tmp = P.sb([128, NCP], F32)
        cosT = P.sb([128, NCP], F32)
        sinT = P.sb([128, NCP], F32)
        P.dma("sp", posi[:], posc.partition_broadcast(128), writes=["posi"])
        P.op("dve", lambda e: e.tensor_copy(out=posf[:], in_=posi[:]), reads=["posi"], writes=["posf"])
        rope_tables(P, posf[:], "posf", invf, ang[:], kf[:], tmp[:], cosT[:], sinT[:], NCP, "rp")
        raw = {n: P.sb([128, S], BF16) for n in "kv"}
        P.dma("sp", raw["k"][:], KCT[:, :], writes=[("raw", "k")])
        P.dma("sp", raw["v"][:], VCT[:, :], writes=[("raw", "v")])
        w1s = {n: P.sb([128, 32, 256], BF16) for n in "kv"}
        w2sb = {n: P.sb([128, 2, 128], BF16) for n in ("k", "v", "ks")}
        pes = {n: P.sb([128, 32], BF16) for n in "kv"}
        for n in "kv":
            for half in range(2):
                P.dma("pool", w1s[n][:, half * 16:(half + 1) * 16, :],
                      w1[n][half * 16:(half + 1) * 16].rearrange("p d h -> d p h"), writes=[("w1", n)])
            P.dma("pool", w2sb[n][:], w2[n].rearrange("(c p) d -> p c d", p=128), writes=[("w2", n)])
            P.dma("pool", pes[n][:], peT[n][:, :], writes=[("pe", n)])
        P.dma("pool", w2sb["ks"][:], w2s.rearrange("(c p) d -> p c d", p=128), writes=[("w2", "ks")])
        sil = {n: P.sb([128, 2, NCP], BF16) for n in "kv"}
        bias = P.sb([128, 4], F32)
        pb = P.ps([128, 4], F32)
        ph = [P.ps([128, 512], F32) for _ in range(2)]
        pk = [P.ps([128, 512], F32) for _ in range(2)]
        t1 = P.sb([128, 512], F32)
        t2 = P.sb([128, 512], F32)
        ko = P.sb([128, NCP], BF16)
        vo = P.sb([128, 128], BF16)
        P.op("pool", lambda e: e.memset(ko[:], 0.0), writes=["ko"])
        for n in "kv":
            P.op("pool", lambda e, n=n: e.memset(sil[n][:], 0.0), writes=[("sil", n)])
        halves = [(a, min(a + 512, NC)) for a in range(0, NC, 512)]
        cnt = [0]
        for ni, n in enumerate("kv"):
            for hh in range(2):
                col = ni * 2 + hh
                for p in range(32):
                    P.op("pe", lambda e, n=n, hh=hh, p=p, col=col: e.matmul(
                        pb[:, col:col + 1], lhsT=w1s[n][:, p, hh * 128:(hh + 1) * 128], rhs=pes[n][:, p:p + 1],
                        start=(p == 0), stop=(p == 31)), reads=[("w1", n), ("pe", n)], writes=["pb"])
                P.op("dve", lambda e, col=col: e.tensor_copy(out=bias[:, col:col + 1], in_=pb[:, col:col + 1]),
                     reads=["pb"], writes=[("bias", col)])
                for (a, b) in halves:
                    pi = cnt[0] % 2
                    cnt[0] += 1
                    nn = b - a
                    for p in range(32):
                        P.op("pe", lambda e, n=n, hh=hh, p=p, a=a, nn=nn, pi=pi: e.matmul(
                            ph[pi][:, 0:nn], lhsT=w1s[n][:, p, hh * 128:(hh + 1) * 128],
                            rhs=raw[n][:, 16 * a + p:16 * a + p + 16 * (nn - 1) + 1:16],
                            start=(p == 0), stop=(p == 31)), reads=[("w1", n), ("raw", n)], writes=[("ph", pi)])
                    P.op("act", lambda e, n=n, hh=hh, a=a, b=b, nn=nn, pi=pi, col=col: e.activation(
                        out=sil[n][:, hh, a:b], in_=ph[pi][:, 0:nn], func=AF.Silu, bias=bias[:, col:col + 1]),
                        reads=[("ph", pi), ("bias", col)], writes=[("sil", n)])
        for (a, b) in halves:
            nn = b - a
            for vi, wn in enumerate(("k", "ks")):
                for hh in range(2):
                    P.op("pe", lambda e, wn=wn, hh=hh, a=a, b=b, nn=nn, vi=vi: e.matmul(
                        pk[vi][:, 0:nn], lhsT=w2sb[wn][:, hh, :], rhs=sil["k"][:, hh, a:b],
                        start=(hh == 0), stop=(hh == 1)), reads=[("w2", wn), ("sil", "k")], writes=[("pk", vi)])
            P.op("dve", lambda e, a=a, b=b, nn=nn: e.tensor_tensor(out=t1[:, 0:nn], in0=pk[0][:, 0:nn],
                                                                   in1=cosT[:, a:b], op=ALU.mult),
                 reads=[("pk", 0), ("rp", "cos")], writes=["t1"])
            P.op("dve", lambda e, a=a, b=b, nn=nn: e.tensor_tensor(out=t2[:, 0:nn], in0=pk[1][:, 0:nn],
                                                                   in1=sinT[:, a:b], op=ALU.mult),
                 reads=[("pk", 1), ("rp", "sin")], writes=["t2"])
            P.op("dve", lambda e, a=a, b=b, nn=nn: e.tensor_tensor(out=ko[:, a:b], in0=t1[:, 0:nn], in1=t2[:, 0:nn],
                                                                   op=ALU.add),
                 reads=["t1", "t2"], writes=["ko"])
        P.dma("sp", KCMPT[:, :], ko[:], reads=["ko"])
        for ct in range(NCP // 128):
            for hh in range(2):
                P.op("pe", lambda e, hh=hh, ct=ct: e.matmul(
                    pk[0][:, 0:128], lhsT=sil["v"][:, hh, ct * 128:(ct + 1) * 128], rhs=w2sb["v"][:, hh, :],
                    start=(hh == 0), stop=(hh == 1)), reads=[("w2", "v"), ("sil", "v")], writes=[("pk", 0)])
            P.op("dve", lambda e: e.tensor_copy(out=vo[:], in_=pk[0][:, 0:128]), reads=[("pk", 0)], writes=["vo"])
            P.dma("sp", VCMP[ct * 128:(ct + 1) * 128, :], vo[:], reads=["vo"])
        P.finish()
    return nc


SCALE = 128 ** -0.5
BIGV = 1.0e4


def attn_consts(par):
    p = np.arange(128)[:, None]
    q = np.arange(128)[None, :]
    mc = np.zeros((128, 8, 128), np.float32)
    for r in range(8):
        mc[:, r, :] = (16 * (p - 16 * r - 8 * par) + 31 <= q)
    mcp = np.ones((128, 128), np.float32)
    mcp[127, :] = (15 - 128 * par <= np.arange(128))
    A = np.zeros((128, 8, 256), np.float32)
    blk = np.arange(256)[None, :]
    for jc in range(8):
        c = 128 * jc + p
        A[:, jc, :] = (4 * blk - 1 <= c) & (c <= 4 * blk + 3)
    qq = np.arange(128)[:, None]
    d = np.arange(512)[None, :] - 256
    curd = 2 * par + (qq >= 64)
    bc = np.where((d == curd) | (d == curd - 1), BIGV, np.where(d > curd, -BIGV, 0.0)).astype(np.float32)
    tri = (p <= q).astype(np.float32)
    gtr = (p > q).astype(np.float32)
    one = np.ones((128, 128), np.float32)
    zero = np.zeros((128, 128), np.float32)
    cms = np.stack([tri, zero] if par == 0 else [one, tri], 1)
    cmw = np.stack([gtr, one, one, one, tri, zero] if par == 0 else [zero, gtr, one, one, one, tri], 1)
    y = np.arange(8192)[None, :]
    G = (y // 64 == np.arange(128)[:, None]).astype(np.float32)
    f = lambda a: np.ascontiguousarray(a).astype(NPBF)
    return dict(mc=f(mc), mcp=f(mcp), amat=f(A), bcst=bc, cms=f(cms), cmw=f(cmw), gexp=f(G),
                zeros=np.zeros((128, 512), NPBF))


def build_attn(S):
    SQ = S // 2
    NQ = SQ // 128
    NKT = S // 128
    NCP = S // 16
    NCT = max(1, NCP // 128)
    nc = bass.Bass("TRN2", target_bir_lowering=False)
    dt = nc.dram_tensor
    inp = lambda n, sh, d: dt(n, sh, d, kind="ExternalInput").ap()
    QT = inp("QT", [8, 128, SQ], BF16)
    KST = inp("KST", [128, S], BF16)
    KWT = inp("KWT", [128, S], BF16)
    VG = inp("VG", [S, 288], BF16)
    GQ = inp("GQ", [SQ, 32], BF16)
    KCMPT = inp("KCMPT", [128, NCP], BF16)
    VCMP = inp("VCMP", [NCP, 128], BF16)
    mc_d = inp("mc", [128, 8, 128], BF16)
    mcp_d = inp("mcp", [128, 128], BF16)
    a_d = inp("amat", [128, 8, 256], BF16)
    bc_d = inp("bcst", [128, 512], F32)
    cms_d = inp("cms", [128, 2, 128], BF16)
    cmw_d = inp("cmw", [128, 6, 128], BF16)
    g_d = inp("gexp", [128, 8192], BF16)
    z_d = inp("zeros", [128, 512], BF16)
    id_d = inp("ident", [128, 128], BF16)
    OA = dt("OA", [SQ, 1024], BF16, kind="ExternalOutput").ap()
    with ExitStack() as es:
        P = Prog(nc, es)
        kst = P.sb([128, S], BF16)
        kwt = P.sb([128, S], BF16)
        vs1 = P.sb([128, NKT, 129], BF16)
        vw1 = P.sb([128, NKT, 129], BF16)
        kcm = P.sb([128, NCP], BF16)
        vc1 = P.sb([128, NCT, 129], BF16)
        mc = P.sb([128, 8, 128], BF16)
        mcp = P.sb([128, 128], BF16)
        amat = P.sb([128, 8, 256], BF16)
        bcst = P.sb([128, 512], F32)
        cms = P.sb([128, 2, 128], BF16)
        cmw = P.sb([128, 6, 128], BF16)
        gexp = P.sb([128, 8192], BF16)
        zeros = P.sb([128, 512], BF16)
        idn = P.sb([128, 128], BF16)
        for t_, d_, k_ in ((kst, KST, "kst"), (kwt, KWT, "kwt"), (kcm, KCMPT, "kcm"), (mcp, mcp_d, "mcp"),
                           (bcst, bc_d, "bcst"), (gexp, g_d, "gexp"), (zeros, z_d, "zeros"), (idn, id_d, "idn")):
            P.dma("sp", t_[:], d_[:, :], writes=[k_])
        for t_, d_, k_ in ((mc, mc_d, "mc"), (amat, a_d, "amat"), (cms, cms_d, "cms"), (cmw, cmw_d, "cmw")):
            P.dma("sp", t_[:], d_[:, :, :], writes=[k_])
        for t_, k_ in ((vs1, "vs1"), (vw1, "vw1"), (vc1, "vc1")):
            P.op("pool", lambda e, t_=t_: e.memset(t_[:], 1.0), writes=[k_])
        P.dma("sp", vs1[:, :, 0:128], VG[:, 0:128].rearrange("(t p) d -> p t d", p=128), writes=["vs1"])
        P.dma("sp", vw1[:, :, 0:128], VG[:, 128:256].rearrange("(t p) d -> p t d", p=128), writes=["vw1"])
        if NCP >= 128:
            P.dma("sp", vc1[:, :, 0:128], VCMP.rearrange("(t p) d -> p t d", p=128), writes=["vc1"])
        qT = [P.sb([128, 8, 128], BF16) for _ in range(2)]
        gq = [P.sb([128, 32], BF16) for _ in range(2)]
        et = P.sb([128, NCT, 1024], BF16)
        pt = [P.sb([128, 1024], BF16) for _ in range(2)]
        mk = [P.sb([128, 128], BF16) for _ in range(2)]
        imp = P.sb([128, 256], F32)
        sc2 = P.sb([128, 256], F32)
        m8 = P.sb([128, 16], F32)
        sel = P.sb([128, 256], BF16)
        selT = P.sb([128, 2, 128], BF16)
        zall = P.sb([128, 8], F32)
        coef = P.sb([128, 8], F32)
        oacc = P.sb([128, 1024], F32)
        obf = [P.sb([128, 1024], BF16) for _ in range(2)]
        ST = [P.ps([128, 1024], F32) for _ in range(2)]
        OB = [P.ps([128, 512], F32) for _ in range(3)]
        MS = P.ps([128, 512], F32)
        cnt = {}

        def nxt(k, n=2):
            v = cnt.get(k, 0)
            cnt[k] = v + 1
            return v % n

        def oreg(h):
            return OB[h // 3][:, (h % 3) * 129:(h % 3) * 129 + 129]

        def zero_acc():
            for b in range(3):
                P.op("pe", lambda e, b=b: e.matmul(OB[b][:], lhsT=zeros[:, 0:128], rhs=zeros[:], start=True,
                                                  stop=False, skip_group_check=True),
                     reads=["zeros"], writes=[("OB", b)])

        def scores(ktile_ap, qi, kkey):
            si = nxt("ST")
            for hf in range(2):
                P.op("pe", lambda e, hf=hf, si=si: e.matmul(
                    ST[si][:, hf * 512:(hf + 1) * 512], lhsT=ktile_ap, rhs=qT[qi][:, hf * 4:(hf + 1) * 4, :],
                    start=True, stop=True), reads=[kkey, ("qT", qi)], writes=[("ST", si)])
            return si

        def pv(p_ap_fn, pkey, v_ap, vkey, last):
            for h in range(8):
                P.op("pe", lambda e, h=h: e.matmul(oreg(h), lhsT=p_ap_fn(h), rhs=v_ap, start=False, stop=last,
                                                   skip_group_check=True),
                     reads=[pkey, vkey], writes=[("OB", h // 3)])

        def combine(br, qi, first):
            for b in range(3):
                nh = 3 if b < 2 else 2
                P.op("dve", lambda e, b=b, nh=nh: e.tensor_copy(
                    out=zall[:, b * 3:b * 3 + nh],
                    in_=OB[b][:, 0:nh * 129].rearrange("p (h c) -> p h c", c=129)[:, :, 128]),
                    reads=[("OB", b)], writes=["zall"])
            P.op("dve", lambda e: e.tensor_scalar(out=zall[:], in0=zall[:], scalar1=1e-30, scalar2=None,
                                                  op0=ALU.max), reads=["zall"], writes=["zall"])
            P.op("dve", lambda e: e.reciprocal(out=zall[:], in_=zall[:]), reads=["zall"], writes=["zall"])
            P.op("dve", lambda e: e.tensor_tensor(
                out=coef[:], in0=zall[:], in1=gq[qi][:, 0:24].rearrange("p (h c) -> p h c", c=3)[:, :, br],
                op=ALU.mult), reads=["zall", ("gq", qi)], writes=["coef"])
            for h in range(8):
                osl = oacc[:, h * 128:(h + 1) * 128]
                if first:
                    P.op("dve", lambda e, h=h, osl=osl: e.tensor_scalar(
                        out=osl, in0=oreg(h)[:, 0:128], scalar1=coef[:, h:h + 1], scalar2=None, op0=ALU.mult),
                        reads=[("OB", h // 3), "coef"], writes=["oacc"])
                else:
                    P.op("dve", lambda e, h=h, osl=osl: e.scalar_tensor_tensor(
                        out=osl, in0=oreg(h)[:, 0:128], scalar=coef[:, h:h + 1], in1=osl, op0=ALU.mult,
                        op1=ALU.add), reads=[("OB", h // 3), "coef", "oacc"], writes=["oacc"])

        def bmul(buf_ap, m_ap):
            return lambda e: e.tensor_tensor(out=buf_ap.rearrange("p (h q) -> p h q", h=8),
                                             in0=buf_ap.rearrange("p (h q) -> p h q", h=8),
                                             in1=m_ap.unsqueeze(1).to_broadcast([128, 8, 128]), op=ALU.mult)

        for i in range(NQ):
            qi = nxt("qT")
            P.dma("sp", qT[qi][:], QT[:, :, i * 128:(i + 1) * 128].rearrange("h d q -> d h q"), writes=[("qT", qi)])
            P.dma("sp", gq[qi][:], GQ[i * 128:(i + 1) * 128, :], writes=[("gq", qi)])
            jl = i // 8
            r = i % 8
            zero_acc()
            for jc in range(jl + 1):
                si = scores(kcm[:, jc * 128:(jc + 1) * 128], qi, "kcm")
                P.op("act", lambda e, si=si, jc=jc: e.activation(out=et[:, jc, :], in_=ST[si][:], func=AF.Exp,
                                                               scale=SCALE),
                     reads=[("ST", si)], writes=[("et", jc)])
                if jc == jl:
                    P.op("dve", bmul(et[:, jc, :], mc[:, r, :]), reads=[("et", jc), "mc"], writes=[("et", jc)])
                elif jc == jl - 1 and r == 0:
                    P.op("dve", bmul(et[:, jc, :], mcp[:]), reads=[("et", jc), "mcp"], writes=[("et", jc)])
            for jc in range(jl + 1):
                pv(lambda h, jc=jc: et[:, jc, h * 128:(h + 1) * 128], ("et", jc), vc1[:, jc, :], "vc1", jc == jl)
            combine(0, qi, True)
            P.op("dve", lambda e: e.tensor_scalar(out=coef[:], in0=zall[:], scalar1=1.0, scalar2=None, op0=ALU.mult),
                 reads=["zall"], writes=["coef2"]) if False else None
            for h in range(8):
                for jc in range(jl + 1):
                    P.op("pe", lambda e, h=h, jc=jc: e.matmul(MS[:, 0:256], lhsT=et[:, jc, h * 128:(h + 1) * 128],
                                                             rhs=amat[:, jc, :], start=(jc == 0), stop=(jc == jl)),
                         reads=[("et", jc), "amat"], writes=["MS"])
                if h == 0:
                    P.op("dve", lambda e: e.tensor_scalar(out=imp[:], in0=MS[:, 0:256], scalar1=zall[:, 0:1],
                                                          scalar2=None, op0=ALU.mult),
                         reads=["MS", "zall"], writes=["imp"])
                else:
                    P.op("dve", lambda e, h=h: e.scalar_tensor_tensor(
                        out=imp[:], in0=MS[:, 0:256], scalar=zall[:, h:h + 1], in1=imp[:], op0=ALU.mult,
                        op1=ALU.add), reads=["MS", "zall", "imp"], writes=["imp"])
            P.op("dve", lambda e, i=i: e.tensor_tensor(out=imp[:], in0=imp[:], in1=bcst[:, 256 - 4 * i:512 - 4 * i],
                                                      op=ALU.add), reads=["imp", "bcst"], writes=["imp"])
            P.op("dve", lambda e: e.memset(imp[:, 0:1], BIGV), reads=["imp"], writes=["imp"])
            P.op("dve", lambda e: e.max(out=m8[:, 0:8], in_=imp[:]), reads=["imp"], writes=["m8"])
            P.op("dve", lambda e: e.match_replace(out=sc2[:], in_to_replace=m8[:, 0:8], in_values=imp[:],
                                                  imm_value=-3.0e4), reads=["imp", "m8"], writes=["sc2"])
            P.op("dve", lambda e: e.max(out=m8[:, 8:16], in_=sc2[:]), reads=["sc2"], writes=["m8"])
            P.op("dve", lambda e: e.tensor_scalar(out=sel[:], in0=imp[:], scalar1=m8[:, 15:16], scalar2=None,
                                                  op0=ALU.is_ge), reads=["imp", "m8"], writes=["sel"])
            nchunk = 1 if (4 * i + 3) < 128 else 2
            MSb = MS[:].bitcast(BF16)
            for c in range(nchunk):
                P.op("pe", lambda e, c=c: e.transpose(out=MSb[:, c * 128:(c + 1) * 128],
                                                      in_=sel[:, c * 128:(c + 1) * 128], identity=idn[:]),
                     reads=["sel", "idn"], writes=["MS"])
            P.op("dve", lambda e, nchunk=nchunk: e.tensor_copy(
                out=selT[:, 0:nchunk, :], in_=MSb[:, 0:nchunk * 128].rearrange("p (c q) -> p c q", c=nchunk)),
                reads=["MS"], writes=["selT"])
            zero_acc()
            nkt = 2 * i + 2
            def sel_stage1(kt, i=i, qi=qi):
                si = scores(kst[:, kt * 128:(kt + 1) * 128], qi, "kst")
                pi = nxt("pt")
                P.op("act", lambda e, si=si, pi=pi: e.activation(out=pt[pi][:], in_=ST[si][:], func=AF.Exp,
                                                               scale=SCALE),
                     reads=[("ST", si)], writes=[("pt", pi)])
                P.op("pe", lambda e, kt=kt: e.matmul(MS[:, 0:128], lhsT=gexp[:, (kt % 64) * 128:(kt % 64 + 1) * 128],
                                                    rhs=selT[:, kt // 64, :], start=True, stop=True),
                     reads=["gexp", "selT"], writes=["MS"])
                mi = nxt("mk")
                if kt >= 2 * i:
                    P.op("dve", lambda e, mi=mi, kt=kt, i=i: e.tensor_tensor(out=mk[mi][:], in0=MS[:, 0:128],
                                                                          in1=cms[:, kt - 2 * i, :], op=ALU.mult),
                         reads=["MS", "cms"], writes=[("mk", mi)])
                else:
                    P.op("dve", lambda e, mi=mi: e.tensor_copy(out=mk[mi][:], in_=MS[:, 0:128]),
                         reads=["MS"], writes=[("mk", mi)])
                P.op("dve", bmul(pt[pi][:], mk[mi][:]), reads=[("pt", pi), ("mk", mi)], writes=[("pt", pi)])
                return pi

            pis = {0: sel_stage1(0)}
            for kt in range(nkt):
                if kt + 1 < nkt:
                    pis[kt + 1] = sel_stage1(kt + 1)
                pi = pis.pop(kt)
                pv(lambda h, pi=pi: pt[pi][:, h * 128:(h + 1) * 128], ("pt", pi), vs1[:, kt, :], "vs1",
                   kt == nkt - 1)
            combine(1, qi, False)
            zero_acc()
            wt_list = [(kt, w) for w, kt in enumerate(range(2 * i - 4, 2 * i + 2)) if kt >= 0]
            def win_stage1(kt, w, qi=qi):
                si = scores(kwt[:, kt * 128:(kt + 1) * 128], qi, "kwt")
                pi = nxt("pt")
                P.op("act", lambda e, si=si, pi=pi: e.activation(out=pt[pi][:], in_=ST[si][:], func=AF.Exp,
                                                               scale=SCALE),
                     reads=[("ST", si)], writes=[("pt", pi)])
                P.op("dve", bmul(pt[pi][:], cmw[:, w, :]), reads=[("pt", pi), "cmw"], writes=[("pt", pi)])
                return pi

            pis = {0: win_stage1(*wt_list[0])}
            for n_, (kt, w) in enumerate(wt_list):
                if n_ + 1 < len(wt_list):
                    pis[n_ + 1] = win_stage1(*wt_list[n_ + 1])
                pi = pis.pop(n_)
                pv(lambda h, pi=pi: pt[pi][:, h * 128:(h + 1) * 128], ("pt", pi), vw1[:, kt, :], "vw1",
                   n_ == len(wt_list) - 1)
            combine(2, qi, False)
            oi = nxt("obf")
            P.op("act", lambda e, oi=oi: e.activation(out=obf[oi][:], in_=oacc[:], func=AF.Copy),
                 reads=["oacc"], writes=[("obf", oi)])
            P.dma("sp", OA[i * 128:(i + 1) * 128, :], obf[oi][:], reads=[("obf", oi)])
        P.finish()
    return nc


_SW = np.concatenate([np.arange(64, 128), np.arange(64)])


def kernel(x, positions, norm_w, w_in, cmp_pe_k, cmp_w1_k, cmp_w2_k, cmp_pe_v, cmp_w1_v, cmp_w2_v,
           conv_w, conv_b, dt_bias, a_log, d_skip, norm_attn_w, norm_ssm_w, w_out, final_norm_w):
    f32 = lambda a: np.ascontiguousarray(np.asarray(a), dtype=np.float32)
    x = f32(x)
    positions = np.ascontiguousarray(np.asarray(positions), dtype=np.int32)
    norm_w, w_in, w_out = f32(norm_w), f32(w_in), f32(w_out)
    cmp_pe_k, cmp_w1_k, cmp_w2_k = f32(cmp_pe_k), f32(cmp_w1_k), f32(cmp_w2_k)
    cmp_pe_v, cmp_w1_v, cmp_w2_v = f32(cmp_pe_v), f32(cmp_w1_v), f32(cmp_w2_v)
    conv_w, conv_b, dt_bias, a_log, d_skip = f32(conv_w), f32(conv_b), f32(dt_bias), f32(a_log), f32(d_skip)
    norm_attn_w, norm_ssm_w, final_norm_w = f32(norm_attn_w), f32(norm_ssm_w), f32(final_norm_w)
    B, S, _ = x.shape
    cores = list(range(8))
    TPC = B * S // 8
    rc = rope_consts()
    ac = [attn_consts(0), attn_consts(1)]
    sc = ssd_consts()
    nc_proj, _ = build_proj(S)
    nc_cmp = build_cmp(S)
    nc_attn = build_attn(S)
    nc_ssd, _ = build_ssd(S)
    run = lambda nc, maps: run_bass_kernel_spmd(nc, maps, core_ids=cores).results
    for l in range(DEPTH):
        w = w_in[l]
        maps = []
        for c in cores:
            b, g, par, j = core_coords(c)
            wf, wt = proj_weight_layout(w, g, j)
            maps.append(dict(x=x[b], pos=positions[b], xq=my_tiles(x[b], par), posq=my_tiles(positions[b], par),
                             nw=norm_w[l], wf=wf, wt=wt, ident=IDENT_BF, invf=rc))
        rp = run(nc_proj, maps)
        del maps
        maps = []
        for c in cores:
            b, g, par, j = core_coords(c)
            pc = positions[b, 31::16]
            maps.append(dict(KCT=rp[c]["KCT"], VCT=rp[c]["VCT"], w1k=cmp_w1_k[l], w1v=cmp_w1_v[l],
                             w2k=cmp_w2_k[l], w2v=cmp_w2_v[l], w2ks=np.ascontiguousarray(cmp_w2_k[l][:, _SW]),
                             peTk=np.ascontiguousarray(cmp_pe_k[l].T), peTv=np.ascontiguousarray(cmp_pe_v[l].T),
                             posc=np.ascontiguousarray(np.concatenate([pc, pc[-1:]]), dtype=np.int32), invf=rc))
        rcm = run(nc_cmp, maps)
        maps = []
        for c in cores:
            b, g, par, j = core_coords(c)
            vg = np.asarray(rp[c]["VG"])
            m = dict(QT=rp[c]["QT"], KST=rp[c]["KST"], KWT=rp[c]["KWT"], VG=vg,
                     GQ=my_tiles(np.ascontiguousarray(vg[:, 256:288]), par),
                     KCMPT=rcm[c]["KCMPT"], VCMP=rcm[c]["VCMP"], ident=IDENT_BF)
            m.update(ac[par])
            maps.append(m)
        ra = run(nc_attn, maps)
        maps = []
        for c in cores:
            b, g, par, j = core_coords(c)
            m = dict(XBCT=rp[c]["XBCT"], DTT=rp[c]["DTT"], ident=IDENT_BF)
            m.update(sc)
            m.update(ssd_param_layout(conv_w[l], conv_b[l], dt_bias[l], a_log[l], d_skip[l], j))
            maps.append(m)
        rs = run(nc_ssd, maps)
        del rp
        oa = np.zeros((B, S, 2048), NPBF)
        ys = np.zeros((B, S, 2048), NPBF)
        for c in cores:
            b, g, par, j = core_coords(c)
            oa[b].reshape(S // 256, 2, 128, 2048)[:, par, :, g * 1024:(g + 1) * 1024] = \
                np.asarray(ra[c]["OA"]).reshape(S // 256, 128, 1024)
            ys[b, :, 512 * j:512 * (j + 1)] = np.asarray(rs[c]["YS"])
        xf = x.reshape(B * S, D_MODEL)
        oaf = oa.reshape(B * S, 2048)
        ysf = ys.reshape(B * S, 2048)
        wz = np.ascontiguousarray(np.concatenate([w[:, OFF_ZA:OFF_ZA + 2048], w[:, OFF_ZS:OFF_ZS + 2048]], 1))
        nc_out = build_out(TPC, l == DEPTH - 1)
        maps = []
        for c in cores:
            r = slice(c * TPC, (c + 1) * TPC)
            maps.append(dict(x=xf[r], oa=oaf[r], ys=ysf[r], wz=wz, wo=w_out[l], nw=norm_w[l], nwa=norm_attn_w[l],
                             nws=norm_ssm_w[l], fnw=final_norm_w, ident=IDENT_BF))
        ro = run(nc_out, maps)
        x = np.concatenate([np.asarray(ro[c]["y"]) for c in cores], 0).reshape(B, S, D_MODEL).astype(np.float32)
    return x
```

```python
import math, os
from contextlib import ExitStack
import numpy as np
import ml_dtypes
import concourse.bass as bass
import concourse.mybir as mybir
from concourse.bass_utils import run_bass_kernel_spmd

F32 = mybir.dt.float32
BF16 = mybir.dt.bfloat16
I32 = mybir.dt.int32
AF = mybir.ActivationFunctionType
ALU = mybir.AluOpType
AX = mybir.AxisListType
NPBF = ml_dtypes.bfloat16

D_MODEL = 2048
DEPTH = 2
HEAD_DIM = 128
D_PROJ = 11856
EPS = 1e-6
OFF_Q, OFF_KC, OFF_VC, OFF_KS, OFF_VS, OFF_KW, OFF_VW = 0, 2048, 2304, 2560, 2816, 3072, 3328
OFF_GATE, OFF_ZA, OFF_X, OFF_B, OFF_C, OFF_DT, OFF_ZS = 3584, 3632, 5680, 7728, 8752, 9776, 9808
KC = D_MODEL // 128


class Prog:
    STREAMS = ("pe", "act", "dve", "pool", "sp")

    def __init__(self, nc, es, n_dma_sems=10):
        self.nc, self.es = nc, es
        self.ops = {s: [] for s in self.STREAMS}
        self.sems = []
        self.csem = {}
        for s in ("pe", "act", "dve", "pool"):
            self.csem[s] = self._new_sem("c_" + s)
        self.ccnt = {s: 0 for s in self.csem}
        self.dsem = {q: [[self._new_sem(f"d_{q}{i}"), 0] for i in range(n_dma_sems)]
                     for q in ("sp", "act", "pool")}
        self.drr = {q: 0 for q in self.dsem}
        self.lastw = {}
        self.readers = {}
        self.known = {s: {} for s in self.STREAMS}
        self.nsb = 0

    def _new_sem(self, name):
        h = self.es.enter_context(self.nc.semaphore(name))
        self.sems.append(h)
        return len(self.sems) - 1

    def sb(self, shape, dt, name=None):
        self.nsb += 1
        return self.es.enter_context(self.nc.sbuf_tensor(name or f"sb{self.nsb}", list(shape), dt))

    def ps(self, shape, dt, name=None):
        self.nsb += 1
        return self.es.enter_context(self.nc.psum_tensor(name or f"ps{self.nsb}", list(shape), dt))

    def _emit(self, stream, fn, reads, writes, dma):
        deps = []
        for b in reads:
            w = self.lastw.get(b)
            if w:
                deps.append(w)
        for b in writes:
            w = self.lastw.get(b)
            if w:
                deps.append(w)
            deps.extend(self.readers.get(b, {}).items())
        if dma:
            pool = self.dsem[stream]
            slot = pool[self.drr[stream] % len(pool)]
            self.drr[stream] += 1
            if slot[1] > 0:
                deps.append((slot[0], slot[1]))
            slot[1] += 16
            tok = (slot[0], slot[1])
            inc = 16
        else:
            self.ccnt[stream] += 1
            tok = (self.csem[stream], self.ccnt[stream])
            inc = 1
        waits = {}
        kn = self.known[stream]
        for sid, v in deps:
            if stream == "pe" and not dma and sid == self.csem["pe"]:
                continue
            if kn.get(sid, 0) >= v:
                continue
            if waits.get(sid, 0) < v:
                waits[sid] = v
        for sid, v in waits.items():
            kn[sid] = v
        self.ops[stream].append((sorted(waits.items()), fn, tok[0], inc))
        for b in reads:
            r = self.readers.setdefault(b, {})
            if r.get(tok[0], 0) < tok[1]:
                r[tok[0]] = tok[1]
        for b in writes:
            self.lastw[b] = tok
            self.readers[b] = {}
        return tok

    def op(self, stream, fn, reads=(), writes=()):
        return self._emit(stream, fn, reads, writes, False)

    def dma(self, queue, out, in_, reads=(), writes=(), **kw):
        return self._emit(queue, lambda e: e.dma_start(out=out, in_=in_, **kw), reads, writes, True)

    def check(self):
        val = [0] * len(self.sems)
        pc = {s: 0 for s in self.STREAMS}
        prog = True
        while prog:
            prog = False
            for s in self.STREAMS:
                ops = self.ops[s]
                while pc[s] < len(ops):
                    waits, fn, sid, inc = ops[pc[s]]
                    if all(val[ws] >= wv for ws, wv in waits):
                        val[sid] += inc
                        pc[s] += 1
                        prog = True
                    else:
                        break
        stuck = {s: (pc[s], len(self.ops[s])) for s in self.STREAMS if pc[s] < len(self.ops[s])}
        if stuck:
            msg = []
            for s, (i, n) in stuck.items():
                waits = self.ops[s][i][0]
                msg.append(f"{s} stuck at {i}/{n}: waits {[(ws, wv, val[ws]) for ws, wv in waits]}")
            raise RuntimeError("DEADLOCK: " + "; ".join(msg))

    def finish(self):
        self.check()
        finals = []
        for q in self.dsem:
            for sid, v in self.dsem[q]:
                if v > 0:
                    finals.append((sid, v))
        for s in self.csem:
            if self.ccnt[s] > 0:
                finals.append((self.csem[s], self.ccnt[s]))
        nc = self.nc
        ops, sems = self.ops, self.sems
        with nc.Block() as blk:
            def run(stream, e):
                for waits, fn, sid, inc in ops[stream]:
                    for ws, wv in waits:
                        e.wait_ge(sems[ws], wv)
                    fn(e).then_inc(sems[sid], inc)

            @blk.tensor
            def _(e):
                run("pe", e)

            @blk.scalar
            def _(e):
                run("act", e)

            @blk.vector
            def _(e):
                run("dve", e)

            @blk.gpsimd
            def _(e):
                run("pool", e)

            @blk.sync
            def _(e):
                run("sp", e)
                for ws, wv in finals:
                    e.wait_ge(sems[ws], wv)


def _bcast_rows(ap_1d, n=128):
    return ap_1d.partition_broadcast(n)


def rms_rstd(P, ss, rstd, inv_n, kss, krs):
    P.op("dve", lambda e: e.tensor_scalar(out=ss, in0=ss, scalar1=inv_n, scalar2=EPS,
                                          op0=ALU.mult, op1=ALU.add), reads=[kss], writes=[kss])
    P.op("act", lambda e: e.activation(out=ss, in_=ss, func=AF.Sqrt), reads=[kss], writes=[kss])
    P.op("dve", lambda e: e.reciprocal(out=rstd, in_=ss), reads=[kss], writes=[krs])


def norm_to_hT(P, xt, kx, hbt, khb, sst, kss, rst, krs, nw_bc, knw, tpt, ktp, idn, hT_dst, khT):
    P.op("act", lambda e: e.activation(out=hbt, in_=xt, func=AF.Square, accum_out=sst),
         reads=[kx], writes=[khb, kss])
    rms_rstd(P, sst, rst, 1.0 / D_MODEL, kss, krs)
    P.op("dve", lambda e: e.scalar_tensor_tensor(out=hbt, in0=xt, scalar=rst, in1=nw_bc,
                                                 op0=ALU.mult, op1=ALU.mult),
         reads=[kx, krs, knw], writes=[khb])
    for k in range(KC):
        P.op("pe", lambda e, k=k: e.transpose(out=tpt[:, k * 128:(k + 1) * 128],
                                              in_=hbt[:, k * 128:(k + 1) * 128], identity=idn),
             reads=[khb, "idn"], writes=[ktp])
    P.op("act", lambda e: e.activation(out=hT_dst, in_=tpt.rearrange("p (k t) -> p k t", k=KC), func=AF.Copy),
         reads=[ktp], writes=[khT])


def build_out(TPC, last):
    nc = bass.Bass("TRN2", target_bir_lowering=False)
    dt = nc.dram_tensor
    x = dt("x", [TPC, D_MODEL], F32, kind="ExternalInput").ap()
    oa = dt("oa", [TPC, 2048], BF16, kind="ExternalInput").ap()
    ys = dt("ys", [TPC, 2048], BF16, kind="ExternalInput").ap()
    wz = dt("wz", [D_MODEL, 4096], F32, kind="ExternalInput").ap()
    wo = dt("wo", [4096, D_MODEL], F32, kind="ExternalInput").ap()
    nw = dt("nw", [D_MODEL], F32, kind="ExternalInput").ap()
    nwa = dt("nwa", [2048], F32, kind="ExternalInput").ap()
    nws = dt("nws", [2048], F32, kind="ExternalInput").ap()
    fnw = dt("fnw", [D_MODEL], F32, kind="ExternalInput").ap()
    ident = dt("ident", [128, 128], BF16, kind="ExternalInput").ap()
    y = dt("y", [TPC, D_MODEL], F32, kind="ExternalOutput").ap()
    TB = min(512, TPC)
    NT = TB // 128
    NB = TPC // TB
    wz_v = wz.rearrange("(kc p) c -> p kc c", p=128)
    wo_v = wo.rearrange("(kc p) c -> p kc c", p=128)
    with ExitStack() as es:
        P = Prog(nc, es)
        idn = P.sb([128, 128], BF16)
        nw_bc = P.sb([128, 2048], F32)
        nwa_bc = P.sb([128, 2048], BF16)
        nws_bc = P.sb([128, 2048], BF16)
        fnw_bc = P.sb([128, 2048], F32)
        P.dma("sp", idn[:], ident[:, :], writes=["idn"])
        P.dma("sp", nw_bc[:], _bcast_rows(nw), writes=["nw"])
        P.dma("pool", nwa_bc[:], _bcast_rows(nwa), writes=["nwa"])
        P.dma("pool", nws_bc[:], _bcast_rows(nws), writes=["nws"])
        P.dma("sp", fnw_bc[:], _bcast_rows(fnw), writes=["fnw"])
        res = [P.sb([128, 2048], F32) for _ in range(NT)]
        hb = [P.sb([128, 2048], BF16) for _ in range(2)]
        hT = P.sb([128, KC, TB], BF16)
        wblk = [P.sb([128, 8192], BF16) for _ in range(2)]
        ob = [P.sb([128, 512], BF16) for _ in range(2)]
        zs = [P.sb([128, 512], F32) for _ in range(2)]
        u = [P.sb([128, 4096], BF16) for _ in range(NT)]
        mT = P.sb([128, 32, TB], BF16)
        ss = [P.sb([128, 16], F32) for _ in range(NT)]
        rs = [P.sb([128, 16], F32) for _ in range(NT)]
        tp = [P.ps([128, 2048], BF16) for _ in range(2)]
        mm = [P.ps([128, 512], F32) for _ in range(2)]
        cnt = {"hb": 0, "w": 0, "mm": 0, "tp": 0, "ob": 0}

        def nxt(k, n=2):
            v = cnt[k] % n
            cnt[k] += 1
            return v

        for blk_i in range(NB):
            t0 = blk_i * TB
            for t in range(NT):
                rows = slice(t0 + t * 128, t0 + (t + 1) * 128)
                P.dma("sp", res[t][:], x[rows, :], writes=[("res", t)])
                b = nxt("hb")
                pb = nxt("tp")
                norm_to_hT(P, res[t][:], ("res", t), hb[b][:], ("hb", b), ss[t][:, 15:16], ("ssn", t),
                           rs[t][:, 15:16], ("rsn", t), nw_bc[:], "nw", tp[pb][:], ("tp", pb), idn[:],
                           hT[:, :, t * 128:(t + 1) * 128], ("hT", t))
            for t in range(NT):
                P.op("pool", lambda e, t=t: e.memset(ss[t][:, 0:13], 0.0), writes=[("ss2", t)])
            for cb in range(8):
                wbi = nxt("w")
                wv = wblk[wbi][:].rearrange("p (k c) -> p k c", k=KC)
                P.dma("pool", wv, wz_v[:, :, cb * 512:(cb + 1) * 512], writes=[("w", wbi)])
                for t in range(NT):
                    rows = slice(t0 + t * 128, t0 + (t + 1) * 128)
                    mi = nxt("mm")
                    for k in range(KC):
                        P.op("pe", lambda e, k=k, t=t, mi=mi, wv=wv: e.matmul(
                            mm[mi][:], lhsT=hT[:, k, t * 128:(t + 1) * 128], rhs=wv[:, k, :],
                            start=(k == 0), stop=(k == KC - 1)),
                            reads=[("hT", t), ("w", wbi)], writes=[("mm", mi)])
                    oi = nxt("ob")
                    src = oa if cb < 4 else ys
                    c0 = (cb % 4) * 512
                    P.dma("sp", ob[oi][:], src[rows, c0:c0 + 512], writes=[("ob", oi)])
                    P.op("act", lambda e, mi=mi, oi=oi: e.activation(out=zs[oi][:], in_=mm[mi][:], func=AF.Silu),
                         reads=[("mm", mi)], writes=[("zs", oi)])
                    ucol = cb * 512
                    P.op("dve", lambda e, oi=oi, t=t, ucol=ucol: e.tensor_tensor(
                        out=u[t][:, ucol:ucol + 512], in0=zs[oi][:], in1=ob[oi][:], op=ALU.mult),
                        reads=[("zs", oi), ("ob", oi)], writes=[("u", t)])
                    if cb < 4:
                        P.op("act", lambda e, oi=oi, t=t, ucol=ucol, cb=cb: e.activation(
                            out=zs[oi][:], in_=u[t][:, ucol:ucol + 512], func=AF.Square,
                            accum_out=ss[t][:, cb:cb + 1]),
                            reads=[("u", t), ("ss2", t)], writes=[("zs", oi), ("ss2", t)])
                    else:
                        for hh in range(2):
                            g = (cb - 4) * 2 + hh
                            P.op("act", lambda e, oi=oi, t=t, ucol=ucol, g=g, hh=hh: e.activation(
                                out=zs[oi][:, hh * 256:hh * 256 + 256],
                                in_=u[t][:, ucol + hh * 256:ucol + hh * 256 + 256], func=AF.Square,
                                accum_out=ss[t][:, 4 + g:5 + g]),
                                reads=[("u", t), ("ss2", t)], writes=[("zs", oi), ("ss2", t)])
            for t in range(NT):
                k2 = ("ss2", t)
                P.op("dve", lambda e, t=t: e.tensor_reduce(out=ss[t][:, 12:13], in_=ss[t][:, 0:4], axis=AX.X,
                                                          op=ALU.add), reads=[k2], writes=[k2])
                P.op("dve", lambda e, t=t: e.tensor_scalar(out=ss[t][:, 12:13], in0=ss[t][:, 12:13],
                                                          scalar1=1.0 / 2048, scalar2=EPS, op0=ALU.mult, op1=ALU.add),
                     reads=[k2], writes=[k2])
                P.op("dve", lambda e, t=t: e.tensor_scalar(out=ss[t][:, 4:12], in0=ss[t][:, 4:12],
                                                          scalar1=1.0 / 256, scalar2=EPS, op0=ALU.mult, op1=ALU.add),
                     reads=[k2], writes=[k2])
                P.op("act", lambda e, t=t: e.activation(out=ss[t][:, 4:13], in_=ss[t][:, 4:13], func=AF.Sqrt),
                     reads=[k2], writes=[k2])
                P.op("dve", lambda e, t=t: e.reciprocal(out=rs[t][:, 4:13], in_=ss[t][:, 4:13]),
                     reads=[k2], writes=[("rs2", t)])
                P.op("dve", lambda e, t=t: e.scalar_tensor_tensor(
                    out=u[t][:, 0:2048], in0=u[t][:, 0:2048], scalar=rs[t][:, 12:13], in1=nwa_bc[:],
                    op0=ALU.mult, op1=ALU.mult), reads=[("u", t), ("rs2", t), "nwa"], writes=[("u", t)])
                for g in range(8):
                    cs = slice(2048 + g * 256, 2048 + (g + 1) * 256)
                    P.op("dve", lambda e, t=t, g=g, cs=cs: e.scalar_tensor_tensor(
                        out=u[t][:, cs], in0=u[t][:, cs], scalar=rs[t][:, 4 + g:5 + g],
                        in1=nws_bc[:, g * 256:(g + 1) * 256], op0=ALU.mult, op1=ALU.mult),
                        reads=[("u", t), ("rs2", t), "nws"], writes=[("u", t)])
                for half in range(2):
                    pb = nxt("tp")
                    for k in range(16):
                        kk = half * 16 + k
                        P.op("pe", lambda e, t=t, k=k, kk=kk, pb=pb: e.transpose(
                            out=tp[pb][:, k * 128:(k + 1) * 128], in_=u[t][:, kk * 128:(kk + 1) * 128],
                            identity=idn[:]), reads=[("u", t), "idn"], writes=[("tp", pb)])
                    P.op("act", lambda e, pb=pb, t=t, half=half: e.activation(
                        out=mT[:, half * 16:(half + 1) * 16, t * 128:(t + 1) * 128],
                        in_=tp[pb][:].rearrange("p (k t) -> p k t", k=16), func=AF.Copy),
                        reads=[("tp", pb)], writes=[("mT", t)])
            for cb in range(8):
                wbi = nxt("w")
                wv = wblk[wbi][:].rearrange("p (k c) -> p k c", k=32)
                P.dma("pool", wv, wo_v[:, :, cb * 256:(cb + 1) * 256], writes=[("w", wbi)])
                for t in range(NT):
                    mi = nxt("mm")
                    for k in range(32):
                        P.op("pe", lambda e, k=k, t=t, mi=mi, wv=wv: e.matmul(
                            mm[mi][:, 0:256], lhsT=mT[:, k, t * 128:(t + 1) * 128], rhs=wv[:, k, :],
                            start=(k == 0), stop=(k == 31)),
                            reads=[("mT", t), ("w", wbi)], writes=[("mm", mi)])
                    P.op("dve", lambda e, mi=mi, t=t, cb=cb: e.tensor_tensor(
                        out=res[t][:, cb * 256:(cb + 1) * 256], in0=mm[mi][:, 0:256],
                        in1=res[t][:, cb * 256:(cb + 1) * 256], op=ALU.add),
                        reads=[("mm", mi), ("res", t)], writes=[("res", t)])
            for t in range(NT):
                rows = slice(t0 + t * 128, t0 + (t + 1) * 128)
                if last:
                    b = nxt("hb")
                    P.op("act", lambda e, t=t, b=b: e.activation(out=hb[b][:], in_=res[t][:], func=AF.Square,
                                                               accum_out=ss[t][:, 14:15]),
                         reads=[("res", t)], writes=[("hb", b), ("ssf", t)])
                    rms_rstd(P, ss[t][:, 14:15], rs[t][:, 14:15], 1.0 / D_MODEL, ("ssf", t), ("rsf", t))
                    P.op("dve", lambda e, t=t: e.scalar_tensor_tensor(
                        out=res[t][:], in0=res[t][:], scalar=rs[t][:, 14:15], in1=fnw_bc[:],
                        op0=ALU.mult, op1=ALU.mult), reads=[("res", t), ("rsf", t), "fnw"], writes=[("res", t)])
                P.dma("sp", y[rows, :], res[t][:], reads=[("res", t)])
        P.finish()
    return nc


NFB = 31
TWO_PI = 2.0 * math.pi
CW1 = 6.28125
CW2 = TWO_PI - 6.28125
PI_LO = 3.141592


def proj_decl(nc, S, kind_out):
    dt = nc.dram_tensor
    D = {}
    D["x"] = dt("x", [S, D_MODEL], F32, kind="ExternalInput").ap()
    D["pos"] = dt("pos", [S], I32, kind="ExternalInput").ap()
    D["xq"] = dt("xq", [S // 2, D_MODEL], F32, kind="ExternalInput").ap()
    D["posq"] = dt("posq", [S // 2], I32, kind="ExternalInput").ap()
    D["nw"] = dt("nw", [D_MODEL], F32, kind="ExternalInput").ap()
    D["wf"] = dt("wf", [NFB, 128, KC * 128], F32, kind="ExternalInput").ap()
    D["wt"] = dt("wt", [128, KC * 288], F32, kind="ExternalInput").ap()
    D["ident"] = dt("ident", [128, 128], BF16, kind="ExternalInput").ap()
    D["invf"] = dt("invf", [128, 2], F32, kind="ExternalInput").ap()
    D["QT"] = dt("QT", [8, 128, S // 2], BF16, kind=kind_out).ap()
    for n in ("KST", "KWT", "KCT", "VCT"):
        D[n] = dt(n, [128, S], BF16, kind=kind_out).ap()
    D["VG"] = dt("VG", [S, 288], BF16, kind=kind_out).ap()
    D["XBCT"] = dt("XBCT", [1024, S], BF16, kind=kind_out).ap()
    D["DTT"] = dt("DTT", [8, S], F32, kind=kind_out).ap()
    return D


def rope_tables(P, posf, kpos, invf, ang, kf, tmp, cosT, sinT, n, tag):
    ka, kk, kt = (tag, "ang"), (tag, "kf"), (tag, "tmp")
    kfi = kf.bitcast(I32)
    P.op("dve", lambda e: e.tensor_scalar(out=ang, in0=posf, scalar1=invf[:, 0:1], scalar2=None, op0=ALU.mult),
         reads=[kpos, "invf"], writes=[ka])
    P.op("dve", lambda e: e.tensor_scalar(out=tmp, in0=ang, scalar1=1.0 / TWO_PI, scalar2=None, op0=ALU.mult),
         reads=[ka], writes=[kt])
    P.op("dve", lambda e: e.tensor_copy(out=kfi, in_=tmp), reads=[kt], writes=[kk])
    P.op("dve", lambda e: e.tensor_copy(out=tmp, in_=kfi), reads=[kk], writes=[kt])
    P.op("dve", lambda e: e.scalar_tensor_tensor(out=ang, in0=tmp, scalar=-CW1, in1=ang, op0=ALU.mult, op1=ALU.add),
         reads=[kt, ka], writes=[ka])
    P.op("dve", lambda e: e.scalar_tensor_tensor(out=ang, in0=tmp, scalar=-CW2, in1=ang, op0=ALU.mult, op1=ALU.add),
         reads=[kt, ka], writes=[ka])

    def wrap(buf, kb):
        P.op("dve", lambda e: e.tensor_scalar(out=tmp, in0=buf, scalar1=math.pi, scalar2=-TWO_PI,
                                              op0=ALU.is_gt, op1=ALU.mult), reads=[kb], writes=[kt])
        P.op("dve", lambda e: e.tensor_tensor(out=buf, in0=buf, in1=tmp, op=ALU.add), reads=[kb, kt], writes=[kb])
        P.op("dve", lambda e: e.tensor_scalar(out=tmp, in0=buf, scalar1=-math.pi, scalar2=TWO_PI,
                                              op0=ALU.is_lt, op1=ALU.mult), reads=[kb], writes=[kt])
        P.op("dve", lambda e: e.tensor_tensor(out=buf, in0=buf, in1=tmp, op=ALU.add), reads=[kb, kt], writes=[kb])
        P.op("dve", lambda e: e.tensor_scalar(out=buf, in0=buf, scalar1=PI_LO, scalar2=-PI_LO,
                                              op0=ALU.min, op1=ALU.max), reads=[kb], writes=[kb])

    wrap(ang, ka)
    kc_, ks_ = (tag, "cos"), (tag, "sin")
    P.op("act", lambda e: e.activation(out=sinT, in_=ang, func=AF.Sin, scale=invf[:, 1:2]),
         reads=[ka, "invf"], writes=[ks_])
    P.op("dve", lambda e: e.tensor_scalar(out=kf, in0=ang, scalar1=math.pi / 2, scalar2=None, op0=ALU.add),
         reads=[ka], writes=[kk])
    wrap(kf, kk)
    P.op("act", lambda e: e.activation(out=cosT, in_=kf, func=AF.Sin), reads=[kk], writes=[kc_])
    return kc_, ks_


def emit_proj(P, S, D):
    SBT = min(2048, S)
    NSB = S // SBT
    NTS = SBT // 128
    x, pos = D["x"], D["pos"]
    idn = P.sb([128, 128], BF16)
    nw_bc = P.sb([128, 2048], F32)
    invf = P.sb([128, 2], F32)
    P.dma("sp", idn[:], D["ident"][:, :], writes=["idn"])
    P.dma("sp", nw_bc[:], _bcast_rows(D["nw"]), writes=["nw"])
    P.dma("sp", invf[:], D["invf"][:, :], writes=["invf"])
    xb = [P.sb([128, 2048], F32) for _ in range(2)]
    hb = [P.sb([128, 2048], BF16) for _ in range(2)]
    ssn = P.sb([128, 4], F32)
    hT = P.sb([128, KC, SBT], BF16)
    posi = P.sb([128, SBT], I32)
    posf = P.sb([128, SBT], F32)
    ang = P.sb([128, SBT], F32)
    kf = P.sb([128, SBT], F32)
    tmp = P.sb([128, SBT], F32)
    cosT = P.sb([128, SBT], F32)
    sinT = P.sb([128, SBT], F32)
    wb = [P.sb([128, KC * 128], BF16) for _ in range(3)]
    wt = P.sb([128, KC * 288], BF16)
    stg = [P.sb([128, 512], BF16) for _ in range(4)]
    stf = [P.sb([128, 512], F32) for _ in range(2)]
    t1 = [P.sb([128, 512], F32) for _ in range(2)]
    t2 = [P.sb([128, 512], F32) for _ in range(2)]
    vst = [P.sb([128, 288], BF16) for _ in range(2)]
    tp = [P.ps([128, 2048], BF16) for _ in range(2)]
    mm = [P.ps([128, 512], F32) for _ in range(4)]
    cnt = {}

    def nxt(k, n):
        v = cnt.get(k, 0)
        cnt[k] = v + 1
        return v % n

    P.dma("pool", wt[:].rearrange("p (a c) -> p a c", c=1152), D["wt"].rearrange("p (a c) -> p a c", c=1152), writes=["wt"])
    wtv = wt[:].rearrange("p (k c) -> p k c", k=KC)
    kcos, ksin = ("rp", "cos"), ("rp", "sin")

    def prep_block(xsrc, psrc, r0, n):
        SK = os.environ.get('PROJ_SKIP', '')
        if 'r' not in SK:
            P.dma("sp", posi[:, 0:n], psrc[r0:r0 + n].partition_broadcast(128), writes=["posi"])
            P.op("dve", lambda e: e.tensor_copy(out=posf[:, 0:n], in_=posi[:, 0:n]), reads=["posi"], writes=["posf"])
        if 'r' not in SK and 'R' not in SK:
            rope_tables(P, posf[:, 0:n], "posf", invf, ang[:, 0:n], kf[:, 0:n], tmp[:, 0:n], cosT[:, 0:n],
                        sinT[:, 0:n], n, "rp")
        for t in range(n // 128):
            rows = slice(r0 + t * 128, r0 + (t + 1) * 128)
            b = nxt("xb", 2)
            pb = nxt("tp", 2)
            P.dma("sp", xb[b][:], xsrc[rows, :], writes=[("xb", b)])
            norm_to_hT(P, xb[b][:], ("xb", b), hb[b][:], ("hb", b), ssn[:, b:b + 1], ("ssn", b),
                       ssn[:, 2 + b:3 + b], ("rsn", b), nw_bc[:], "nw", tp[pb][:], ("tp", pb), idn[:],
                       hT[:, :, t * 128:(t + 1) * 128], ("hT", t))

    def load_w(fb):
        wi = nxt("wb", 3)
        P.dma("pool", wb[wi][:].rearrange("p (a c) -> p a c", c=1024),
              D["wf"][fb, :, :].rearrange("p (a c) -> p a c", c=1024), writes=[("wb", wi)])
        return wi, wb[wi][:].rearrange("p (k c) -> p k c", k=KC)

    def mm_block(wi, wv, rhs_fn, n, reads_h):
        mi = nxt("mm", 4)
        for k in range(KC):
            P.op("pe", lambda e, k=k, mi=mi, wv=wv: e.matmul(
                mm[mi][:, 0:n], lhsT=wv[:, k, :], rhs=rhs_fn(k), start=(k == 0), stop=(k == KC - 1)),
                reads=reads_h + [("wb", wi)], writes=[("mm", mi)])
        return mi

    def rope_store(ma, mb, cs, sn, dst, n):
        ti = nxt("t12", 2)
        si = nxt("stg", 4)
        P.op("dve", lambda e: e.tensor_tensor(out=t1[ti][:, 0:n], in0=mm[ma][:, 0:n], in1=cs, op=ALU.mult),
             reads=[("mm", ma), kcos], writes=[("t1", ti)])
        P.op("dve", lambda e: e.tensor_tensor(out=t2[ti][:, 0:n], in0=mm[mb][:, 0:n], in1=sn, op=ALU.mult),
             reads=[("mm", mb), ksin], writes=[("t2", ti)])
        P.op("pool", lambda e: e.tensor_tensor(out=stg[si][:, 0:n], in0=t1[ti][:, 0:n], in1=t2[ti][:, 0:n],
                                               op=ALU.add),
             reads=[("t1", ti), ("t2", ti)], writes=[("stg", si)])
        P.dma("sp", dst, stg[si][:, 0:n], reads=[("stg", si)])

    SQ = S // 2
    SBQ = min(2048, SQ)
    SKIP = os.environ.get('PROJ_SKIP', '')
    for qb in range(0 if 'q' in SKIP else SQ // SBQ):
        q0 = qb * SBQ
        prep_block(D["xq"], D["posq"], q0, SBQ)
        hq_all = [("hT", t) for t in range(SBQ // 128)]
        CHQ = min(512, SBQ)
        for r in range(8):
            wa, wva = load_w(r)
            wb_, wvb = load_w(8 + r)
            for c in range(SBQ // CHQ):
                cs_ = slice(c * CHQ, (c + 1) * CHQ)
                rf = lambda k, cs_=cs_: hT[:, k, cs_]
                ma = mm_block(wa, wva, rf, CHQ, hq_all)
                mb = mm_block(wb_, wvb, rf, CHQ, hq_all)
                rope_store(ma, mb, cosT[:, cs_], sinT[:, cs_], D["QT"][r, :, q0 + c * CHQ:q0 + (c + 1) * CHQ], CHQ)

    for sbi in range(NSB):
        s0 = sbi * SBT
        prep_block(x, pos, s0, SBT)
        hT_all = [("hT", t) for t in range(NTS)]
        for t in range(0 if 't' in SKIP else NTS):
            rows = slice(s0 + t * 128, s0 + (t + 1) * 128)
            mi = nxt("mm", 4)
            for k in range(KC):
                P.op("pe", lambda e, k=k, t=t, mi=mi: e.matmul(
                    mm[mi][:, 0:288], lhsT=hT[:, k, t * 128:(t + 1) * 128], rhs=wtv[:, k, :],
                    start=(k == 0), stop=(k == KC - 1)), reads=[("hT", t), "wt"], writes=[("mm", mi)])
            vi = nxt("vst", 2)
            P.op("act", lambda e, mi=mi, vi=vi: e.activation(out=vst[vi][:, 0:256], in_=mm[mi][:, 0:256],
                                                           func=AF.Copy),
                 reads=[("mm", mi)], writes=[("vstA", vi)])
            P.op("act", lambda e, mi=mi, vi=vi: e.activation(out=vst[vi][:, 256:288], in_=mm[mi][:, 256:288],
                                                           func=AF.Sigmoid),
                 reads=[("mm", mi)], writes=[("vstB", vi)])
            P.dma("sp", D["VG"][rows, :], vst[vi][:], reads=[("vstA", vi), ("vstB", vi)])

        CH = min(512, SBT)
        for (fa, name) in (() if 'k' in SKIP else ((16, "KST"), (18, "KWT"))):
            wa, wva = load_w(fa)
            wb_, wvb = load_w(fa + 1)
            for c in range(SBT // CH):
                cs_ = slice(c * CH, (c + 1) * CH)
                rf = lambda k, cs_=cs_: hT[:, k, cs_]
                ma = mm_block(wa, wva, rf, CH, hT_all)
                mb = mm_block(wb_, wvb, rf, CH, hT_all)
                rope_store(ma, mb, cosT[:, cs_], sinT[:, cs_], D[name][:, s0 + c * CH:s0 + (c + 1) * CH], CH)
        plain = [(20, "KCT", 0), (21, "VCT", 0)] + [(22 + i, "XBCT", i * 128) for i in range(8)] + [(30, "DTT", 0)]
        for (fb, name, r0) in ([] if 'p' in SKIP else plain):
            wa, wva = load_w(fb)
            for c in range(SBT // CH):
                cs_ = slice(c * CH, (c + 1) * CH)
                rf = lambda k, cs_=cs_: hT[:, k, cs_]
                ma = mm_block(wa, wva, rf, CH, hT_all)
                tok = slice(s0 + c * CH, s0 + (c + 1) * CH)
                if name == "DTT":
                    fi = nxt("stf", 2)
                    P.op("act", lambda e, ma=ma, fi=fi: e.activation(out=stf[fi][0:8, 0:CH], in_=mm[ma][0:8, 0:CH],
                                                                   func=AF.Copy),
                         reads=[("mm", ma)], writes=[("stf", fi)])
                    P.dma("sp", D["DTT"][:, tok], stf[fi][0:8, 0:CH], reads=[("stf", fi)])
                else:
                    si = nxt("stg", 4)
                    eng = "act" if (cnt["stg"] % 2) else "dve"
                    if eng == "act":
                        P.op("act", lambda e, ma=ma, si=si: e.activation(out=stg[si][:, 0:CH], in_=mm[ma][:, 0:CH],
                                                                       func=AF.Copy),
                             reads=[("mm", ma)], writes=[("stg", si)])
                    else:
                        P.op("dve", lambda e, ma=ma, si=si: e.tensor_copy(out=stg[si][:, 0:CH], in_=mm[ma][:, 0:CH]),
                             reads=[("mm", ma)], writes=[("stg", si)])
                    P.dma("sp", D[name][r0:r0 + 128, tok], stg[si][:, 0:CH], reads=[("stg", si)])


def build_proj(S):
    nc = bass.Bass("TRN2", target_bir_lowering=False)
    D = proj_decl(nc, S, "ExternalOutput")
    with ExitStack() as es:
        P = Prog(nc, es)
        emit_proj(P, S, D)
        P.finish()
    return nc, D


def core_coords(c):
    return c // 4, (c % 4) // 2, c % 2, c % 4


def _swap(a):
    return np.concatenate([a[64:], a[:64]])


def proj_weight_layout(w, g, j):
    cols = []
    for r in range(8):
        h = g * 8 + r
        cols.append(np.arange(h * 128, (h + 1) * 128))
    for r in range(8):
        h = g * 8 + r
        cols.append(_swap(np.arange(h * 128, (h + 1) * 128)))
    kv = lambda off: np.arange(off + g * 128, off + (g + 1) * 128)
    cols += [kv(OFF_KS), _swap(kv(OFF_KS)), kv(OFF_KW), _swap(kv(OFF_KW)), kv(OFF_KC), kv(OFF_VC)]
    for i in range(4):
        cols.append(np.arange(OFF_X + 512 * j + i * 128, OFF_X + 512 * j + (i + 1) * 128))
    for i in range(2):
        cols.append(np.arange(OFF_B + 256 * j + i * 128, OFF_B + 256 * j + (i + 1) * 128))
    for i in range(2):
        cols.append(np.arange(OFF_C + 256 * j + i * 128, OFF_C + 256 * j + (i + 1) * 128))
    wf = np.zeros((NFB, 128, KC, 128), np.float32)
    for fb, cc in enumerate(cols):
        wf[fb] = w[:, cc].reshape(KC, 128, 128).transpose(1, 0, 2)
    wf[30, :, :, 0:8] = w[:, OFF_DT + 8 * j:OFF_DT + 8 * j + 8].reshape(KC, 128, 8).transpose(1, 0, 2)
    tc = np.concatenate([kv(OFF_VS), kv(OFF_VW), np.arange(OFF_GATE + g * 24, OFF_GATE + (g + 1) * 24)])
    wt = np.zeros((128, KC, 288), np.float32)
    wt[:, :, 0:280] = w[:, tc].reshape(KC, 128, 280).transpose(1, 0, 2)
    return wf.reshape(NFB, 128, KC * 128), wt.reshape(128, KC * 288)


def rope_consts():
    half = 64
    inv = (10000.0 ** (-np.arange(half, dtype=np.float32) / half)).astype(np.float32)
    c = np.zeros((128, 2), np.float32)
    c[:, 0] = np.concatenate([inv, inv])
    c[:64, 1] = -1.0
    c[64:, 1] = 1.0
    return c


def my_tiles(a, par):
    s = a.shape[0]
    v = a.reshape((s // 256, 2, 128) + a.shape[1:])
    return np.ascontiguousarray(v[:, par].reshape((s // 2,) + a.shape[1:]))


IDENT_BF = np.eye(128, dtype=np.float32).astype(NPBF)


def ssd_decl(nc, S, kind_in, kind_out, D=None):
    dt = nc.dram_tensor
    D = {} if D is None else D
    if "XBCT" not in D:
        D["XBCT"] = dt("XBCT", [1024, S], BF16, kind=kind_in).ap()
        D["DTT"] = dt("DTT", [8, S], F32, kind=kind_in).ap()
    D["convp"] = dt("convp", [128, 8, 5], F32, kind="ExternalInput").ap()
    D["ssmp"] = dt("ssmp", [8, 2], F32, kind="ExternalInput").ap()
    D["dskip"] = dt("dskip", [8], F32, kind="ExternalInput").ap()
    D["negm"] = dt("negm", [128, 128], F32, kind="ExternalInput").ap()
    D["onehot"] = dt("onehot", [8, 1024], F32, kind="ExternalInput").ap()
    D["id8"] = dt("id8", [8, 8], F32, kind="ExternalInput").ap()
    if "ident" not in D:
        D["ident"] = dt("ident", [128, 128], BF16, kind="ExternalInput").ap()
    D["YS"] = dt("YS", [S, 512], BF16, kind=kind_out).ap()
    return D


def emit_ssd(P, S, D, pfx="s"):
    SC = min(2048, S)
    NSC = S // SC
    NCH = SC // 128
    K = lambda *a: (pfx,) + a
    idn = P.sb([128, 128], BF16)
    id8 = P.sb([8, 8], F32)
    convp = P.sb([128, 8, 5], F32)
    ssmp = P.sb([8, 2], F32)
    negm = P.sb([128, 128], F32)
    onehot = P.sb([8, 1024], F32)
    dsk = P.sb([128, 8], F32)
    P.dma("sp", idn[:], D["ident"][:, :], writes=[K("idn")])
    P.dma("sp", id8[:], D["id8"][:, :], writes=[K("id8")])
    P.dma("sp", convp[:], D["convp"][:, :, :], writes=[K("convp")])
    P.dma("sp", ssmp[:], D["ssmp"][:, :], writes=[K("ssmp")])
    P.dma("sp", negm[:], D["negm"][:, :], writes=[K("negm")])
    P.dma("sp", onehot[:], D["onehot"][:, :], writes=[K("onehot")])
    P.dma("sp", dsk[:], D["dskip"].partition_broadcast(128), writes=[K("dsk")])
    aneg = P.sb([8, 1], F32)
    P.op("act", lambda e: e.activation(out=aneg[:], in_=ssmp[:, 1:2], func=AF.Exp), reads=[K("ssmp")],
         writes=[K("aneg")])
    P.op("dve", lambda e: e.tensor_scalar(out=aneg[:], in0=aneg[:], scalar1=-1.0, scalar2=None, op0=ALU.mult),
         reads=[K("aneg")], writes=[K("aneg")])
    raw = P.sb([128, 8, SC + 4], BF16)
    acc = [P.sb([128, SC], F32) for _ in range(2)]
    cv = P.sb([128, 8, SC], BF16)
    dtf = P.sb([8, SC], F32)
    daf = P.sb([8, SC], F32)
    cum = P.sb([8, SC], F32)
    ones8 = P.sb([8, 128], F32)
    P.op("pool", lambda e: e.memset(ones8[:], 1.0), writes=[K("ones8")])
    P.op("pool", lambda e: e.memset(raw[:, :, 0:4], 0.0), writes=[K("raw")])
    state = [P.sb([128, 64], F32) for _ in range(8)]
    stbf = [P.sb([128, 64], BF16) for _ in range(8)]
    for h in range(8):
        P.op("pool", lambda e, h=h: e.memset(state[h][:], 0.0), writes=[K("state", h)])
        P.op("pool", lambda e, h=h: e.memset(stbf[h][:], 0.0), writes=[K("stbf", h)])
    xb_tok = [P.sb([128, 768], BF16) for _ in range(2)]
    dtc = [P.sb([128, 24], F32) for _ in range(2)]
    lastbc = [P.sb([128, 8], F32) for _ in range(2)]
    elast = [P.sb([128, 8], F32) for _ in range(2)]
    eend = [P.sb([128, 8], F32) for _ in range(2)]
    ecum = [P.sb([128, 8], F32) for _ in range(2)]
    tmpd = [P.sb([128, 128], F32) for _ in range(2)]
    dec = [P.sb([128, 128], F32) for _ in range(2)]
    WT = [P.sb([128, 128], BF16) for _ in range(2)]
    xdt = [P.sb([128, 64], BF16) for _ in range(2)]
    xend = [P.sb([128, 64], BF16) for _ in range(2)]
    t1 = [P.sb([128, 64], F32) for _ in range(2)]
    yt = [P.sb([128, 512], BF16) for _ in range(2)]
    pA = [P.ps([128, 128], F32) for _ in range(2)]
    pCB = P.ps([128, 256], F32)
    pC = [P.ps([128, 128], F32) for _ in range(2)]
    pD = P.ps([128, 64], F32)
    pT = P.ps([128, 768], BF16)
    pT2 = P.ps([128, 16], F32)
    cnt = {}

    def nxt(k, n=2):
        v = cnt.get(k, 0)
        cnt[k] = v + 1
        return v % n

    for sc in range(NSC):
        s0 = sc * SC
        if sc > 0:
            P.op("pool", lambda e: e.tensor_copy(out=raw[:, :, 1:4], in_=raw[:, :, SC + 1:SC + 4]),
                 reads=[K("raw")], writes=[K("raw")])
        P.dma("sp", raw[:, :, 4:SC + 4], D["XBCT"][:, s0:s0 + SC].rearrange("(b p) t -> p b t", p=128),
              writes=[K("raw")])
        for b in range(8):
            ai = nxt("acc")
            P.op("dve", lambda e, b=b, ai=ai: e.tensor_scalar(
                out=acc[ai][:], in0=raw[:, b, 1:SC + 1], scalar1=convp[:, b, 0:1], scalar2=convp[:, b, 4:5],
                op0=ALU.mult, op1=ALU.add), reads=[K("raw"), K("convp")], writes=[K("acc", ai)])
            for k in range(1, 4):
                P.op("dve", lambda e, b=b, ai=ai, k=k: e.scalar_tensor_tensor(
                    out=acc[ai][:], in0=raw[:, b, 1 + k:SC + 1 + k], scalar=convp[:, b, k:k + 1], in1=acc[ai][:],
                    op0=ALU.mult, op1=ALU.add), reads=[K("raw"), K("convp"), K("acc", ai)], writes=[K("acc", ai)])
            P.op("act", lambda e, b=b, ai=ai: e.activation(out=cv[:, b, :], in_=acc[ai][:], func=AF.Silu),
                 reads=[K("acc", ai)], writes=[K("cv", b)])
        P.dma("sp", dtf[:], D["DTT"][:, s0:s0 + SC], writes=[K("dtf")])
        P.op("act", lambda e: e.activation(out=dtf[:], in_=dtf[:], func=AF.Exp, bias=ssmp[:, 0:1]),
             reads=[K("dtf"), K("ssmp")], writes=[K("dtf")])
        P.op("act", lambda e: e.activation(out=dtf[:], in_=dtf[:], func=AF.Ln, bias=1.0),
             reads=[K("dtf")], writes=[K("dtf")])
        P.op("dve", lambda e: e.tensor_scalar(out=daf[:], in0=dtf[:], scalar1=aneg[:, 0:1], scalar2=None,
                                              op0=ALU.mult), reads=[K("dtf"), K("aneg")], writes=[K("daf")])
        for c in range(NCH):
            cs = slice(c * 128, (c + 1) * 128)
            P.op("dve", lambda e, cs=cs: e.tensor_tensor_scan(out=cum[:, cs], data0=ones8[:], data1=daf[:, cs],
                                                              initial=0.0, op0=ALU.mult, op1=ALU.add),
                 reads=[K("daf"), K("ones8")], writes=[K("cum")])
        cv_all = [K("cv", b) for b in range(8)]
        for c in range(NCH):
            cs = slice(c * 128, (c + 1) * 128)
            rows = slice(s0 + c * 128, s0 + (c + 1) * 128)
            for b in range(6):
                P.op("pe", lambda e, b=b, cs=cs: e.transpose(out=pT[:, b * 128:(b + 1) * 128], in_=cv[:, b, cs],
                                                            identity=idn[:]),
                     reads=[K("cv", b), K("idn")], writes=[K("pT")])
            xi = nxt("xb")
            P.op("act", lambda e, xi=xi: e.activation(out=xb_tok[xi][:], in_=pT[:], func=AF.Copy),
                 reads=[K("pT")], writes=[K("xb", xi)])
            P.op("pe", lambda e, cs=cs: e.transpose(out=pT2[:, 0:8], in_=dtf[:, cs], identity=id8[:]),
                 reads=[K("dtf"), K("id8")], writes=[K("pT2")])
            P.op("pe", lambda e, cs=cs: e.transpose(out=pT2[:, 8:16], in_=cum[:, cs], identity=id8[:]),
                 reads=[K("cum"), K("id8")], writes=[K("pT2")])
            di = nxt("dtc")
            P.op("dve", lambda e, di=di: e.tensor_copy(out=dtc[di][:, 0:16], in_=pT2[:]),
                 reads=[K("pT2")], writes=[K("dtc", di)])
            P.op("dve", lambda e, di=di: e.tensor_scalar(out=dtc[di][:, 16:24], in0=dtc[di][:, 8:16], scalar1=-1.0,
                                                        scalar2=None, op0=ALU.mult),
                 reads=[K("dtc", di)], writes=[K("dtc", di)])
            P.op("act", lambda e, di=di: e.activation(out=ecum[di][:], in_=dtc[di][:, 8:16], func=AF.Exp),
                 reads=[K("dtc", di)], writes=[K("ecum", di)])
            for g in range(2):
                P.op("pe", lambda e, g=g, cs=cs: e.matmul(pCB[:, g * 128:(g + 1) * 128], lhsT=cv[:, 4 + g, cs],
                                                         rhs=cv[:, 6 + g, cs], start=True, stop=True),
                     reads=[K("cv", 4 + g), K("cv", 6 + g)], writes=[K("pCB", g)])
            yi = nxt("yt")
            def emit_pA(h, cs=cs):
                ai = nxt("pA")
                P.op("pe", lambda e, h=h, ai=ai, cs=cs: e.matmul(pA[ai][:], lhsT=onehot[:, h * 128:(h + 1) * 128],
                                                               rhs=cum[:, cs], start=True, stop=True),
                     reads=[K("onehot"), K("cum")], writes=[K("pA", ai)])
                return ai

            ai_next = emit_pA(0)
            for h in range(8):
                g = h // 4
                ai = ai_next
                if h < 7:
                    ai_next = emit_pA(h + 1)
                ti = nxt("tmpd")
                P.op("dve", lambda e, ai=ai, ti=ti: e.tensor_tensor(out=tmpd[ti][:], in0=pA[ai][:], in1=negm[:],
                                                                    op=ALU.add),
                     reads=[K("pA", ai), K("negm")], writes=[K("tmpd", ti)])
                P.op("dve", lambda e, ai=ai, di=di, h=h: e.tensor_copy(out=lastbc[di][:, h:h + 1],
                                                                      in_=pA[ai][:, 127:128]),
                     reads=[K("pA", ai)], writes=[K("lastbc", di, h)])
                P.op("act", lambda e, ti=ti, di=di, h=h: e.activation(out=dec[ti][:], in_=tmpd[ti][:], func=AF.Exp,
                                                                     bias=dtc[di][:, 16 + h:17 + h]),
                     reads=[K("tmpd", ti), K("dtc", di)], writes=[K("dec", ti)])
                P.op("dve", lambda e, ti=ti, g=g: e.tensor_tensor(out=WT[ti][:], in0=dec[ti][:],
                                                                 in1=pCB[:, g * 128:(g + 1) * 128], op=ALU.mult),
                     reads=[K("dec", ti), K("pCB", g)], writes=[K("WT", ti)])
                xi2 = nxt("xdt")
                P.op("dve", lambda e, xi=xi, xi2=xi2, di=di, h=h: e.tensor_scalar(
                    out=xdt[xi2][:], in0=xb_tok[xi][:, h * 64:(h + 1) * 64], scalar1=dtc[di][:, h:h + 1],
                    scalar2=None, op0=ALU.mult), reads=[K("xb", xi), K("dtc", di)], writes=[K("xdt", xi2)])
                ci = nxt("pC")
                P.op("pe", lambda e, ti=ti, xi2=xi2, ci=ci: e.matmul(pC[ci][:, 0:64], lhsT=WT[ti][:], rhs=xdt[xi2][:],
                                                                    start=True, stop=True),
                     reads=[K("WT", ti), K("xdt", xi2)], writes=[K("pC", ci)])
                P.op("pe", lambda e, g=g, h=h, ci=ci, cs=cs: e.matmul(pC[ci][:, 64:128], lhsT=cv[:, 6 + g, cs],
                                                                     rhs=stbf[h][:], start=True, stop=True),
                     reads=[K("cv", 6 + g), K("stbf", h)], writes=[K("pC", ci)])
                P.op("dve", lambda e, ci=ci, ti=ti, di=di, h=h: e.scalar_tensor_tensor(
                    out=t1[ti][:], in0=pC[ci][:, 64:128], scalar=ecum[di][:, h:h + 1], in1=pC[ci][:, 0:64],
                    op0=ALU.mult, op1=ALU.add) if False else e.tensor_scalar(
                    out=t1[ti][:], in0=pC[ci][:, 64:128], scalar1=ecum[di][:, h:h + 1], scalar2=None, op0=ALU.mult),
                    reads=[K("pC", ci), K("ecum", di)], writes=[K("t1", ti)])
                P.op("dve", lambda e, ci=ci, ti=ti: e.tensor_tensor(out=t1[ti][:], in0=t1[ti][:], in1=pC[ci][:, 0:64],
                                                                    op=ALU.add),
                     reads=[K("pC", ci), K("t1", ti)], writes=[K("t1", ti)])
                P.op("dve", lambda e, xi=xi, ti=ti, yi=yi, h=h: e.scalar_tensor_tensor(
                    out=yt[yi][:, h * 64:(h + 1) * 64], in0=xb_tok[xi][:, h * 64:(h + 1) * 64],
                    scalar=dsk[:, h:h + 1], in1=t1[ti][:], op0=ALU.mult, op1=ALU.add),
                    reads=[K("xb", xi), K("dsk"), K("t1", ti)], writes=[K("yt", yi)])
                P.op("act", lambda e, di=di, h=h: e.activation(out=eend[di][:, h:h + 1], in_=dtc[di][:, 16 + h:17 + h],
                                                              func=AF.Exp, bias=lastbc[di][:, h:h + 1]),
                     reads=[K("dtc", di), K("lastbc", di, h)], writes=[K("eend", di, h)])
                P.op("act", lambda e, di=di, h=h: e.activation(out=elast[di][:, h:h + 1], in_=lastbc[di][:, h:h + 1],
                                                              func=AF.Exp),
                     reads=[K("lastbc", di, h)], writes=[K("elast", di, h)])
                P.op("dve", lambda e, xi2=xi2, di=di, h=h: e.tensor_scalar(
                    out=xend[xi2][:], in0=xdt[xi2][:], scalar1=eend[di][:, h:h + 1], scalar2=None, op0=ALU.mult),
                    reads=[K("xdt", xi2), K("eend", di, h)], writes=[K("xend", xi2)])
                P.op("pe", lambda e, xi=xi, xi2=xi2, g=g: e.matmul(pD[:], lhsT=xb_tok[xi][:, 512 + g * 128:640 + g * 128],
                                                                  rhs=xend[xi2][:], start=True, stop=True),
                     reads=[K("xb", xi), K("xend", xi2)], writes=[K("pD")])
                P.op("dve", lambda e, di=di, h=h: e.scalar_tensor_tensor(
                    out=state[h][:], in0=state[h][:], scalar=elast[di][:, h:h + 1], in1=pD[:],
                    op0=ALU.mult, op1=ALU.add), reads=[K("state", h), K("elast", di, h), K("pD")],
                    writes=[K("state", h)])
                P.op("pool", lambda e, h=h: e.tensor_copy(out=stbf[h][:], in_=state[h][:]),
                     reads=[K("state", h)], writes=[K("stbf", h)])
            P.dma("sp", D["YS"][rows, :], yt[yi][:], reads=[K("yt", yi)])


def build_ssd(S):
    nc = bass.Bass("TRN2", target_bir_lowering=False)
    D = ssd_decl(nc, S, "ExternalInput", "ExternalOutput")
    with ExitStack() as es:
        P = Prog(nc, es)
        emit_ssd(P, S, D)
        P.finish()
    return nc, D


def ssd_consts():
    l = np.arange(128)
    negm = np.where(l[None, :] >= l[:, None], 0.0, -1e30).astype(np.float32)
    onehot = np.zeros((8, 1024), np.float32)
    for h in range(8):
        onehot[h, h * 128:(h + 1) * 128] = 1.0
    return dict(negm=negm, onehot=onehot, id8=np.eye(8, dtype=np.float32))


def ssd_param_layout(conv_w, conv_b, dt_bias, a_log, d_skip, j):
    ch = np.concatenate([np.arange(512 * j, 512 * (j + 1)), 2048 + np.arange(256 * j, 256 * (j + 1)),
                         3072 + np.arange(256 * j, 256 * (j + 1))])
    cp = np.zeros((128, 8, 5), np.float32)
    cp[:, :, 0:4] = conv_w[:, ch].T.reshape(8, 128, 4).transpose(1, 0, 2)
    cp[:, :, 4] = conv_b[ch].reshape(8, 128).T
    hs = slice(8 * j, 8 * j + 8)
    ssmp = np.stack([dt_bias[hs], a_log[hs]], 1).astype(np.float32)
    return dict(convp=cp, ssmp=np.ascontiguousarray(ssmp), dskip=np.ascontiguousarray(d_skip[hs]))


def build_cmp(S):
    NCP = S // 16
    NC = NCP - 1
    nc = bass.Bass("TRN2", target_bir_lowering=False)
    dt = nc.dram_tensor
    KCT = dt("KCT", [128, S], BF16, kind="ExternalInput").ap()
    VCT = dt("VCT", [128, S], BF16, kind="ExternalInput").ap()
    w1 = {n: dt("w1" + n, [32, 128, 256], F32, kind="ExternalInput").ap() for n in "kv"}
    w2 = {n: dt("w2" + n, [256, 128], F32, kind="ExternalInput").ap() for n in "kv"}
    w2s = dt("w2ks", [256, 128], F32, kind="ExternalInput").ap()
    peT = {n: dt("peT" + n, [128, 32], F32, kind="ExternalInput").ap() for n in "kv"}
    posc = dt("posc", [NCP], I32, kind="ExternalInput").ap()
    invf_d = dt("invf", [128, 2], F32, kind="ExternalInput").ap()
    KCMPT = dt("KCMPT", [128, NCP], BF16, kind="ExternalOutput").ap()
    VCMP = dt("VCMP", [NCP, 128], BF16, kind="ExternalOutput").ap()
    with ExitStack() as es:
        P = Prog(nc, es)
        invf = P.sb([128, 2], F32)
        P.dma("sp", invf[:], invf_d[:, :], writes=["invf"])
        posi = P.sb([128, NCP], I32)
        posf = P.sb([128, NCP], F32)
        ang = P.sb([128, NCP], F32)
        kf = P.sb([128, NCP], F32)
        tmp = P.sb([128, NCP], F32)
        cosT = P.sb([128, NCP], F32)
        sinT = P.sb([128, NCP], F32)
        P.dma("sp", posi[:], posc.partition_broadcast(128), writes=["posi"])
        P.op("dve", lambda e: e.tensor_copy(out=posf[:], in_=posi[:]), reads=["posi"], writes=["posf"])
        rope_tables(P, posf[:], "posf", invf, ang[:], kf[:], tmp[:], cosT[:], sinT[:], NCP, "rp")
        raw = {n: P.sb([128, S], BF16) for n in "kv"}
        P.dma("sp", raw["k"][:], KCT[:, :], writes=[("raw", "k")])
        P.dma("sp", raw["v"][:], VCT[:, :], writes=[("raw", "v")])
        w1s = {n: P.sb([128, 32, 256], BF16) for n in "kv"}
        w2sb = {n: P.sb([128, 2, 128], BF16) for n in ("k", "v", "ks")}
        pes = {n: P.sb([128, 32], BF16) for n in "kv"}
        for n in "kv":
            for half in range(2):
                P.dma("pool", w1s[n][:, half * 16:(half + 1) * 16, :],
                      w1[n][half * 16:(half + 1) * 16].rearrange("p d h -> d p h"), writes=[("w1", n)])
            P.dma("pool", w2sb[n][:], w2[n].rearrange("(c p) d -> p c d", p=128), writes=[("w2", n)])
            P.dma("pool", pes[n][:], peT[n][:, :], writes=[("pe", n)])
        P.dma("pool", w2sb["ks"][:], w2s.rearrange("(c p) d -> p c d", p=128), writes=[("w2", "ks")])
        sil = {n: P.sb([128, 2, NCP], BF16) for n in "kv"}
        bias = P.sb([128, 4], F32)
        pb = P.ps([128, 4], F32)
        ph = [P.ps([128, 512], F32) for _ in range(2)]
        pk = [P.ps([128, 512], F32) for _ in range(2)]
        t1 = P.sb([128, 512], F32)
        t2 = P.sb([128, 512], F32)
        ko = P.sb([128, NCP], BF16)
        vo = P.sb([128, 128], BF16)
        P.op("pool", lambda e: e.memset(ko[:], 0.0), writes=["ko"])
        for n in "kv":
            P.op("pool", lambda e, n=n: e.memset(sil[n][:], 0.0), writes=[("sil", n)])
        halves = [(a, min(a + 512, NC)) for a in range(0, NC, 512)]
        cnt = [0]
        for ni, n in enumerate("kv"):
            for hh in range(2):
                col = ni * 2 + hh
                for p in range(32):
                    P.op("pe", lambda e, n=n, hh=hh, p=p, col=col: e.matmul(
                        pb[:, col:col + 1], lhsT=w1s[n][:, p, hh * 128:(hh + 1) * 128], rhs=pes[n][:, p:p + 1],
                        start=(p == 0), stop=(p == 31)), reads=[("w1", n), ("pe", n)], writes=["pb"])
                P.op("dve", lambda e, col=col: e.tensor_copy(out=bias[:, col:col + 1], in_=pb[:, col:col + 1]),
                     reads=["pb"], writes=[("bias", col)])
                for (a, b) in halves:
                    pi = cnt[0] % 2
                    cnt[0] += 1
                    nn = b - a
                    for p in range(32):
                        P.op("pe", lambda e, n=n, hh=hh, p=p, a=a, nn=nn, pi=pi: e.matmul(
                            ph[pi][:, 0:nn], lhsT=w1s[n][:, p, hh * 128:(hh + 1) * 128],
                            rhs=raw[n][:, 16 * a + p:16 * a + p + 16 * (nn - 1) + 1:16],
                            start=(p == 0), stop=(p == 31)), reads=[("w1", n), ("raw", n)], writes=[("ph", pi)])
                    P.op("act", lambda e, n=n, hh=hh, a=a, b=b, nn=nn, pi=pi, col=col: e.activation(
                        out=sil[n][:, hh, a:b], in_=ph[pi][:, 0:nn], func=AF.Silu, bias=bias[:, col:col + 1]),
                        reads=[("ph", pi), ("bias", col)], writes=[("sil", n)])
        for (a, b) in halves:
            nn = b - a
            for vi, wn in enumerate(("k", "ks")):
                for hh in range(2):
                    P.op("pe", lambda e, wn=wn, hh=hh, a=a, b=b, nn=nn, vi=vi: e.matmul(
                        pk[vi][:, 0:nn], lhsT=w2sb[wn][:, hh, :], rhs=sil["k"][:, hh, a:b],
                        start=(hh == 0), stop=(hh == 1)), reads=[("w2", wn), ("sil", "k")], writes=[("pk", vi)])
            P.op("dve", lambda e, a=a, b=b, nn=nn: e.tensor_tensor(out=t1[:, 0:nn], in0=pk[0][:, 0:nn],
                                                                   in1=cosT[:, a:b], op=ALU.mult),
                 reads=[("pk", 0), ("rp", "cos")], writes=["t1"])
            P.op("dve", lambda e, a=a, b=b, nn=nn: e.tensor_tensor(out=t2[:, 0:nn], in0=pk[1][:, 0:nn],
                                                                   in1=sinT[:, a:b], op=ALU.mult),
                 reads=[("pk", 1), ("rp", "sin")], writes=["t2"])
            P.op("dve", lambda e, a=a, b=b, nn=nn: e.tensor_tensor(out=ko[:, a:b], in0=t1[:, 0:nn], in1=t2[:, 0:nn],
                                                                   op=ALU.add),
                 reads=["t1", "t2"], writes=["ko"])
        P.dma("sp", KCMPT[:, :], ko[:], reads=["ko"])
        for ct in range(NCP // 128):
            for hh in range(2):
                P.op("pe", lambda e, hh=hh, ct=ct: e.matmul(
                    pk[0][:, 0:128], lhsT=sil["v"][:, hh, ct * 128:(ct + 1) * 128], rhs=w2sb["v"][:, hh, :],
                    start=(hh == 0), stop=(hh == 1)), reads=[("w2", "v"), ("sil", "v")], writes=[("pk", 0)])
            P.op("dve", lambda e: e.tensor_copy(out=vo[:], in_=pk[0][:, 0:128]), reads=[("pk", 0)], writes=["vo"])
            P.dma("sp", VCMP[ct * 128:(ct + 1) * 128, :], vo[:], reads=["vo"])
        P.finish()
    return nc


SCALE = 128 ** -0.5
BIGV = 1.0e4


def attn_consts(par):
    p = np.arange(128)[:, None]
    q = np.arange(128)[None, :]
    mc = np.zeros((128, 8, 128), np.float32)
    for r in range(8):
        mc[:, r, :] = (16 * (p - 16 * r - 8 * par) + 31 <= q)
    mcp = np.ones((128, 128), np.float32)
    mcp[127, :] = (15 - 128 * par <= np.arange(128))
    A = np.zeros((128, 8, 256), np.float32)
    blk = np.arange(256)[None, :]
    for jc in range(8):
        c = 128 * jc + p
        A[:, jc, :] = (4 * blk - 1 <= c) & (c <= 4 * blk + 3)
    qq = np.arange(128)[:, None]
    d = np.arange(512)[None, :] - 256
    curd = 2 * par + (qq >= 64)
    bc = np.where((d == curd) | (d == curd - 1), BIGV, np.where(d > curd, -BIGV, 0.0)).astype(np.float32)
    tri = (p <= q).astype(np.float32)
    gtr = (p > q).astype(np.float32)
    one = np.ones((128, 128), np.float32)
    zero = np.zeros((128, 128), np.float32)
    cms = np.stack([tri, zero] if par == 0 else [one, tri], 1)
    cmw = np.stack([gtr, one, one, one, tri, zero] if par == 0 else [zero, gtr, one, one, one, tri], 1)
    y = np.arange(8192)[None, :]
    G = (y // 64 == np.arange(128)[:, None]).astype(np.float32)
    f = lambda a: np.ascontiguousarray(a).astype(NPBF)
    return dict(mc=f(mc), mcp=f(mcp), amat=f(A), bcst=bc, cms=f(cms), cmw=f(cmw), gexp=f(G),
                zeros=np.zeros((128, 512), NPBF))


def build_attn(S):
    SQ = S // 2
    NQ = SQ // 128
    NKT = S // 128
    NCP = S // 16
    NCT = max(1, NCP // 128)
    nc = bass.Bass("TRN2", target_bir_lowering=False)
    dt = nc.dram_tensor
    inp = lambda n, sh, d: dt(n, sh, d, kind="ExternalInput").ap()
    QT = inp("QT", [8, 128, SQ], BF16)
    KST = inp("KST", [128, S], BF16)
    KWT = inp("KWT", [128, S], BF16)
    VG = inp("VG", [S, 288], BF16)
    GQ = inp("GQ", [SQ, 32], BF16)
    KCMPT = inp("KCMPT", [128, NCP], BF16)
    VCMP = inp("VCMP", [NCP, 128], BF16)
    mc_d = inp("mc", [128, 8, 128], BF16)
    mcp_d = inp("mcp", [128, 128], BF16)
    a_d = inp("amat", [128, 8, 256], BF16)
    bc_d = inp("bcst", [128, 512], F32)
    cms_d = inp("cms", [128, 2, 128], BF16)
    cmw_d = inp("cmw", [128, 6, 128], BF16)
    g_d = inp("gexp", [128, 8192], BF16)
    z_d = inp("zeros", [128, 512], BF16)
    id_d = inp("ident", [128, 128], BF16)
    OA = dt("OA", [SQ, 1024], BF16, kind="ExternalOutput").ap()
    with ExitStack() as es:
        P = Prog(nc, es)
        kst = P.sb([128, S], BF16)
        kwt = P.sb([128, S], BF16)
        vs1 = P.sb([128, NKT, 129], BF16)
        vw1 = P.sb([128, NKT, 129], BF16)
        kcm = P.sb([128, NCP], BF16)
        vc1 = P.sb([128, NCT, 129], BF16)
        mc = P.sb([128, 8, 128], BF16)
        mcp = P.sb([128, 128], BF16)
        amat = P.sb([128, 8, 256], BF16)
        bcst = P.sb([128, 512], F32)
        cms = P.sb([128, 2, 128], BF16)
        cmw = P.sb([128, 6, 128], BF16)
        gexp = P.sb([128, 8192], BF16)
        zeros = P.sb([128, 512], BF16)
        idn = P.sb([128, 128], BF16)
        for t_, d_, k_ in ((kst, KST, "kst"), (kwt, KWT, "kwt"), (kcm, KCMPT, "kcm"), (mcp, mcp_d, "mcp"),
                           (bcst, bc_d, "bcst"), (gexp, g_d, "gexp"), (zeros, z_d, "zeros"), (idn, id_d, "idn")):
            P.dma("sp", t_[:], d_[:, :], writes=[k_])
        for t_, d_, k_ in ((mc, mc_d, "mc"), (amat, a_d, "amat"), (cms, cms_d, "cms"), (cmw, cmw_d, "cmw")):
            P.dma("sp", t_[:], d_[:, :, :], writes=[k_])
        for t_, k_ in ((vs1, "vs1"), (vw1, "vw1"), (vc1, "vc1")):
            P.op("pool", lambda e, t_=t_: e.memset(t_[:], 1.0), writes=[k_])
        P.dma("sp", vs1[:, :, 0:128], VG[:, 0:128].rearrange("(t p) d -> p t d", p=128), writes=["vs1"])
        P.dma("sp", vw1[:, :, 0:128], VG[:, 128:256].rearrange("(t p) d -> p t d", p=128), writes=["vw1"])
        if NCP >= 128:
            P.dma("sp", vc1[:, :, 0:128], VCMP.rearrange("(t p) d -> p t d", p=128), writes=["vc1"])
        qT = [P.sb([128, 8, 128], BF16) for _ in range(2)]
        gq = [P.sb([128, 32], BF16) for _ in range(2)]
        et = P.sb([128, NCT, 1024], BF16)
        pt = [P.sb([128, 1024], BF16) for _ in range(3)]
        mk = [P.sb([128, 128], BF16) for _ in range(3)]
        imp = P.sb([128, 256], F32)
        sc2 = P.sb([128, 256], F32)
        m8 = P.sb([128, 16], F32)
        sel = P.sb([128, 256], BF16)
        selT = P.sb([128, 2, 128], BF16)
        zall = P.sb([128, 8], F32)
        coef = P.sb([128, 8], F32)
        oacc = P.sb([128, 1024], F32)
        obf = [P.sb([128, 1024], BF16) for _ in range(2)]
        ST = [P.ps([128, 1024], F32) for _ in range(2)]
        OB = [P.ps([128, 512], F32) for _ in range(3)]
        MS = P.ps([128, 512], F32)
        cnt = {}

        def nxt(k, n=2):
            v = cnt.get(k, 0)
            cnt[k] = v + 1
            return v % n

        def oreg(h):
            return OB[h // 3][:, (h % 3) * 129:(h % 3) * 129 + 129]

        def zero_acc():
            for b in range(3):
                P.op("pe", lambda e, b=b: e.matmul(OB[b][:], lhsT=zeros[:, 0:128], rhs=zeros[:], start=True,
                                                  stop=False, skip_group_check=True),
                     reads=["zeros"], writes=[("OB", b)])

        def scores(ktile_ap, qi, kkey):
            si = nxt("ST")
            for hf in range(2):
                P.op("pe", lambda e, hf=hf, si=si: e.matmul(
                    ST[si][:, hf * 512:(hf + 1) * 512], lhsT=ktile_ap, rhs=qT[qi][:, hf * 4:(hf + 1) * 4, :],
                    start=True, stop=True), reads=[kkey, ("qT", qi)], writes=[("ST", si)])
            return si

        def pv(p_ap_fn, pkey, v_ap, vkey, last):
            for h in range(8):
                P.op("pe", lambda e, h=h: e.matmul(oreg(h), lhsT=p_ap_fn(h), rhs=v_ap, start=False, stop=last,
                                                   skip_group_check=True),
                     reads=[pkey, vkey], writes=[("OB", h // 3)])

        def combine(br, qi, first):
            for b in range(3):
                nh = 3 if b < 2 else 2
                P.op("dve", lambda e, b=b, nh=nh: e.tensor_copy(
                    out=zall[:, b * 3:b * 3 + nh],
                    in_=OB[b][:, 0:nh * 129].rearrange("p (h c) -> p h c", c=129)[:, :, 128]),
                    reads=[("OB", b)], writes=["zall"])
            P.op("dve", lambda e: e.tensor_scalar(out=zall[:], in0=zall[:], scalar1=1e-30, scalar2=None,
                                                  op0=ALU.max), reads=["zall"], writes=["zall"])
            P.op("dve", lambda e: e.reciprocal(out=zall[:], in_=zall[:]), reads=["zall"], writes=["zall"])
            P.op("dve", lambda e: e.tensor_tensor(
                out=coef[:], in0=zall[:], in1=gq[qi][:, 0:24].rearrange("p (h c) -> p h c", c=3)[:, :, br],
                op=ALU.mult), reads=["zall", ("gq", qi)], writes=["coef"])
            for h in range(8):
                osl = oacc[:, h * 128:(h + 1) * 128]
                if first:
                    P.op("dve", lambda e, h=h, osl=osl: e.tensor_scalar(
                        out=osl, in0=oreg(h)[:, 0:128], scalar1=coef[:, h:h + 1], scalar2=None, op0=ALU.mult),
                        reads=[("OB", h // 3), "coef"], writes=["oacc"])
                else:
                    P.op("dve", lambda e, h=h, osl=osl: e.scalar_tensor_tensor(
                        out=osl, in0=oreg(h)[:, 0:128], scalar=coef[:, h:h + 1], in1=osl, op0=ALU.mult,
                        op1=ALU.add), reads=[("OB", h // 3), "coef", "oacc"], writes=["oacc"])

        def bmul(buf_ap, m_ap):
            return lambda e: e.tensor_tensor(out=buf_ap.rearrange("p (h q) -> p h q", h=8),
                                             in0=buf_ap.rearrange("p (h q) -> p h q", h=8),
                                             in1=m_ap.unsqueeze(1).to_broadcast([128, 8, 128]), op=ALU.mult)

        for i in range(NQ):
            qi = nxt("qT")
            P.dma("sp", qT[qi][:], QT[:, :, i * 128:(i + 1) * 128].rearrange("h d q -> d h q"), writes=[("qT", qi)])
            P.dma("sp", gq[qi][:], GQ[i * 128:(i + 1) * 128, :], writes=[("gq", qi)])
            jl = i // 8
            r = i % 8
            zero_acc()
            for jc in range(jl + 1):
                si = scores(kcm[:, jc * 128:(jc + 1) * 128], qi, "kcm")
                P.op("act", lambda e, si=si, jc=jc: e.activation(out=et[:, jc, :], in_=ST[si][:], func=AF.Exp,
                                                               scale=SCALE),
                     reads=[("ST", si)], writes=[("et", jc)])
                if jc == jl:
                    P.op("dve", bmul(et[:, jc, :], mc[:, r, :]), reads=[("et", jc), "mc"], writes=[("et", jc)])
                elif jc == jl - 1 and r == 0:
                    P.op("dve", bmul(et[:, jc, :], mcp[:]), reads=[("et", jc), "mcp"], writes=[("et", jc)])
            for jc in range(jl + 1):
                pv(lambda h, jc=jc: et[:, jc, h * 128:(h + 1) * 128], ("et", jc), vc1[:, jc, :], "vc1", jc == jl)
            combine(0, qi, True)
            P.op("dve", lambda e: e.tensor_scalar(out=coef[:], in0=zall[:], scalar1=1.0, scalar2=None, op0=ALU.mult),
                 reads=["zall"], writes=["coef2"]) if False else None
            for h in range(8):
                for jc in range(jl + 1):
                    P.op("pe", lambda e, h=h, jc=jc: e.matmul(MS[:, 0:256], lhsT=et[:, jc, h * 128:(h + 1) * 128],
                                                             rhs=amat[:, jc, :], start=(jc == 0), stop=(jc == jl)),
                         reads=[("et", jc), "amat"], writes=["MS"])
                if h == 0:
                    P.op("dve", lambda e: e.tensor_scalar(out=imp[:], in0=MS[:, 0:256], scalar1=zall[:, 0:1],
                                                          scalar2=None, op0=ALU.mult),
                         reads=["MS", "zall"], writes=["imp"])
                else:
                    P.op("dve", lambda e, h=h: e.scalar_tensor_tensor(
                        out=imp[:], in0=MS[:, 0:256], scalar=zall[:, h:h + 1], in1=imp[:], op0=ALU.mult,
                        op1=ALU.add), reads=["MS", "zall", "imp"], writes=["imp"])
            P.op("dve", lambda e, i=i: e.tensor_tensor(out=imp[:], in0=imp[:], in1=bcst[:, 256 - 4 * i:512 - 4 * i],
                                                      op=ALU.add), reads=["imp", "bcst"], writes=["imp"])
            P.op("dve", lambda e: e.memset(imp[:, 0:1], BIGV), reads=["imp"], writes=["imp"])
            P.op("dve", lambda e: e.max(out=m8[:, 0:8], in_=imp[:]), reads=["imp"], writes=["m8"])
            P.op("dve", lambda e: e.match_replace(out=sc2[:], in_to_replace=m8[:, 0:8], in_values=imp[:],
                                                  imm_value=-3.0e4), reads=["imp", "m8"], writes=["sc2"])
            P.op("dve", lambda e: e.max(out=m8[:, 8:16], in_=sc2[:]), reads=["sc2"], writes=["m8"])
            P.op("dve", lambda e: e.tensor_scalar(out=sel[:], in0=imp[:], scalar1=m8[:, 15:16], scalar2=None,
                                                  op0=ALU.is_ge), reads=["imp", "m8"], writes=["sel"])
            nchunk = 1 if (4 * i + 3) < 128 else 2
            MSb = MS[:].bitcast(BF16)
            for c in range(nchunk):
                P.op("pe", lambda e, c=c: e.transpose(out=MSb[:, c * 128:(c + 1) * 128],
                                                      in_=sel[:, c * 128:(c + 1) * 128], identity=idn[:]),
                     reads=["sel", "idn"], writes=["MS"])
            P.op("dve", lambda e, nchunk=nchunk: e.tensor_copy(
                out=selT[:, 0:nchunk, :], in_=MSb[:, 0:nchunk * 128].rearrange("p (c q) -> p c q", c=nchunk)),
                reads=["MS"], writes=["selT"])
            zero_acc()
            nkt = 2 * i + 2
            def sel_stage1(kt, i=i, qi=qi):
                si = scores(kst[:, kt * 128:(kt + 1) * 128], qi, "kst")
                pi = nxt("pt", 3)
                P.op("act", lambda e, si=si, pi=pi: e.activation(out=pt[pi][:], in_=ST[si][:], func=AF.Exp,
                                                               scale=SCALE),
                     reads=[("ST", si)], writes=[("pt", pi)])
                P.op("pe", lambda e, kt=kt: e.matmul(MS[:, 0:128], lhsT=gexp[:, (kt % 64) * 128:(kt % 64 + 1) * 128],
                                                    rhs=selT[:, kt // 64, :], start=True, stop=True),
                     reads=["gexp", "selT"], writes=["MS"])
                mi = nxt("mk", 3)
                if kt >= 2 * i:
                    P.op("dve", lambda e, mi=mi, kt=kt, i=i: e.tensor_tensor(out=mk[mi][:], in0=MS[:, 0:128],
                                                                          in1=cms[:, kt - 2 * i, :], op=ALU.mult),
                         reads=["MS", "cms"], writes=[("mk", mi)])
                else:
                    P.op("dve", lambda e, mi=mi: e.tensor_copy(out=mk[mi][:], in_=MS[:, 0:128]),
                         reads=["MS"], writes=[("mk", mi)])
                P.op("dve", bmul(pt[pi][:], mk[mi][:]), reads=[("pt", pi), ("mk", mi)], writes=[("pt", pi)])
                return pi

            pis = {kk: sel_stage1(kk) for kk in range(min(2, nkt))}
            for kt in range(nkt):
                if kt + 2 < nkt:
                    pis[kt + 2] = sel_stage1(kt + 2)
                pi = pis.pop(kt)
                pv(lambda h, pi=pi: pt[pi][:, h * 128:(h + 1) * 128], ("pt", pi), vs1[:, kt, :], "vs1",
                   kt == nkt - 1)
            combine(1, qi, False)
            zero_acc()
            wt_list = [(kt, w) for w, kt in enumerate(range(2 * i - 4, 2 * i + 2)) if kt >= 0]
            def win_stage1(kt, w, qi=qi):
                si = scores(kwt[:, kt * 128:(kt + 1) * 128], qi, "kwt")
                pi = nxt("pt", 3)
                P.op("act", lambda e, si=si, pi=pi: e.activation(out=pt[pi][:], in_=ST[si][:], func=AF.Exp,
                                                               scale=SCALE),
                     reads=[("ST", si)], writes=[("pt", pi)])
                P.op("dve", bmul(pt[pi][:], cmw[:, w, :]), reads=[("pt", pi), "cmw"], writes=[("pt", pi)])
                return pi

            pis = {kk: win_stage1(*wt_list[kk]) for kk in range(min(2, len(wt_list)))}
            for n_, (kt, w) in enumerate(wt_list):
                if n_ + 2 < len(wt_list):
                    pis[n_ + 2] = win_stage1(*wt_list[n_ + 2])
                pi = pis.pop(n_)
                pv(lambda h, pi=pi: pt[pi][:, h * 128:(h + 1) * 128], ("pt", pi), vw1[:, kt, :], "vw1",
                   n_ == len(wt_list) - 1)
            combine(2, qi, False)
            oi = nxt("obf")
            P.op("act", lambda e, oi=oi: e.activation(out=obf[oi][:], in_=oacc[:], func=AF.Copy),
                 reads=["oacc"], writes=[("obf", oi)])
            P.dma("sp", OA[i * 128:(i + 1) * 128, :], obf[oi][:], reads=[("obf", oi)])
        P.finish()
    return nc


_SW = np.concatenate([np.arange(64, 128), np.arange(64)])


def kernel(x, positions, norm_w, w_in, cmp_pe_k, cmp_w1_k, cmp_w2_k, cmp_pe_v, cmp_w1_v, cmp_w2_v,
           conv_w, conv_b, dt_bias, a_log, d_skip, norm_attn_w, norm_ssm_w, w_out, final_norm_w):
    f32 = lambda a: np.ascontiguousarray(np.asarray(a), dtype=np.float32)
    x = f32(x)
    positions = np.ascontiguousarray(np.asarray(positions), dtype=np.int32)
    norm_w, w_in, w_out = f32(norm_w), f32(w_in), f32(w_out)
    cmp_pe_k, cmp_w1_k, cmp_w2_k = f32(cmp_pe_k), f32(cmp_w1_k), f32(cmp_w2_k)
    cmp_pe_v, cmp_w1_v, cmp_w2_v = f32(cmp_pe_v), f32(cmp_w1_v), f32(cmp_w2_v)
    conv_w, conv_b, dt_bias, a_log, d_skip = f32(conv_w), f32(conv_b), f32(dt_bias), f32(a_log), f32(d_skip)
    norm_attn_w, norm_ssm_w, final_norm_w = f32(norm_attn_w), f32(norm_ssm_w), f32(final_norm_w)
    B, S, _ = x.shape
    cores = list(range(8))
    TPC = B * S // 8
    rc = rope_consts()
    ac = [attn_consts(0), attn_consts(1)]
    sc = ssd_consts()
    nc_proj, _ = build_proj(S)
    nc_cmp = build_cmp(S)
    nc_attn = build_attn(S)
    nc_ssd, _ = build_ssd(S)
    run = lambda nc, maps: run_bass_kernel_spmd(nc, maps, core_ids=cores).results
    for l in range(DEPTH):
        w = w_in[l]
        maps = []
        for c in cores:
            b, g, par, j = core_coords(c)
            wf, wt = proj_weight_layout(w, g, j)
            maps.append(dict(x=x[b], pos=positions[b], xq=my_tiles(x[b], par), posq=my_tiles(positions[b], par),
                             nw=norm_w[l], wf=wf, wt=wt, ident=IDENT_BF, invf=rc))
        rp = run(nc_proj, maps)
        del maps
        maps = []
        for c in cores:
            b, g, par, j = core_coords(c)
            pc = positions[b, 31::16]
            maps.append(dict(KCT=rp[c]["KCT"], VCT=rp[c]["VCT"], w1k=cmp_w1_k[l], w1v=cmp_w1_v[l],
                             w2k=cmp_w2_k[l], w2v=cmp_w2_v[l], w2ks=np.ascontiguousarray(cmp_w2_k[l][:, _SW]),
                             peTk=np.ascontiguousarray(cmp_pe_k[l].T), peTv=np.ascontiguousarray(cmp_pe_v[l].T),
                             posc=np.ascontiguousarray(np.concatenate([pc, pc[-1:]]), dtype=np.int32), invf=rc))
        rcm = run(nc_cmp, maps)
        maps = []
        for c in cores:
            b, g, par, j = core_coords(c)
            vg = np.asarray(rp[c]["VG"])
            m = dict(QT=rp[c]["QT"], KST=rp[c]["KST"], KWT=rp[c]["KWT"], VG=vg,
                     GQ=my_tiles(np.ascontiguousarray(vg[:, 256:288]), par),
                     KCMPT=rcm[c]["KCMPT"], VCMP=rcm[c]["VCMP"], ident=IDENT_BF)
            m.update(ac[par])
            maps.append(m)
        ra = run(nc_attn, maps)
        maps = []
        for c in cores:
            b, g, par, j = core_coords(c)
            m = dict(XBCT=rp[c]["XBCT"], DTT=rp[c]["DTT"], ident=IDENT_BF)
            m.update(sc)
            m.update(ssd_param_layout(conv_w[l], conv_b[l], dt_bias[l], a_log[l], d_skip[l], j))
            maps.append(m)
        rs = run(nc_ssd, maps)
        del rp
        oa = np.zeros((B, S, 2048), NPBF)
        ys = np.zeros((B, S, 2048), NPBF)
        for c in cores:
            b, g, par, j = core_coords(c)
            oa[b].reshape(S // 256, 2, 128, 2048)[:, par, :, g * 1024:(g + 1) * 1024] = \
                np.asarray(ra[c]["OA"]).reshape(S // 256, 128, 1024)
            ys[b, :, 512 * j:512 * (j + 1)] = np.asarray(rs[c]["YS"])
        xf = x.reshape(B * S, D_MODEL)
        oaf = oa.reshape(B * S, 2048)
        ysf = ys.reshape(B * S, 2048)
        wz = np.ascontiguousarray(np.concatenate([w[:, OFF_ZA:OFF_ZA + 2048], w[:, OFF_ZS:OFF_ZS + 2048]], 1))
        nc_out = build_out(TPC, l == DEPTH - 1)
        maps = []
        for c in cores:
            r = slice(c * TPC, (c + 1) * TPC)
            maps.append(dict(x=xf[r], oa=oaf[r], ys=ysf[r], wz=wz, wo=w_out[l], nw=norm_w[l], nwa=norm_attn_w[l],
                             nws=norm_ssm_w[l], fnw=final_norm_w, ident=IDENT_BF))
        ro = run(nc_out, maps)
        x = np.concatenate([np.asarray(ro[c]["y"]) for c in cores], 0).reshape(B, S, D_MODEL).astype(np.float32)
    return x
```
